# Optimizing a Trainium2 kernel written in Bass

```python
import jax, jax.numpy as jnp
from jax import lax
import numpy as np

D_MODEL = 1024
BATCH = 2
SEQ = 8192
DEPTH = 2

HEAD_DIM = 64
A_HEADS = 8
DILATED_PATTERNS = ((128, 1), (512, 4), (2048, 16))
B_Q_HEADS = 8
B_KV_HEADS = 2
B_HALF_WINDOW = 128
ATTN_BLOCK = 128
ROPE_THETA = 10000.0
C_GROUPS = 4
C_GROUP_CH = 128
C_CHUNK = 128
C_WIDTH = C_GROUPS * C_GROUP_CH
D_HEADS = 8
D_WIDTH = D_HEADS * HEAD_DIM
D_DECAY_LORA = 64
D_AAA_LORA = 64
D_GATE_LORA = 128
GN_EPS = 64e-5
MEM_LEN = 256
MEM_HEADS = 4
MEM_HEAD_DIM = D_MODEL // MEM_HEADS
N_EXPERTS = 16
CAPACITY_FACTOR = 2
D_EXPERT = 2816

NORM_EPS = 1e-6
NEG_INF = -1e30
A_WIDTH = A_HEADS * HEAD_DIM
B_WIDTH = B_Q_HEADS * HEAD_DIM
B_KV_WIDTH = B_KV_HEADS * HEAD_DIM
MIX_WIDTH = A_WIDTH + B_WIDTH
EVEN_IN = 3 * A_WIDTH + B_WIDTH + 2 * B_KV_WIDTH
D_SHIFT_WIDTH = 3 * D_WIDTH + 2 * D_DECAY_LORA + 2 * D_AAA_LORA + D_GATE_LORA
ODD_IN = 2 * C_WIDTH + D_SHIFT_WIDTH

kernel_name = "hybrid_dilated_window_gmlp_rwkv7_ecmoe"


def rmsnorm(t, g):
    tf = t.astype(jnp.float32)
    y = tf * lax.rsqrt(jnp.mean(tf * tf, axis=-1, keepdims=True) + NORM_EPS) * g
    return y.astype(t.dtype)


def layernorm(t, g, b):
    tf = t.astype(jnp.float32)
    mu = jnp.mean(tf, axis=-1, keepdims=True)
    var = jnp.mean(jnp.square(tf - mu), axis=-1, keepdims=True)
    return ((tf - mu) * lax.rsqrt(var + 1e-5) * g + b).astype(t.dtype)


def split_cols(t, widths):
    offs = np.cumsum(widths)[:-1].tolist()
    return jnp.split(t, offs, axis=-1)


def rope_tables(seq_len, dh):
    inv = 1.0 / (ROPE_THETA ** (jnp.arange(0, dh, 2, dtype=jnp.float32) / dh))
    ang = jnp.arange(seq_len, dtype=jnp.float32)[:, None] * inv[None, :]
    ang = jnp.concatenate([ang, ang], axis=-1)
    return jnp.cos(ang), jnp.sin(ang)


def rotary(t, cos, sin):
    t1, t2 = jnp.split(t, 2, axis=-1)
    rot = jnp.concatenate([-t2, t1], axis=-1)
    return (t * cos[:, None, :] + rot * sin[:, None, :]).astype(t.dtype)


def banded_attention(q, k, v, half_window, block, sink_logit=None):
    B, G, L, Hq, dh = q.shape
    Hkv = k.shape[3]
    rep = Hq // Hkv
    nb = -(-L // block)
    Lp = nb * block
    span = block + 2 * half_window
    qp = jnp.pad(q, ((0, 0), (0, 0), (0, Lp - L), (0, 0), (0, 0))).reshape(B, G, nb, block, Hkv, rep, dh)
    pad = ((0, 0), (0, 0), (half_window, Lp - L + half_window), (0, 0), (0, 0))
    kp = jnp.pad(k, pad)
    vp = jnp.pad(v, pad)
    kidx = (jnp.arange(nb) * block)[:, None] + jnp.arange(span)[None, :]
    kb = kp[:, :, kidx]
    vb = vp[:, :, kidx]
    kpos = (kidx - half_window)[:, None, :]
    qpos = ((jnp.arange(nb) * block)[:, None] + jnp.arange(block)[None, :])[:, :, None]
    valid = (kpos >= 0) & (kpos < L) & (jnp.abs(kpos - qpos) <= half_window)
    s = jnp.einsum('bgnqhrd,bgnkhd->bgnhrqk', qp, kb, preferred_element_type=jnp.float32) * (dh ** -0.5)
    s = jnp.where(valid[None, None, :, None, None, :, :], s, NEG_INF)
    m = jnp.max(s, axis=-1, keepdims=True)
    if sink_logit is None:
        p = jnp.exp(s - m)
        denom = jnp.sum(p, axis=-1, keepdims=True)
    else:
        sk = sink_logit.astype(jnp.float32).reshape(Hkv, rep)[None, None, None, :, :, None, None]
        m = jnp.maximum(m, sk)
        p = jnp.exp(s - m)
        denom = jnp.sum(p, axis=-1, keepdims=True) + jnp.exp(sk - m)
    out = jnp.einsum('bgnhrqk,bgnkhd->bgnqhrd', (p / denom).astype(v.dtype), vb)
    out = out.reshape(B, G, Lp, Hq, dh)[:, :, :L]
    lse = (m + jnp.log(denom))[..., 0]
    lse = lse.transpose(0, 1, 2, 5, 3, 4).reshape(B, G, Lp, Hq)[:, :, :L]
    return out, lse


def dilated_window_attention(q, k, v):
    B, S, H, dh = q.shape
    outs, lses = [], []
    for window, dil in DILATED_PATTERNS:
        def to_res(t):
            return t.reshape(B, S // dil, dil, H, dh).transpose(0, 2, 1, 3, 4)
        o, lse = banded_attention(to_res(q), to_res(k), to_res(v), window // (2 * dil), ATTN_BLOCK)
        outs.append(o.transpose(0, 2, 1, 3, 4).reshape(B, S, H, dh))
        lses.append(lse.transpose(0, 2, 1, 3).reshape(B, S, H))
    wts = jax.nn.softmax(jnp.stack(lses), axis=0)
    return jnp.einsum('pbsh,pbshd->bshd', wts.astype(q.dtype), jnp.stack(outs))


def even_mixer(h, w_in, sink, w_out, cos, sin):
    B, S, _ = h.shape
    p = h @ w_in
    qa, ka, va, qb, kb, vb = split_cols(p, [A_WIDTH, A_WIDTH, A_WIDTH, B_WIDTH, B_KV_WIDTH, B_KV_WIDTH])
    heads = lambda t, n: t.reshape(B, S, n, HEAD_DIM)
    qa = rotary(heads(qa, A_HEADS), cos, sin)
    ka = rotary(heads(ka, A_HEADS), cos, sin)
    oa = dilated_window_attention(qa, ka, heads(va, A_HEADS))
    qb = rotary(heads(qb, B_Q_HEADS), cos, sin)
    kb = rotary(heads(kb, B_KV_HEADS), cos, sin)
    ob, _ = banded_attention(qb[:, None], kb[:, None], heads(vb, B_KV_HEADS)[:, None], B_HALF_WINDOW, ATTN_BLOCK, sink)
    y = jnp.concatenate([oa.reshape(B, S, A_WIDTH), ob[:, 0].reshape(B, S, B_WIDTH)], axis=-1)
    return y @ w_out


def spatial_gating(cu, cv, ln_g, ln_b, w_s, b_s):
    B, S, _ = cu.shape
    u = jax.nn.gelu(cu)
    v = layernorm(jax.nn.gelu(cv), ln_g, ln_b).reshape(B, S // C_CHUNK, C_CHUNK, C_GROUPS, C_GROUP_CH)
    s = jnp.einsum('gij,bnjgc->bnigc', w_s, v) + b_s.T[None, None, :, :, None]
    return u * s.reshape(B, S, C_WIDTH)


def centred_token_shift(f, mu):
    prev = jnp.pad(f[:, :-1], ((0, 0), (1, 0), (0, 0)))
    nxt = jnp.pad(f[:, 1:], ((0, 0), (0, 1), (0, 0)))
    return f + mu[0] * (prev - f) + mu[1] * (nxt - f)


def wkv7_scan(r, w, k, v, a, b):
    T, N, H, K = r.shape

    def step(state, inp):
        r_t, w_t, k_t, v_t, a_t, b_t = inp
        sa = jnp.einsum('nhvk,nhk->nhv', state, a_t)
        state = state * w_t[:, :, None, :] + sa[..., :, None] * b_t[..., None, :] + v_t[..., :, None] * k_t[..., None, :]
        return state, jnp.einsum('nhvk,nhk->nhv', state, r_t)

    s0 = jnp.zeros((N, H, K, K), jnp.float32)
    _, y = lax.scan(step, s0, (r, w, k, v, a, b))
    return y


def rwkv7_bidirectional(f, w0, w2, a0, a2, g2, k_k, k_a, r_k, ln_g, ln_b):
    B, S, _ = f.shape
    H, K = D_HEADS, HEAD_DIM
    r, k, v, hw, ha, hg = split_cols(f, [D_WIDTH, D_WIDTH, D_WIDTH, 2 * D_DECAY_LORA, 2 * D_AAA_LORA, D_GATE_LORA])
    hw = hw.reshape(B, S, 2, D_DECAY_LORA)
    ha = ha.reshape(B, S, 2, D_AAA_LORA)
    w_log = -jax.nn.softplus(-(w0 + jnp.einsum('bsdr,drc->bsdc', jnp.tanh(hw), w2))) - 0.5
    decay = jnp.exp(-jnp.exp(w_log.astype(jnp.float32)))
    a_lr = jax.nn.sigmoid(a0 + jnp.einsum('bsdr,drc->bsdc', ha, a2))
    g = jax.nn.sigmoid(hg) @ g2
    kk = (k * k_k).astype(jnp.float32).reshape(B, S, H, K)
    kk = (kk / jnp.maximum(jnp.linalg.norm(kk, axis=-1, keepdims=True), 1e-12)).reshape(B, S, D_WIDTH)
    k_dir = k[:, :, None, :] * (1.0 + (a_lr - 1.0) * k_a)

    def seq(t):
        both = jnp.concatenate([t[:, :, 0], jnp.flip(t[:, :, 1], axis=1)], axis=0)
        return both.astype(jnp.float32).reshape(2 * B, S, H, K).transpose(1, 0, 2, 3)

    def shared(t):
        return jnp.stack([t, t], axis=2)

    y = wkv7_scan(seq(shared(r)), seq(decay), seq(k_dir), seq(shared(v)), seq(shared(-kk)), seq(kk[:, :, None, :] * a_lr))
    y = y.transpose(1, 0, 2, 3)
    y = y[:B] + jnp.flip(y[B:], axis=1)
    mu = jnp.mean(y, axis=-1, keepdims=True)
    var = jnp.mean(jnp.square(y - mu), axis=-1, keepdims=True)
    y = ((y - mu) * lax.rsqrt(var + GN_EPS)).reshape(B, S, D_WIDTH) * ln_g + ln_b
    rk = jnp.sum(r[:, :, None, :] * k_dir, axis=2).reshape(B, S, H, K)
    bonus = jnp.sum(rk * r_k, axis=-1, keepdims=True) * v.reshape(B, S, H, K)
    return ((y + bonus.reshape(B, S, D_WIDTH)) * g).astype(f.dtype)


def odd_mixer(h, w_in, c_ln_g, c_ln_b, c_w_s, c_b_s, d_shift, d_w0, d_w2, d_a0, d_a2, d_g2, d_k_k, d_k_a, d_r_k, d_ln_g, d_ln_b, w_out):
    p = h @ w_in
    cu, cv, fd = split_cols(p, [C_WIDTH, C_WIDTH, D_SHIFT_WIDTH])
    yc = spatial_gating(cu, cv, c_ln_g, c_ln_b, c_w_s, c_b_s)
    yd = rwkv7_bidirectional(centred_token_shift(fd, d_shift), d_w0, d_w2, d_a0, d_a2, d_g2, d_k_k, d_k_a, d_r_k, d_ln_g, d_ln_b)
    return jnp.concatenate([yc, yd], axis=-1) @ w_out


def memory_cross_attention(h, m, wq, wkv, wo):
    B, S, _ = h.shape
    M = m.shape[1]
    q = (h @ wq).reshape(B, S, MEM_HEADS, MEM_HEAD_DIM)
    kv = (m @ wkv).reshape(B, M, 2, MEM_HEADS, MEM_HEAD_DIM)
    s = jnp.einsum('bshd,bmhd->bhsm', q, kv[:, :, 0], preferred_element_type=jnp.float32) * (MEM_HEAD_DIM ** -0.5)
    p = jax.nn.softmax(s, axis=-1).astype(h.dtype)
    o = jnp.einsum('bhsm,bmhd->bshd', p, kv[:, :, 1]).reshape(B, S, MEM_HEADS * MEM_HEAD_DIM)
    return o @ wo


def expert_choice_ffn(h, w_router, w_gate, w_up, w_down):
    B, N, _ = h.shape
    cap = max(1, CAPACITY_FACTOR * N // N_EXPERTS)
    aff = jax.nn.softmax((h @ w_router).astype(jnp.float32), axis=-1)
    gate, idx = lax.top_k(jnp.swapaxes(aff, 1, 2), cap)
    bidx = jnp.arange(B)[:, None, None]
    xe = h[bidx, idx]
    hid = jax.nn.silu(jnp.einsum('becd,edf->becf', xe, w_gate)) * jnp.einsum('becd,edf->becf', xe, w_up)
    ye = jnp.einsum('becf,efd->becd', hid, w_down) * gate[..., None].astype(h.dtype)
    return jnp.zeros_like(h).at[bidx, idx].add(ye)


def setup_inputs(seed: int = 0) -> dict:
    key = jax.random.key(seed)
    ks = iter(jax.random.split(key, 64))
    f32 = jnp.float32
    ne, no, nl = (DEPTH + 1) // 2, DEPTH // 2, DEPTH

    def nrm(shape, scale):
        return jax.random.normal(next(ks), shape, f32) * scale

    def gain(shape):
        return 1.0 + nrm(shape, 0.05)

    def unif(shape, lo, hi):
        return jax.random.uniform(next(ks), shape, f32, lo, hi)

    return {
        "x": nrm((BATCH, SEQ, D_MODEL), 1.0),
        "mem": nrm((BATCH, MEM_LEN, D_MODEL), 1.0),
        "e_norm": gain((ne, D_MODEL)),
        "e_w_in": nrm((ne, D_MODEL, EVEN_IN), D_MODEL ** -0.5),
        "e_sink": nrm((ne, B_Q_HEADS), 0.5),
        "e_w_out": nrm((ne, MIX_WIDTH, D_MODEL), MIX_WIDTH ** -0.5),
        "o_norm": gain((no, D_MODEL)),
        "o_w_in": nrm((no, D_MODEL, ODD_IN), D_MODEL ** -0.5),
        "c_ln_g": gain((no, C_WIDTH)),
        "c_ln_b": nrm((no, C_WIDTH), 0.02),
        "c_w_s": nrm((no, C_GROUPS, C_CHUNK, C_CHUNK), C_CHUNK ** -0.5),
        "c_b_s": gain((no, C_GROUPS, C_CHUNK)),
        "d_shift": unif((no, 2, D_SHIFT_WIDTH), 0.0, 0.5),
        "d_w0": unif((no, 2, D_WIDTH), -6.0, -1.0),
        "d_w2": nrm((no, 2, D_DECAY_LORA, D_WIDTH), 0.1 * D_DECAY_LORA ** -0.5),
        "d_a0": nrm((no, 2, D_WIDTH), 0.1),
        "d_a2": nrm((no, 2, D_AAA_LORA, D_WIDTH), 0.1 * D_AAA_LORA ** -0.5),
        "d_g2": nrm((no, D_GATE_LORA, D_WIDTH), D_GATE_LORA ** -0.5),
        "d_k_k": 0.85 + nrm((no, D_WIDTH), 0.05),
        "d_k_a": gain((no, D_WIDTH)),
        "d_r_k": nrm((no, D_HEADS, HEAD_DIM), 0.1),
        "d_ln_g": gain((no, D_WIDTH)),
        "d_ln_b": nrm((no, D_WIDTH), 0.02),
        "o_w_out": nrm((no, C_WIDTH + D_WIDTH, D_MODEL), (C_WIDTH + D_WIDTH) ** -0.5),
        "x_norm": gain((nl, D_MODEL)),
        "m_norm": gain((nl, D_MODEL)),
        "x_wq": nrm((nl, D_MODEL, MEM_HEADS * MEM_HEAD_DIM), D_MODEL ** -0.5),
        "x_wkv": nrm((nl, D_MODEL, 2 * MEM_HEADS * MEM_HEAD_DIM), D_MODEL ** -0.5),
        "x_wo": nrm((nl, MEM_HEADS * MEM_HEAD_DIM, D_MODEL), D_MODEL ** -0.5),
        "f_norm": gain((nl, D_MODEL)),
        "f_router": nrm((nl, D_MODEL, N_EXPERTS), D_MODEL ** -0.5),
        "f_w_gate": nrm((nl, N_EXPERTS, D_MODEL, D_EXPERT), D_MODEL ** -0.5),
        "f_w_up": nrm((nl, N_EXPERTS, D_MODEL, D_EXPERT), D_MODEL ** -0.5),
        "f_w_down": nrm((nl, N_EXPERTS, D_EXPERT, D_MODEL), D_EXPERT ** -0.5),
        "final_norm": gain((D_MODEL,)),
    }


def reference(x, mem, e_norm, e_w_in, e_sink, e_w_out, o_norm, o_w_in, c_ln_g, c_ln_b, c_w_s, c_b_s, d_shift, d_w0, d_w2, d_a0, d_a2, d_g2, d_k_k, d_k_a, d_r_k, d_ln_g, d_ln_b, o_w_out, x_norm, m_norm, x_wq, x_wkv, x_wo, f_norm, f_router, f_w_gate, f_w_up, f_w_down, final_norm):
    cos, sin = rope_tables(x.shape[1], HEAD_DIM)
    for layer in range(DEPTH):
        i = layer // 2
        if layer % 2 == 0:
            x = x + even_mixer(rmsnorm(x, e_norm[i]), e_w_in[i], e_sink[i], e_w_out[i], cos, sin)
        else:
            x = x + odd_mixer(rmsnorm(x, o_norm[i]), o_w_in[i], c_ln_g[i], c_ln_b[i], c_w_s[i], c_b_s[i], d_shift[i], d_w0[i], d_w2[i], d_a0[i], d_a2[i], d_g2[i], d_k_k[i], d_k_a[i], d_r_k[i], d_ln_g[i], d_ln_b[i], o_w_out[i])
        x = x + memory_cross_attention(rmsnorm(x, x_norm[layer]), rmsnorm(mem, m_norm[layer]), x_wq[layer], x_wkv[layer], x_wo[layer])
        x = x + expert_choice_ffn(rmsnorm(x, f_norm[layer]), f_router[layer], f_w_gate[layer], f_w_up[layer], f_w_down[layer])
    return rmsnorm(x, final_norm)
```

```python
import ml_dtypes
import numpy as np
from contextlib import ExitStack
import concourse.bass as bass
import concourse.mybir as mybir
from concourse.bass_utils import run_bass_kernel_spmd

F32 = mybir.dt.float32
BF16 = mybir.dt.bfloat16
I32 = mybir.dt.int32
AF = mybir.ActivationFunctionType
ALU = mybir.AluOpType
AX = mybir.AxisListType

ENGS = ("pe", "act", "dve", "pool", "sp")
NPOOL = 6


class Op:
    __slots__ = ("eng", "fn", "idx", "deps", "dma", "dsem", "dval", "gidx")


class Prog:
    def __init__(self):
        self.nc = bass.Bass("TRN2", target_bir_lowering=False)
        self.stack = ExitStack()
        self.ops = {e: [] for e in ENGS}
        self.lastw = {}
        self.readers = {}
        self.n = 0
        self.out_dmas = []
        self.dma_count = {e: 0 for e in ENGS}
        self.same_engine_sync = True
        self._uid = 0

    def dram(self, name, shape, dt, kind="ExternalInput"):
        return self.nc.dram_tensor(name, list(shape), dt, kind=kind).ap()

    def sb(self, shape, dt, name=None):
        self._uid += 1
        name = name or f"sb{self._uid}"
        return self.stack.enter_context(self.nc.sbuf_tensor(name, list(shape), dt))

    def ps(self, shape, dt=F32, name=None):
        self._uid += 1
        name = name or f"ps{self._uid}"
        return self.stack.enter_context(self.nc.psum_tensor(name, list(shape), dt))

    def op(self, eng, fn, r=(), w=(), dma=False, out=False):
        o = Op()
        o.eng = eng
        o.fn = fn
        o.dma = dma
        o.idx = len(self.ops[eng])
        o.gidx = self.n
        self.n += 1
        deps = set()
        for k in r:
            lw = self.lastw.get(k)
            if lw is not None:
                deps.add(lw)
        for k in w:
            lw = self.lastw.get(k)
            if lw is not None:
                deps.add(lw)
            for rd in self.readers.get(k, ()):
                deps.add(rd)
        for k in r:
            self.readers.setdefault(k, []).append(o)
        for k in w:
            self.lastw[k] = o
            self.readers[k] = []
        deps.discard(o)
        o.deps = deps
        self.ops[eng].append(o)
        if out:
            self.out_dmas.append(o)
        return o

    def dma(self, eng, out, in_, r=(), w=(), is_out=False, **kw):
        return self.op(eng, lambda e: e.dma_start(out=out, in_=in_, **kw), r, w, dma=True, out=is_out)

    def mm(self, out, lhsT, rhs, start=True, stop=True, r=(), w=()):
        return self.op("pe", lambda e: e.matmul(out, lhsT, rhs, start=start, stop=stop), r, w)

    def tr(self, out, in_, ident, r=(), w=()):
        return self.op("pe", lambda e: e.transpose(out, in_, ident), r, w)

    def act(self, out, in_, func, r=(), w=(), eng="act", **kw):
        return self.op(eng, lambda e: e.activation(out=out, in_=in_, func=func, **kw), r, w)

    def tt(self, eng, out, in0, in1, op, r=(), w=()):
        return self.op(eng, lambda e: e.tensor_tensor(out=out, in0=in0, in1=in1, op=op), r, w)

    def ts(self, eng, out, in0, s1, s2, op0, op1=None, r=(), w=(), **kw):
        if op1 is None:
            return self.op(eng, lambda e: e.tensor_scalar(out=out, in0=in0, scalar1=s1, scalar2=None, op0=op0, **kw), r, w)
        return self.op(eng, lambda e: e.tensor_scalar(out=out, in0=in0, scalar1=s1, scalar2=s2, op0=op0, op1=op1, **kw), r, w)

    def stt(self, eng, out, in0, scalar, in1, op0, op1, r=(), w=(), **kw):
        return self.op(eng, lambda e: e.scalar_tensor_tensor(out=out, in0=in0, scalar=scalar, in1=in1, op0=op0, op1=op1, **kw), r, w)

    def copy(self, eng, out, in_, r=(), w=()):
        if eng == "act":
            return self.op(eng, lambda e: e.copy(out=out, in_=in_), r, w)
        return self.op(eng, lambda e: e.tensor_copy(out=out, in_=in_), r, w)

    def memset(self, eng, ap, val, w=()):
        return self.op(eng, lambda e: e.memset(ap, val), (), w)

    def build(self):
        nc = self.nc
        st = self.stack
        csem = {e: st.enter_context(nc.semaphore(f"c_{e}")) for e in ENGS}
        dsem = {e: [st.enter_context(nc.semaphore(f"d_{e}{i}")) for i in range(NPOOL)] for e in ("sp", "act", "pool")}
        duse = {e: [0] * NPOOL for e in dsem}
        for e in dsem:
            k = 0
            for o in self.ops[e]:
                if o.dma:
                    i = k % NPOOL
                    duse[e][i] += 1
                    o.dsem = (e, i)
                    o.dval = 16 * duse[e][i]
                    k += 1
        ccount = {}
        for e in ENGS:
            c = 0
            for o in self.ops[e]:
                if not o.dma:
                    c += 1
                    o.dval = c
                    o.dsem = None
        out_dmas = self.out_dmas
        prog = self

        def emit(ename, eng):
            seen = {}

            def wait(key, sem, val):
                if seen.get(key, 0) >= val:
                    return
                eng.wait_ge(sem, val)
                seen[key] = val

            for o in prog.ops[ename]:
                for d in sorted(o.deps, key=lambda x: x.gidx):
                    if d.dma:
                        wait(("d",) + d.dsem, dsem[d.dsem[0]][d.dsem[1]], d.dval)
                    else:
                        if d.eng == ename and not prog.same_engine_sync:
                            continue
                        if d.eng == ename and ename == "pe":
                            continue
                        wait(("c", d.eng), csem[d.eng], d.dval)
                if o.dma:
                    if o.dval > 16:
                        wait(("d",) + o.dsem, dsem[o.dsem[0]][o.dsem[1]], o.dval - 16)
                    ins = o.fn(eng)
                    ins.then_inc(dsem[o.dsem[0]][o.dsem[1]], 16)
                else:
                    ins = o.fn(eng)
                    ins.then_inc(csem[ename], 1)
            if ename == "sp":
                for d in out_dmas:
                    wait(("d",) + d.dsem, dsem[d.dsem[0]][d.dsem[1]], d.dval)

        with nc.Block() as block:
            @block.sync
            def _(e):
                emit("sp", e)

            @block.scalar
            def _(e):
                emit("act", e)

            @block.vector
            def _(e):
                emit("dve", e)

            @block.gpsimd
            def _(e):
                emit("pool", e)

            @block.tensor
            def _(e):
                emit("pe", e)
        self.stack.close()
        return nc


def run(nc, in_maps, trace=False):
    res = run_bass_kernel_spmd(nc, in_maps, core_ids=list(range(len(in_maps))), trace=trace)
    return res


D = 1024
S = 8192
NB = 2
TOK = 2048
NT = TOK // 128
NPBF = ml_dtypes.bfloat16
_cache = {}


def _ident_np():
    return np.eye(128, dtype=np.float32)


def rmsnorm_tile(P, xb, xkey, gb, h, hkey, scr, eps=1e-6, d=1024):
    sq, ss, rstd = scr["sq"], scr["ss"], scr["rstd"]
    P.act(sq[:, :d], xb, AF.Square, r=[xkey], w=["sq", "ss"], accum_out=ss[:])
    P.ts("dve", rstd[:], ss[:], 1.0 / d, eps, ALU.mult, ALU.add, r=["ss"], w=["rstd"])
    P.act(rstd[:], rstd[:], AF.Sqrt, r=["rstd"], w=["rstd"])
    P.op("dve", lambda e: e.reciprocal(out=rstd[:], in_=rstd[:]), ["rstd"], ["rstd"])
    P.stt("dve", h, xb, rstd[:], gb, ALU.mult, ALU.mult, r=[xkey, "rstd", "gb"], w=[hkey])


def norm_scratch(P):
    return {"sq": P.sb([128, 1024], F32), "ss": P.sb([128, 1], F32), "rstd": P.sb([128, 1], F32)}


def build_A1(var=0):
    P = Prog()
    x = P.dram("x", [TOK, D], F32)
    g = P.dram("g", [1, D], F32)
    wqk_d = P.dram("wqk", [D, 1664], F32)
    wsw_d = P.dram("wsw", [D, 1664], F32)
    wv_d = P.dram("wv", [D, 640], F32)
    cos_d = P.dram("cos", [128, TOK], F32)
    sin_d = P.dram("sin", [128, TOK], F32)
    ident_d = P.dram("ident", [128, 128], F32)
    qk_o = P.dram("qk_o", [13, 128, TOK], BF16, kind="ExternalOutput")
    v_o = P.dram("v_o", [TOK, 640], BF16, kind="ExternalOutput")

    gb = P.sb([128, D], F32)
    wqk = P.sb([128, 8, 1664], BF16)
    wsw = P.sb([128, 8, 1664], BF16)
    wv = P.sb([128, 8, 640], BF16)
    cos = P.sb([128, TOK], F32)
    sin = P.sb([128, TOK], F32)
    identf = P.sb([128, 128], F32)
    ident = P.sb([128, 128], BF16)
    scr = norm_scratch(P)
    xt = [P.sb([128, D], F32) for _ in range(2)]
    h = P.sb([128, D], BF16)
    hT4 = [P.sb([128, 8, 512], BF16) for _ in range(2)]
    ptr = [P.ps([128, 8, 128], BF16) for _ in range(2)]
    ps1 = [P.ps([128, 512], F32) for _ in range(2)]
    ps2 = [P.ps([128, 512], F32) for _ in range(2)]
    psv = P.ps([128, 512], F32)
    psv2 = P.ps([128, 128], F32)
    t1 = [P.sb([128, 512], F32) for _ in range(2)]
    t2 = [P.sb([128, 512], F32) for _ in range(2)]
    ro = [P.sb([128, 512], BF16) for _ in range(2)]
    vo = [P.sb([128, 640], BF16) for _ in range(2)]

    P.dma("sp", gb[:], g.partition_broadcast(128), w=["gb"])
    P.dma("sp", identf[:], ident_d, w=["identf"])
    P.copy("dve", ident[:], identf[:], r=["identf"], w=["ident"])
    for k in range(8):
        P.dma("pool", wqk[:, k, :], wqk_d[k * 128:(k + 1) * 128, :], w=[("wqk", k)])
        P.dma("pool", wsw[:, k, :], wsw_d[k * 128:(k + 1) * 128, :], w=[("wsw", k)])
        P.dma("pool", wv[:, k, :], wv_d[k * 128:(k + 1) * 128, :], w=[("wv", k)])
    P.dma("sp", cos[:], cos_d, w=["cos"])
    P.dma("sp", sin[:], sin_d, w=["sin"])
    nblk = 0
    for grp in range(NT // 4):
        hb = hT4[grp % 2]
        hk = ("hT4", grp % 2)
        for j in range(4):
            t = grp * 4 + j
            xb = xt[t % 2]
            P.dma("sp", xb[:], x[t * 128:(t + 1) * 128, :], w=[("x", t % 2)])
            rmsnorm_tile(P, xb[:], ("x", t % 2), gb[:], h[:], "h", scr)
            pt = ptr[t % 2]
            for k in range(8):
                P.tr(pt[:, k, :], h[:, k * 128:(k + 1) * 128], ident[:], r=["h", "ident"], w=[("ptr", t % 2)])
            P.copy("act", hb[:, :, j * 128:(j + 1) * 128], pt[:], r=[("ptr", t % 2)], w=[hk + (j,)])
        hkeys = [hk + (j,) for j in range(4)]
        for cb in range(13):
            i = nblk % 2
            nblk += 1
            for k in range(8):
                P.mm(ps1[i][:], wqk[:, k, cb * 128:(cb + 1) * 128], hb[:, k, :], start=(k == 0), stop=(k == 7),
                     r=hkeys + [("wqk", k)], w=[("ps1", i)])
            for k in range(8):
                P.mm(ps2[i][:], wsw[:, k, cb * 128:(cb + 1) * 128], hb[:, k, :], start=(k == 0), stop=(k == 7),
                     r=hkeys + [("wsw", k)], w=[("ps2", i)])
            cs = slice(grp * 512, (grp + 1) * 512)
            P.tt("dve", t1[i][:], ps1[i][:], cos[:, cs], ALU.mult, r=[("ps1", i), "cos"], w=[("t1", i)])
            P.tt("dve", t2[i][:], ps2[i][:], sin[:, cs], ALU.mult, r=[("ps2", i), "sin"], w=[("t2", i)])
            P.tt("dve" if var == 1 else "pool", ro[i][:], t1[i][:], t2[i][:], ALU.add, r=[("t1", i), ("t2", i)], w=[("ro", i)])
            P.dma("sp", qk_o[cb, :, cs], ro[i][:], r=[("ro", i)], w=[("qk_o", cb, grp)], is_out=True)
        for j in range(4):
            t = grp * 4 + j
            i = t % 2
            for k in range(8):
                P.mm(psv[:], hb[:, k, j * 128:(j + 1) * 128], wv[:, k, 0:512], start=(k == 0), stop=(k == 7),
                     r=hkeys + [("wv", k)], w=["psv"])
            for k in range(8):
                P.mm(psv2[:], hb[:, k, j * 128:(j + 1) * 128], wv[:, k, 512:640], start=(k == 0), stop=(k == 7),
                     r=hkeys + [("wv", k)], w=["psv2"])
            P.copy("act", vo[i][:, 0:512], psv[:], r=["psv"], w=[("vo", i, 0)])
            P.copy("act", vo[i][:, 512:640], psv2[:], r=["psv2"], w=[("vo", i, 1)])
            P.dma("sp", v_o[t * 128:(t + 1) * 128, :], vo[i][:], r=[("vo", i, 0), ("vo", i, 1)], w=[("v_o", t)], is_out=True)
    return P.build()


def rope_tables_np(pos):
    inv = (1.0 / (np.float32(10000.0) ** (np.arange(0, 64, 2, dtype=np.float32) / np.float32(64)))).astype(np.float32)
    ang = pos.astype(np.float32)[:, None] * inv[None, :]
    ang = np.concatenate([ang, ang], axis=-1)
    c = np.cos(ang).astype(np.float32)
    s = np.sin(ang).astype(np.float32)
    s[:, :32] *= -1.0
    cT = np.concatenate([c.T, c.T], axis=0)
    sT = np.concatenate([s.T, s.T], axis=0)
    return np.ascontiguousarray(cT), np.ascontiguousarray(sT)


QK_COLS = np.concatenate([np.arange(0, 512), np.arange(512, 1024), np.arange(1536, 2048), np.arange(2048, 2176)])
V_COLS = np.concatenate([np.arange(1024, 1536), np.arange(2176, 2304)])


def _swap_halves_cols(n):
    idx = np.arange(n)
    hd = idx // 64
    d = idx % 64
    return hd * 64 + (d + 32) % 64


def run_A1(x, e_norm, e_w_in):
    if "A1" not in _cache:
        _cache["A1"] = build_A1()
    nc = _cache["A1"]
    wqk = np.ascontiguousarray(e_w_in[:, QK_COLS])
    wsw = np.ascontiguousarray(wqk[:, _swap_halves_cols(1664)])
    wv = np.ascontiguousarray(e_w_in[:, V_COLS])
    maps = []
    for c in range(8):
        b, q = c // 4, c % 4
        cT, sT = rope_tables_np(np.arange(q * TOK, (q + 1) * TOK))
        maps.append({"x": np.ascontiguousarray(x[b, q * TOK:(q + 1) * TOK]), "g": e_norm.reshape(1, D), "wqk": wqk, "wsw": wsw,
                     "wv": wv, "cos": cT, "sin": sT, "ident": _ident_np()})
    res = run(nc, maps)
    qk = [res.results[c]["qk_o"] for c in range(8)]
    v = [res.results[c]["v_o"] for c in range(8)]
    return qk, v


NKA = 32
NKB = 18


def build_A2(var=0):
    P = Prog()
    ACTQ = "sp" if var & 1 else "act"
    POOLE = "dve" if var & 4 else "pool"
    qa_d = P.dram("qa", [4, 128, TOK], BF16)
    qb_d = P.dram("qb", [4, 128, TOK], BF16)
    ka_d = P.dram("ka", [4, 128, NKA * 128], BF16)
    kb_d = P.dram("kb", [2, 128, NKB * 128], BF16)
    va_d = P.dram("va", [NKA * 128, 8 * 65], BF16)
    vb_d = P.dram("vb", [NKB * 128, 2 * 65], BF16)
    kva_d = P.dram("kva", [128, NKA], F32)
    kvb_d = P.dram("kvb", [128, NKB], F32)
    ma_d = P.dram("ma", [128, 17, 128], BF16)
    mb_d = P.dram("mb", [128, 3, 128], BF16)
    x_d = P.dram("x", [TOK, D], F32)
    wo_d = P.dram("wo", [D, D], F32)
    sink_d = P.dram("sink", [1, 8], F32)
    ident_d = P.dram("ident", [128, 128], F32)
    x1_o = P.dram("x1_o", [TOK, D], F32, kind="ExternalOutput")

    qa = P.sb([128, 4, TOK], BF16)
    qb = P.sb([128, 4, TOK], BF16)
    ka = P.sb([128, 4, NKA * 128], BF16)
    kb = P.sb([128, 2, NKB * 128], BF16)
    va = P.sb([128, NKA, 8 * 65], BF16)
    vb = P.sb([128, NKB, 2 * 65], BF16)
    kva = P.sb([128, NKA], F32)
    kvb = P.sb([128, NKB], F32)
    ma = P.sb([128, 17, 128], BF16)
    mb = P.sb([128, 3, 128], BF16)
    wo = P.sb([128, 8, D], BF16)
    sink = P.sb([128, 8], F32)
    esink = P.sb([128, 8], F32)
    identf = P.sb([128, 128], F32)
    ident = P.sb([128, 128], BF16)
    xt = [P.sb([128, D], F32) for _ in range(2)]
    y = P.sb([128, D], BF16)
    yT = P.sb([128, 8, 128], BF16)
    pT = [P.sb([128, 4, 128], BF16) for _ in range(3)]
    pm = [P.sb([128, 4, 128], BF16) for _ in range(3)]
    den = P.sb([128, 4], F32)
    rden = P.sb([128, 4], F32)
    xo = [P.sb([128, D], F32) for _ in range(2)]
    ps_s = [P.ps([128, 4, 128], F32) for _ in range(2)]
    ps_o = [P.ps([128, 512], F32) for _ in range(4)]
    ptr = P.ps([128, 8, 128], BF16)
    ps_out1 = P.ps([128, 512], F32)
    ps_out = [ps_out1, ps_out1]

    P.dma("sp", identf[:], ident_d, w=["identf"])
    P.copy("dve", ident[:], identf[:], r=["identf"], w=["ident"])
    P.dma("sp", sink[:], sink_d.partition_broadcast(128), w=["sink"])
    P.act(esink[:], sink[:], AF.Exp, r=["sink"], w=["esink"])
    P.dma("sp", kva[:], kva_d, w=["kva"])
    P.dma("sp", kvb[:], kvb_d, w=["kvb"])
    P.dma("sp", ma[:], ma_d, w=["ma"])
    P.dma("sp", mb[:], mb_d, w=["mb"])
    for i in range(4):
        P.dma("sp", qa[:, i, :], qa_d[i], w=[("qa", i)])
        P.dma(ACTQ, qb[:, i, :], qb_d[i], w=[("qb", i)])
        if var & 2:
            for j in range(4):
                P.dma("sp", ka[:, i, j * 1024:(j + 1) * 1024], ka_d[i, :, j * 1024:(j + 1) * 1024], w=[("ka", i)])
        else:
            P.dma("sp", ka[:, i, :], ka_d[i], w=[("ka", i)])
    for i in range(2):
        P.dma(ACTQ, kb[:, i, :], kb_d[i], w=[("kb", i)])
    for kt in range(NKA):
        P.dma("sp" if kt % 2 else ACTQ, va[:, kt, :], va_d[kt * 128:(kt + 1) * 128, :], w=[("va", kt)])
    for kt in range(NKB):
        P.dma("sp" if kt % 2 else ACTQ, vb[:, kt, :], vb_d[kt * 128:(kt + 1) * 128, :], w=[("vb", kt)])
    for k in range(8):
        P.dma("pool", wo[:, k, :], wo_d[k * 128:(k + 1) * 128, :], w=[("wo", k)])

    cnt = 0
    gcnt = 0
    for qt in range(NT):
        qs = slice(qt * 128, (qt + 1) * 128)
        xb = xt[qt % 2]
        P.dma("sp", xb[:], x_d[qs, :], w=[("x", qt % 2)])
        for part in ("A", "B"):
            deltas = range(-8, 9) if part == "A" else range(-1, 2)
            for g in range(2):
                gcnt += 1
                nd = len(deltas)
                for di, dl in enumerate(deltas):
                    i2 = cnt % 2
                    i3 = cnt % 3
                    cnt += 1
                    if part == "A":
                        kt = qt + 8 + dl
                    else:
                        kt = qt + 1 + dl
                    ks = slice(kt * 128, (kt + 1) * 128)
                    for hh in range(4):
                        hd = 2 * hh + g
                        pr, pb = hh, g * 64
                        if part == "A":
                            P.mm(ps_s[i2][:, hh, :], ka[pb:pb + 64, pr, ks], qa[pb:pb + 64, pr, qs],
                                 r=[("ka", pr), ("qa", pr)], w=[("ps_s", i2)])
                        else:
                            P.mm(ps_s[i2][:, hh, :], kb[pb:pb + 64, hd // 4, ks], qb[pb:pb + 64, pr, qs],
                                 r=[("kb", hd // 4), ("qb", pr)], w=[("ps_s", i2)])
                    P.act(pT[i3][:], ps_s[i2][:], AF.Exp, r=[("ps_s", i2)], w=[("pT", i3)], scale=0.125)
                    if part == "A":
                        msk = ma[:, dl + 8, :].unsqueeze(1).broadcast_to([128, 4, 128])
                        kv = kva[:, kt:kt + 1]
                        mkeys = ["ma", "kva"]
                    else:
                        msk = mb[:, dl + 1, :].unsqueeze(1).broadcast_to([128, 4, 128])
                        kv = kvb[:, kt:kt + 1]
                        mkeys = ["mb", "kvb"]
                    P.tt("dve" if cnt % 2 else POOLE, pm[i3][:], pT[i3][:], msk, ALU.mult,
                         r=[("pT", i3)] + mkeys, w=[("pm", i3)])
                    for hh in range(4):
                        hd = 2 * hh + g
                        if part == "A":
                            rhs = va[:, kt, hd * 65:(hd + 1) * 65]
                            rk = ("va", kt)
                        else:
                            rhs = vb[:, kt, (hd // 4) * 65:(hd // 4 + 1) * 65]
                            rk = ("vb", kt)
                        P.mm(ps_o[hh][:, 0:65], pm[i3][:, hh, :], rhs, start=(di == 0), stop=(di == nd - 1),
                             r=[("pm", i3), rk], w=[("ps_o", hh)])
                base = (0 if part == "A" else 512)
                for hh in range(4):
                    hd = 2 * hh + g
                    pok = ("ps_o", hh)
                    if part == "A":
                        P.copy("dve", den[:, hh:hh + 1], ps_o[hh][:, 64:65], r=[pok], w=[("den", hh)])
                    else:
                        P.tt("dve", den[:, hh:hh + 1], ps_o[hh][:, 64:65], esink[:, hd:hd + 1], ALU.add,
                             r=[pok, "esink"], w=[("den", hh)])
                    P.op("dve", lambda e, hh=hh: e.reciprocal(out=rden[:, hh:hh + 1], in_=den[:, hh:hh + 1]), [("den", hh)], [("rden", hh)])
                    P.ts("dve", y[:, base + hd * 64:base + (hd + 1) * 64], ps_o[hh][:, 0:64], rden[:, hh:hh + 1], None, ALU.mult,
                         r=[pok, ("rden", hh)], w=[("y", part, g, hh)])
        ykeys = [("y", p_, g_, h_) for p_ in "AB" for g_ in range(2) for h_ in range(4)]
        for k in range(8):
            P.tr(ptr[:, k, :], y[:, k * 128:(k + 1) * 128], ident[:], r=ykeys + ["ident"], w=["ptr"])
        P.copy("act", yT[:], ptr[:], r=["ptr"], w=["yT"])
        for hf in range(2):
            for k in range(8):
                P.mm(ps_out[hf][:], yT[:, k, :], wo[:, k, hf * 512:(hf + 1) * 512], start=(k == 0), stop=(k == 7),
                     r=["yT", ("wo", k)], w=[("ps_out", 0)])
            P.tt("dve", xo[qt % 2][:, hf * 512:(hf + 1) * 512], ps_out[hf][:], xb[:, hf * 512:(hf + 1) * 512], ALU.add,
                 r=[("ps_out", 0), ("x", qt % 2)], w=[("xo", qt % 2, hf)])
        P.dma("sp", x1_o[qs, :], xo[qt % 2][:], r=[("xo", qt % 2, 0), ("xo", qt % 2, 1)], w=[("x1_o", qt)], is_out=True)
    return P.build()


def _masks_np():
    ql = np.arange(128)[None, :]
    kl = np.arange(128)[:, None]
    ma = np.zeros((128, 17, 128), np.float32)
    for dl in range(-8, 9):
        diff = ql - kl - 128 * dl
        ad = np.abs(diff)
        ma[:, dl + 8, :] = (ad <= 64).astype(np.float32) + ((ad <= 256) & (diff % 4 == 0)) + ((ad <= 1024) & (diff % 16 == 0))
    mb = np.zeros((128, 3, 128), np.float32)
    for dl in range(-1, 2):
        diff = ql - kl - 128 * dl
        mb[:, dl + 1, :] = (np.abs(diff) <= 128)
    return ma.astype(NPBF), mb.astype(NPBF)


def _halo(arr_list, b, q, axis, lo, hi, zero_shape_fn):
    full = np.concatenate([arr_list[b * 4 + i] for i in range(4)], axis=axis)
    padw = [(0, 0)] * full.ndim
    padw[axis] = (lo, hi)
    full = np.pad(full, padw)
    sl = [slice(None)] * full.ndim
    sl[axis] = slice(q * TOK, (q + 1) * TOK + lo + hi)
    return np.ascontiguousarray(full[tuple(sl)])


def run_A2(qk, v, x, e_sink, e_w_out, var=0):
    if ("A2", var) not in _cache:
        _cache[("A2", var)] = build_A2(var)
    nc = _cache[("A2", var)]
    ma, mb = _masks_np()
    ones = np.ones((TOK, 1), NPBF)
    vaug_a, vaug_b, ka_l, kb_l = [], [], [], []
    for c in range(8):
        vv = np.asarray(v[c])
        va = np.concatenate([np.concatenate([vv[:, h * 64:(h + 1) * 64], ones], 1) for h in range(8)], 1)
        vb = np.concatenate([np.concatenate([vv[:, 512 + h * 64:512 + (h + 1) * 64], ones], 1) for h in range(2)], 1)
        vaug_a.append(va)
        vaug_b.append(vb)
        qq = np.asarray(qk[c])
        ka_l.append(qq[4:8])
        kbt = qq[12]
        kb_l.append(np.stack([np.concatenate([kbt[0:64], kbt[0:64]], 0), np.concatenate([kbt[64:128], kbt[64:128]], 0)], 0))
    maps = []
    for c in range(8):
        b, q = c // 4, c % 4
        qq = np.asarray(qk[c])
        pos_a = (q * TOK - 1024) + np.arange(NKA) * 128
        pos_b = (q * TOK - 128) + np.arange(NKB) * 128
        kva = np.broadcast_to(((pos_a >= 0) & (pos_a < S)).astype(np.float32)[None, :], (128, NKA))
        kvb = np.broadcast_to(((pos_b >= 0) & (pos_b < S)).astype(np.float32)[None, :], (128, NKB))
        maps.append({
            "qa": np.ascontiguousarray(qq[0:4]), "qb": np.ascontiguousarray(qq[8:12]),
            "ka": _halo(ka_l, b, q, 2, 1024, 1024, None), "kb": _halo(kb_l, b, q, 2, 128, 128, None),
            "va": _halo(vaug_a, b, q, 0, 1024, 1024, None), "vb": _halo(vaug_b, b, q, 0, 128, 128, None),
            "kva": np.ascontiguousarray(kva), "kvb": np.ascontiguousarray(kvb), "ma": ma, "mb": mb,
            "x": np.ascontiguousarray(x[b, q * TOK:(q + 1) * TOK]), "wo": e_w_out, "sink": e_sink.reshape(1, 8),
            "ident": _ident_np()})
    res = run(nc, maps)
    return [res.results[c]["x1_o"] for c in range(8)]


def build_A3():
    P = Prog()
    x_d = P.dram("x", [TOK, D], F32)
    mem_d = P.dram("mem", [256, D], F32)
    xn_d = P.dram("xn", [1, D], F32)
    mn_d = P.dram("mn", [1, D], F32)
    fn_d = P.dram("fn", [1, D], F32)
    wq_d = P.dram("wq", [D, D], F32)
    wkv_d = P.dram("wkv", [D, 2 * D], F32)
    wo_d = P.dram("wo", [D, D], F32)
    wr_d = P.dram("wr", [D, 16], F32)
    ident_d = P.dram("ident", [128, 128], F32)
    x2_o = P.dram("x2_o", [TOK, D], F32, kind="ExternalOutput")
    hf_o = P.dram("hf_o", [TOK, D], BF16, kind="ExternalOutput")
    aff_o = P.dram("aff_o", [TOK, 16], F32, kind="ExternalOutput")

    gx = P.sb([128, D], F32)
    gm = P.sb([128, D], F32)
    gf = P.sb([128, D], F32)
    wq = P.sb([128, 8, D], BF16)
    wkv = P.sb([128, 8, 2 * D], BF16)
    wo = P.sb([128, 8, D], BF16)
    wr = P.sb([128, 8, 16], F32)
    identf = P.sb([128, 128], F32)
    ident = P.sb([128, 128], BF16)
    scr = norm_scratch(P)
    mt_ = P.sb([128, D], F32)
    h = P.sb([128, D], BF16)
    memT = P.sb([128, 8, 256], BF16)
    KT = P.sb([128, 8, 256], BF16)
    Vaug = P.sb([128, 2, 4 * 257], BF16)
    xt4 = P.sb([128, 4, D], F32)
    hT4 = P.sb([128, 8, 512], BF16)
    QT = P.sb([128, 8, 512], BF16)
    pT = [P.sb([128, 4, 128], BF16) for _ in range(2)]
    o = P.sb([128, D], BF16)
    oT = P.sb([128, 8, 128], BF16)
    x2 = [P.sb([128, D], F32) for _ in range(2)]
    hf = P.sb([128, D], F32)
    hfb = [P.sb([128, D], BF16) for _ in range(2)]
    hfT = P.sb([128, 8, 128], F32)
    den = P.sb([128, 4], F32)
    rden = P.sb([128, 4], F32)
    lg = P.sb([128, 16], F32)
    mx = P.sb([128, 1], F32)
    ex = P.sb([128, 16], F32)
    sm = P.sb([128, 1], F32)
    aff = [P.sb([128, 16], F32) for _ in range(2)]
    ps_s = [P.ps([128, 4, 128], F32) for _ in range(2)]
    ps_o = [P.ps([128, 512], F32) for _ in range(4)]
    ptr = P.ps([128, 8, 128], BF16)
    psA = P.ps([128, 512], F32)

    P.dma("sp", identf[:], ident_d, w=["identf"])
    P.copy("dve", ident[:], identf[:], r=["identf"], w=["ident"])
    P.dma("sp", gx[:], xn_d.partition_broadcast(128), w=["gx"])
    P.dma("sp", gm[:], mn_d.partition_broadcast(128), w=["gm"])
    P.dma("sp", gf[:], fn_d.partition_broadcast(128), w=["gf"])
    P.dma("sp", wr[:], wr_d.rearrange("(k p) n -> p k n", p=128), w=["wr"])
    for k in range(8):
        P.dma("pool", wkv[:, k, 0:1024], wkv_d[k * 128:(k + 1) * 128, 0:1024], w=[("wkv", k)])
        P.dma("pool", wkv[:, k, 1024:2048], wkv_d[k * 128:(k + 1) * 128, 1024:2048], w=[("wkv", k)])
    for k in range(8):
        P.dma("pool", wq[:, k, :], wq_d[k * 128:(k + 1) * 128, :], w=[("wq", k)])
    for k in range(8):
        P.dma("pool", wo[:, k, :], wo_d[k * 128:(k + 1) * 128, :], w=[("wo", k)])
    wkvk = [("wkv", k) for k in range(8)]
    P.memset("dve", Vaug[:], 1.0, w=["Vaug"])
    for m in range(2):
        P.dma("sp", mt_[:], mem_d[m * 128:(m + 1) * 128, :], w=["mt"])
        rmsnorm_tile(P, mt_[:], "mt", gm[:], h[:], "h", scr)
        for k in range(8):
            P.tr(ptr[:, k, :], h[:, k * 128:(k + 1) * 128], ident[:], r=["h", "ident"], w=["ptr"])
        P.copy("act", memT[:, :, m * 128:(m + 1) * 128], ptr[:], r=["ptr"], w=[("memT", m)])
    mk = [("memT", 0), ("memT", 1)]
    for c in range(8):
        for k in range(8):
            P.mm(psA[:, 0:256], wkv[:, k, c * 128:(c + 1) * 128], memT[:, k, :], start=(k == 0), stop=(k == 7),
                 r=mk + wkvk, w=["psA"])
        P.copy("act", KT[:, c, :], psA[:, 0:256], r=["psA"], w=["KT"])
    for m in range(2):
        for hf_ in range(2):
            for k in range(8):
                P.mm(psA[:], memT[:, k, m * 128:(m + 1) * 128], wkv[:, k, 1024 + hf_ * 512:1024 + (hf_ + 1) * 512],
                     start=(k == 0), stop=(k == 7), r=mk + wkvk, w=["psA"])
            for hh in range(2):
                hd = hf_ * 2 + hh
                P.copy("act", Vaug[:, m, hd * 257:hd * 257 + 256], psA[:, hh * 256:(hh + 1) * 256], r=["psA"], w=["Vaug"])
    wqk = [("wq", k) for k in range(8)]
    wok = [("wo", k) for k in range(8)]
    for grp in range(NT // 4):
        for j in range(4):
            t = grp * 4 + j
            P.dma("sp", xt4[:, j, :], x_d[t * 128:(t + 1) * 128, :], w=[("x", j)])
            rmsnorm_tile(P, xt4[:, j, :], ("x", j), gx[:], h[:], "h", scr)
            for k in range(8):
                P.tr(ptr[:, k, :], h[:, k * 128:(k + 1) * 128], ident[:], r=["h", "ident"], w=["ptr"])
            P.copy("act", hT4[:, :, j * 128:(j + 1) * 128], ptr[:], r=["ptr"], w=[("hT4", j)])
        hk = [("hT4", j) for j in range(4)]
        for c in range(8):
            for k in range(8):
                P.mm(psA[:], wq[:, k, c * 128:(c + 1) * 128], hT4[:, k, :], start=(k == 0), stop=(k == 7), r=hk + wqk, w=["psA"])
            P.copy("dve" if c % 2 else "act", QT[:, c, :], psA[:], r=["psA"], w=[("QT", c)])
        qk_ = [("QT", c) for c in range(8)]
        for j in range(4):
            t = grp * 4 + j
            js = slice(j * 128, (j + 1) * 128)
            for m in range(2):
                for hd in range(4):
                    for half in range(2):
                        P.mm(ps_s[m][:, hd, :], KT[:, 2 * hd + half, m * 128:(m + 1) * 128], QT[:, 2 * hd + half, js],
                             start=(half == 0), stop=(half == 1), r=["KT"] + qk_, w=[("ps_s", m)])
                P.act(pT[m][:], ps_s[m][:], AF.Exp, r=[("ps_s", m)], w=[("pT", m)], scale=1.0 / 16)
            for hd in range(4):
                for m in range(2):
                    P.mm(ps_o[hd][:, 0:257], pT[m][:, hd, :], Vaug[:, m, hd * 257:(hd + 1) * 257], start=(m == 0), stop=(m == 1),
                         r=[("pT", m), "Vaug"], w=[("ps_o", hd)])
                P.op("dve", lambda e, hd=hd: e.reciprocal(out=rden[:, hd:hd + 1], in_=ps_o[hd][:, 256:257]), [("ps_o", hd)], [("rden", hd)])
                P.ts("dve", o[:, hd * 256:(hd + 1) * 256], ps_o[hd][:, 0:256], rden[:, hd:hd + 1], None, ALU.mult,
                     r=[("ps_o", hd), ("rden", hd)], w=[("o", hd)])
            ok_ = [("o", hd) for hd in range(4)]
            for k in range(8):
                P.tr(ptr[:, k, :], o[:, k * 128:(k + 1) * 128], ident[:], r=ok_ + ["ident"], w=["ptr"])
            P.copy("act", oT[:], ptr[:], r=["ptr"], w=["oT"])
            xb = x2[t % 2]
            for hf_ in range(2):
                for k in range(8):
                    P.mm(psA[:], oT[:, k, :], wo[:, k, hf_ * 512:(hf_ + 1) * 512], start=(k == 0), stop=(k == 7), r=["oT"] + wok, w=["psA"])
                P.tt("dve", xb[:, hf_ * 512:(hf_ + 1) * 512], psA[:], xt4[:, j, hf_ * 512:(hf_ + 1) * 512], ALU.add,
                     r=["psA", ("x", j)], w=[("x2", t % 2, hf_)])
            x2k = [("x2", t % 2, 0), ("x2", t % 2, 1)]
            P.dma("sp", x2_o[t * 128:(t + 1) * 128, :], xb[:], r=x2k, w=[("x2_o", t)], is_out=True)
            sq, ss, rstd = scr["sq"], scr["ss"], scr["rstd"]
            P.act(sq[:], xb[:], AF.Square, r=x2k, w=["sq", "ss"], accum_out=ss[:])
            P.ts("dve", rstd[:], ss[:], 1.0 / D, 1e-6, ALU.mult, ALU.add, r=["ss"], w=["rstd"])
            P.act(rstd[:], rstd[:], AF.Sqrt, r=["rstd"], w=["rstd"])
            P.op("dve", lambda e: e.reciprocal(out=rstd[:], in_=rstd[:]), ["rstd"], ["rstd"])
            P.stt("dve", hf[:], xb[:], rstd[:], gf[:], ALU.mult, ALU.mult, r=x2k + ["rstd", "gf"], w=["hf"])
            P.copy("pool", hfb[t % 2][:], hf[:], r=["hf"], w=[("hfb", t % 2)])
            P.dma("sp", hf_o[t * 128:(t + 1) * 128, :], hfb[t % 2][:], r=[("hfb", t % 2)], w=[("hf_o", t)], is_out=True)
            psAv = psA[:].rearrange("p (a b) -> p a b", b=128)
            for hf_ in range(2):
                for k in range(4):
                    kk = hf_ * 4 + k
                    P.tr(psAv[:, k, :], hf[:, kk * 128:(kk + 1) * 128], identf[:], r=["hf", "identf"], w=["psA"])
                P.copy("act", hfT[:, hf_ * 4:(hf_ + 1) * 4, :], psAv, r=["psA"], w=[("hfT", hf_)])
            for k in range(8):
                P.mm(psA[:, 0:16], hfT[:, k, :], wr[:, k, :], start=(k == 0), stop=(k == 7), r=[("hfT", 0), ("hfT", 1), "wr"], w=["psA"])
            P.copy("dve", lg[:], psA[:, 0:16], r=["psA"], w=["lg"])
            P.op("dve", lambda e: e.reduce_max(out=mx[:], in_=lg[:], axis=AX.X), ["lg"], ["mx"])
            P.ts("dve", mx[:], mx[:], -1.0, None, ALU.mult, r=["mx"], w=["mx"])
            P.act(ex[:], lg[:], AF.Exp, r=["lg", "mx"], w=["ex", "sm"], bias=mx[:], accum_out=sm[:])
            P.op("dve", lambda e: e.reciprocal(out=sm[:], in_=sm[:]), ["sm"], ["sm"])
            P.ts("dve", aff[t % 2][:], ex[:], sm[:], None, ALU.mult, r=["ex", "sm"], w=[("aff", t % 2)])
            P.dma("sp", aff_o[t * 128:(t + 1) * 128, :], aff[t % 2][:], r=[("aff", t % 2)], w=[("aff_o", t)], is_out=True)
    return P.build()


def run_A3(x1, mem, x_norm, m_norm, wq, wkv, wo, f_norm, f_router):
    if "A3" not in _cache:
        _cache["A3"] = build_A3()
    nc = _cache["A3"]
    maps = []
    for c in range(8):
        b = c // 4
        maps.append({"x": np.ascontiguousarray(x1[c]), "mem": np.ascontiguousarray(mem[b]), "xn": x_norm.reshape(1, D), "mn": m_norm.reshape(1, D),
                     "fn": f_norm.reshape(1, D), "wq": wq, "wkv": wkv, "wo": wo, "wr": f_router, "ident": _ident_np()})
    res = run(nc, maps)
    return ([res.results[c]["x2_o"] for c in range(8)], [res.results[c]["hf_o"] for c in range(8)],
            [res.results[c]["aff_o"] for c in range(8)])


CAP = 1024
FE = 2816
NFC = FE // 128


def build_M(nbis=32):
    P = Prog()
    aff_d = P.dram("aff", [128, 4, 64], F32)
    hf_d = P.dram("hf", [NB * S, D], BF16)
    wg_d = P.dram("wg", [2, D, FE], F32)
    wu_d = P.dram("wu", [2, D, FE], F32)
    wd_d = P.dram("wd", [2, FE, D], F32)
    ltri_d = P.dram("ltri", [128, 128], F32)
    iota_d = P.dram("iota", [128, 1024], F32)
    iotap_d = P.dram("iotap", [128, 8], F32)
    ident_d = P.dram("ident", [128, 128], F32)
    out_d = P.dram("out", [NB, S, D], F32, kind="ExternalOutput")

    affc = P.sb([128, 4, 64], F32)
    ltri = P.sb([128, 128], F32)
    iota = P.sb([128, 1024], F32)
    iotap = P.sb([128, 8], F32)
    identf = P.sb([128, 128], F32)
    onesf = P.sb([128, 128], F32)
    lo = P.sb([128, 4], F32)
    hi = P.sb([128, 4], F32)
    mid = P.sb([128, 4], F32)
    cnt = P.sb([128, 4], F32)
    sel = P.sb([128, 4], F32)
    d1 = P.sb([128, 4], F32)
    d2 = P.sb([128, 4], F32)
    cmp_ = P.sb([128, 4, 64], F32)
    msk = P.sb([128, 4, 64], F32)
    posf = P.sb([128, 4, 64], F32)
    tmpm = P.sb([128, 4, 64], F32)
    cs = P.sb([64, 1], F32)
    csb = P.sb([64, 128], F32)
    hft = [P.sb([128, D], BF16) for _ in range(3)]
    selt = [P.sb([128, 512], BF16) for _ in range(3)]
    xeT = P.sb([128, 8, CAP], BF16)
    wgs = [P.sb([128, 8, 256], BF16) for _ in range(2)]
    wus = [P.sb([128, 8, 256], BF16) for _ in range(2)]
    sg = [P.sb([128, 512], F32) for _ in range(2)]
    hidT = P.sb([128, NFC, CAP], BF16)
    wdS = P.sb([128, NFC, D], BF16)
    ye = [P.sb([128, 8, D], BF16) for _ in range(2)]
    diag = [P.sb([128, 128], F32) for _ in range(2)]
    selT = [P.sb([128, 8, 128], BF16) for _ in range(2)]
    otile = [P.sb([128, D], F32) for _ in range(2)]
    B = [P.ps([128, 512], F32) for _ in range(8)]
    bk = lambda i: ("B", i)

    P.dma("sp", affc[:], aff_d, w=["affc"])
    P.dma("sp", ltri[:], ltri_d, w=["ltri"])
    P.dma("sp", iota[:], iota_d, w=["iota"])
    P.dma("sp", iotap[:], iotap_d, w=["iotap"])
    P.dma("sp", identf[:], ident_d, w=["identf"])
    P.memset("dve", onesf[:], 1.0, w=["onesf"])
    P.memset("dve", lo[:], 0.0, w=["lo"])
    P.memset("dve", hi[:], 1.0, w=["hi"])
    for it in range(nbis):
        P.tt("dve", mid[:], lo[:], hi[:], ALU.add, r=["lo", "hi"], w=["mid"])
        P.ts("dve", mid[:], mid[:], 0.5, None, ALU.mult, r=["mid"], w=["mid"])
        P.tt("dve", cmp_[:], affc[:], mid[:].unsqueeze(2).broadcast_to([128, 4, 64]), ALU.is_ge, r=["affc", "mid"], w=["cmp"])
        P.op("dve", lambda e: e.tensor_reduce(out=cnt[:], in_=cmp_[:], axis=AX.X, op=ALU.add), ["cmp"], ["cnt"])
        P.mm(B[0][:, 0:4], onesf[:], cnt[:], r=["onesf", "cnt"], w=[bk(0)])
        P.ts("dve", sel[:], B[0][:, 0:4], float(CAP), None, ALU.is_ge, r=[bk(0)], w=["sel"])
        P.tt("dve", d1[:], mid[:], lo[:], ALU.subtract, r=["mid", "lo"], w=["d1"])
        P.tt("dve", d1[:], d1[:], sel[:], ALU.mult, r=["d1", "sel"], w=["d1"])
        P.tt("dve", d2[:], hi[:], mid[:], ALU.subtract, r=["hi", "mid"], w=["d2"])
        P.tt("dve", d2[:], d2[:], sel[:], ALU.mult, r=["d2", "sel"], w=["d2"])
        P.tt("dve", lo[:], lo[:], d1[:], ALU.add, r=["lo", "d1"], w=["lo"])
        P.tt("dve", hi[:], mid[:], d2[:], ALU.add, r=["mid", "d2"], w=["hi"])
    P.tt("dve", msk[:], affc[:], lo[:].unsqueeze(2).broadcast_to([128, 4, 64]), ALU.is_ge, r=["affc", "lo"], w=["msk"])
    for g in range(4):
        P.mm(B[1][0:64, 0:1], msk[:, g, :], onesf[:, 0:1], r=["msk", "onesf"], w=[bk(1)])
        P.copy("dve", cs[:], B[1][0:64, 0:1], r=[bk(1)], w=["cs"])
        P.ts("dve", csb[:], onesf[0:64, :], cs[:], None, ALU.mult, r=["onesf", "cs"], w=["csb"])
        P.mm(B[2][:, 0:64], csb[:], ltri[0:64, 0:64], start=True, stop=False, r=["csb", "ltri"], w=[bk(2)])
        P.mm(B[2][:, 0:64], ltri[:], msk[:, g, :], start=False, stop=True, r=["ltri", "msk"], w=[bk(2)])
        P.ts("dve", tmpm[:, g, :], msk[:, g, :], -4096.0, 4096.0, ALU.mult, ALU.add, r=["msk"], w=[("tmpm", g)])
        P.tt("dve", posf[:, g, :], B[2][:, 0:64], tmpm[:, g, :], ALU.add, r=[bk(2), ("tmpm", g)], w=[("posf", g)])

    ncnt = {"hf": 0, "w": 0, "sg": 0, "ev": 0, "sc": 0}
    for b in range(NB):
        for el in range(2):
            g = b * 2 + el
            for sh in range(2):
                for j in range(64):
                    i3 = ncnt["hf"] % 3
                    ncnt["hf"] += 1
                    P.dma("sp" if j % 2 else "act", hft[i3][:], hf_d[b * S + j * 128: b * S + (j + 1) * 128, :], w=[("hft", i3)])
                    P.ts("dve", selt[i3][:], iota[:, sh * 512:(sh + 1) * 512], posf[:, g, j:j + 1], None, ALU.is_equal,
                         r=["iota", ("posf", g)], w=[("selt", i3)])
                    for k in range(8):
                        P.mm(B[k][:], hft[i3][:, k * 128:(k + 1) * 128], selt[i3][:], start=(j == 0), stop=(j == 63),
                             r=[("hft", i3), ("selt", i3)], w=[bk(k)])
                for k in range(8):
                    P.copy("act" if k % 2 else "dve", xeT[:, k, sh * 512:(sh + 1) * 512], B[k][:], r=[bk(k)], w=[("xeT", k, sh)])
            xk = [("xeT", k, sh) for k in range(8) for sh in range(2)]
            for fc in range(NFC):
                P.dma("pool", wdS[:, fc, :], wd_d[el, fc * 128:(fc + 1) * 128, :], w=[("wdS", fc)])
            for fb in range(NFC // 2):
                wi = ncnt["w"] % 2
                ncnt["w"] += 1
                for k in range(8):
                    P.dma("pool", wgs[wi][:, k, :], wg_d[el, k * 128:(k + 1) * 128, fb * 256:(fb + 1) * 256], w=[("wgs", wi)])
                    P.dma("pool", wus[wi][:, k, :], wu_d[el, k * 128:(k + 1) * 128, fb * 256:(fb + 1) * 256], w=[("wus", wi)])
                for fcl in range(2):
                    fc = fb * 2 + fcl
                    for sh in range(2):
                        bg = (fc % 2) * 4 + sh
                        bu = (fc % 2) * 4 + 2 + sh
                        for k in range(8):
                            P.mm(B[bg][:], wgs[wi][:, k, fcl * 128:(fcl + 1) * 128], xeT[:, k, sh * 512:(sh + 1) * 512],
                                 start=(k == 0), stop=(k == 7), r=xk + [("wgs", wi)], w=[bk(bg)])
                        for k in range(8):
                            P.mm(B[bu][:], wus[wi][:, k, fcl * 128:(fcl + 1) * 128], xeT[:, k, sh * 512:(sh + 1) * 512],
                                 start=(k == 0), stop=(k == 7), r=xk + [("wus", wi)], w=[bk(bu)])
                        si = ncnt["sg"] % 2
                        ncnt["sg"] += 1
                        P.act(sg[si][:], B[bg][:], AF.Silu, r=[bk(bg)], w=[("sg", si)])
                        P.tt("dve", hidT[:, fc, sh * 512:(sh + 1) * 512], sg[si][:], B[bu][:], ALU.mult,
                             r=[("sg", si), bk(bu)], w=[("hidT", fc, sh)])
            for st in range(8):
                for dh in range(2):
                    bi = ncnt["ev"] % 8
                    ncnt["ev"] += 1
                    for fc in range(NFC):
                        P.mm(B[bi][:], hidT[:, fc, st * 128:(st + 1) * 128], wdS[:, fc, dh * 512:(dh + 1) * 512],
                             start=(fc == 0), stop=(fc == NFC - 1), r=[("hidT", fc, st // 4), ("wdS", fc)], w=[bk(bi)])
                    P.copy("act" if bi % 2 else "dve", ye[el][:, st, dh * 512:(dh + 1) * 512], B[bi][:], r=[bk(bi)], w=[("ye", el, st, dh)])
        for j in range(64):
            ot = otile[j % 2]
            for el in range(2):
                g = b * 2 + el
                si = ncnt["sc"] % 2
                ncnt["sc"] += 1
                P.ts("dve", diag[si][:], identf[:], posf[:, g, j:j + 1], None, ALU.mult, r=["identf", ("posf", g)], w=[("diag", si)])
                pb = 4 + si
                P.mm(B[pb][:, 0:128], onesf[:], diag[si][:], r=["onesf", ("diag", si)], w=[bk(pb)])
                P.tt("dve", selT[si][:], B[pb][:, 0:128].unsqueeze(1).broadcast_to([128, 8, 128]),
                     iotap[:].unsqueeze(2).broadcast_to([128, 8, 128]), ALU.is_equal, r=[bk(pb), "iotap"], w=[("selT", si)])
                for dh in range(2):
                    bi = el * 2 + dh
                    for st in range(8):
                        P.mm(B[bi][:], selT[si][:, st, :], ye[el][:, st, dh * 512:(dh + 1) * 512], start=(st == 0), stop=(st == 7),
                             r=[("selT", si), ("ye", el, st, dh)], w=[bk(bi)])
            for dh in range(2):
                ds_ = slice(dh * 512, (dh + 1) * 512)
                P.ts("dve", ot[:, ds_], B[dh][:], affc[:, b * 2, j:j + 1], None, ALU.mult, r=[bk(dh), "affc"], w=[("ot", j % 2, dh)])
                P.stt("dve", ot[:, ds_], B[2 + dh][:], affc[:, b * 2 + 1, j:j + 1], ot[:, ds_], ALU.mult, ALU.add,
                      r=[bk(2 + dh), "affc", ("ot", j % 2, dh)], w=[("ot", j % 2, dh)])
            P.dma("sp", out_d[b, j * 128:(j + 1) * 128, :], ot[:], r=[("ot", j % 2, 0), ("ot", j % 2, 1)], w=[("out", b, j)], is_out=True)
    return P.build()


def run_M(hf, aff, wg, wu, wd):
    if "M" not in _cache:
        _cache["M"] = build_M()
    nc = _cache["M"]
    hf_all = np.ascontiguousarray(np.concatenate([np.asarray(h) for h in hf], 0))
    aff_all = np.concatenate([np.asarray(a) for a in aff], 0).reshape(NB, S, 16)
    ltri = np.triu(np.ones((128, 128), np.float32), 1)
    iota = np.ascontiguousarray(np.broadcast_to(np.arange(1024, dtype=np.float32)[None, :], (128, 1024)))
    iotap = (np.arange(128, dtype=np.float32)[:, None] + 128.0 * np.arange(8, dtype=np.float32)[None, :]).astype(np.float32)
    maps = []
    for c in range(8):
        a = np.zeros((128, 4, 64), np.float32)
        for b in range(NB):
            for el in range(2):
                a[:, b * 2 + el, :] = aff_all[b, :, 2 * c + el].reshape(64, 128).T
        maps.append({"aff": a, "hf": hf_all, "wg": np.ascontiguousarray(wg[2 * c:2 * c + 2]), "wu": np.ascontiguousarray(wu[2 * c:2 * c + 2]),
                     "wd": np.ascontiguousarray(wd[2 * c:2 * c + 2]), "ltri": ltri, "iota": iota, "iotap": iotap, "ident": _ident_np()})
    res = run(nc, maps)
    return [res.results[c]["out"] for c in range(8)]


CH = 64
NCH = S // CH
NCB = 4


def build_S(nchunks=NCH):
    P = Prog()
    tm_d = P.dram("tm", [4, 64, 4, S], F32)
    fm_d = P.dram("fm", [5, 64, 4, S], F32)
    tri_d = P.dram("tri", [64, 64], F32)
    m2_d = P.dram("m2", [64, 128], F32)
    mT_d = P.dram("mT", [64, 64], F32)
    id_d = P.dram("id64", [64, 64], F32)
    y_d = P.dram("y", [64, 4, S], F32, kind="ExternalOutput")

    tri = P.sb([64, 64], F32)
    m2 = P.sb([64, 128], F32)
    mT = P.sb([64, 64], F32)
    id64 = P.sb([64, 64], F32)
    W = NCB * 64
    tmB = [[P.sb([64, 4, W], F32) for _ in range(4)] for _ in range(2)]
    fmB = [[P.sb([64, 4, W], F32) for _ in range(5)] for _ in range(2)]
    yB = [P.sb([64, 4, W], F32) for _ in range(2)]
    St = P.sb([64, 4, 64], F32)
    two = lambda shape: [P.sb(shape, F32) for _ in range(2)]
    Ep, Emm, dd, Eprev, AR = two([64, 4, 64]), two([64, 4, 128]), two([64, 4, 64]), two([64, 4, 64]), two([64, 4, 128])
    ktf, btf, ktt, btt = two([64, 4, 64]), two([64, 4, 64]), two([64, 4, 64]), two([64, 4, 64])
    MB, MK, Am = two([64, 4, 128]), two([64, 4, 128]), two([64, 4, 64])
    MA2 = [two([64, 4, 128]) for _ in range(2)]
    Pm = [two([64, 4, 64]) for _ in range(2)]
    Z, U, tmpS = P.sb([64, 4, 64], F32), P.sb([64, 4, 64], F32), P.sb([64, 4, 64], F32)
    Q = [P.ps([64, 4, 128], F32) for _ in range(8)]
    qk = lambda i: ("Q", i)

    P.dma("sp", tri[:], tri_d, w=["tri"])
    P.dma("sp", m2[:], m2_d, w=["m2"])
    P.dma("sp", mT[:], mT_d, w=["mT"])
    P.dma("sp", id64[:], id_d, w=["id64"])
    P.memset("dve", St[:], 0.0, w=["St"])
    m2b = m2[:].unsqueeze(1).broadcast_to([64, 4, 128])
    mTb = mT[:].unsqueeze(1).broadcast_to([64, 4, 64])
    idb = id64[:].unsqueeze(1).broadcast_to([64, 4, 64])

    for c in range(nchunks):
        blk, cl = c // NCB, c % NCB
        bp = blk % 2
        if cl == 0:
            bs = slice(blk * W, (blk + 1) * W)
            for a in range(4):
                P.dma("sp", tmB[bp][a][:], tm_d[a, :, :, bs], w=[("tmB", bp, a)])
            for a in range(5):
                P.dma("act", fmB[bp][a][:], fm_d[a, :, :, bs], w=[("fmB", bp, a)])
        cs = slice(cl * 64, (cl + 1) * 64)
        lw_tm, k_tm, b_tm, v_tm = [tmB[bp][a][:, :, cs] for a in range(4)]
        lw_fm, r_fm, a_fm, k_fm, b_fm = [fmB[bp][a][:, :, cs] for a in range(5)]
        tmk = lambda a: ("tmB", bp, a)
        fmk = lambda a: ("fmB", bp, a)
        p = c % 2
        K_ = lambda name: (name, p)
        for ch in range(4):
            P.mm(Q[0][:, ch, 0:64], lw_tm[:, ch, :], tri[:], r=[tmk(0), "tri"], w=[qk(0)])
            P.mm(Q[0][:, ch, 64:128], tri[:], lw_tm[:, ch, :], r=[tmk(0), "tri"], w=[qk(0)])
        P.act(Ep[p][:], Q[0][:, :, 0:64], AF.Exp, r=[qk(0)], w=[K_("Ep"), qk(0)])
        P.act(Emm[p][:], Q[0][:], AF.Exp, r=[qk(0)], w=[K_("Emm"), qk(0)], scale=-1.0)
        P.tt("dve", dd[p][:], Q[0][:, :, 0:64], lw_fm, ALU.subtract, r=[qk(0), fmk(0)], w=[K_("dd"), qk(0)])
        P.act(Eprev[p][:], dd[p][:], AF.Exp, r=[K_("dd")], w=[K_("Eprev")])
        P.tt("dve", AR[p][:, :, 0:64], a_fm, Eprev[p][:], ALU.mult, r=[fmk(2), K_("Eprev")], w=[K_("ARa")])
        P.tt("pool", AR[p][:, :, 64:128], r_fm, Ep[p][:], ALU.mult, r=[fmk(1), K_("Ep")], w=[K_("ARr")])
        P.tt("dve", ktf[p][:], k_fm, Emm[p][:, :, 0:64], ALU.mult, r=[fmk(3), K_("Emm")], w=[K_("ktf")])
        P.tt("pool", btf[p][:], b_fm, Emm[p][:, :, 0:64], ALU.mult, r=[fmk(4), K_("Emm")], w=[K_("btf")])
        P.tt("dve", ktt[p][:], k_tm, Emm[p][:, :, 64:128], ALU.mult, r=[tmk(1), K_("Emm")], w=[K_("ktt")])
        P.tt("pool", btt[p][:], b_tm, Emm[p][:, :, 64:128], ALU.mult, r=[tmk(2), K_("Emm")], w=[K_("btt")])
        ARk = [K_("ARa"), K_("ARr")]
        for ch in range(4):
            P.mm(Q[1][:, ch, :], btf[p][:, ch, :], AR[p][:, ch, :], r=[K_("btf")] + ARk, w=[qk(1)])
            P.mm(Q[2][:, ch, :], ktf[p][:, ch, :], AR[p][:, ch, :], r=[K_("ktf")] + ARk, w=[qk(2)])
            P.mm(Q[3][:, ch, 0:64], AR[p][:, ch, 0:64], btf[p][:, ch, :], r=[K_("btf")] + ARk, w=[qk(3)])
        P.tt("dve", MB[p][:], Q[1][:], m2b, ALU.mult, r=[qk(1), "m2"], w=[K_("MB")])
        P.tt("dve", MK[p][:], Q[2][:], m2b, ALU.mult, r=[qk(2), "m2"], w=[K_("MK")])
        P.tt("dve", Am[p][:], Q[3][:, :, 0:64], mTb, ALU.mult, r=[qk(3), "mT"], w=[K_("Am")])
        P.tt("pool", Pm[p][0][:], MB[p][:, :, 0:64], idb, ALU.add, r=[K_("MB"), "id64"], w=[K_("P0")])
        curM = lambda ch: MB[p][:, ch, 0:64]
        curA = lambda ch: Am[p][:, ch, :]
        curk = [K_("MB"), K_("Am")]
        pi = 0
        for lvl in range(5):
            mi = lvl % 2
            for ch in range(4):
                P.mm(Q[4][:, ch, 0:64], curA(ch), curM(ch), r=curk, w=[qk(4)])
                P.mm(Q[4][:, ch, 64:128], curM(ch), curA(ch), r=curk, w=[qk(4)])
            P.copy("act", MA2[p][mi][:], Q[4][:], r=[qk(4)], w=[K_("MA2_%d" % mi)])
            curM = lambda ch, mi=mi: MA2[p][mi][:, ch, 0:64]
            curA = lambda ch, mi=mi: MA2[p][mi][:, ch, 64:128]
            curk = [K_("MA2_%d" % mi)]
            for ch in range(4):
                P.mm(Q[5][:, ch, 0:64], curA(ch), Pm[p][pi][:, ch, :], r=curk + [K_("P%d" % pi)], w=[qk(5)])
            P.tt("dve", Pm[p][1 - pi][:], Pm[p][pi][:], Q[5][:, :, 0:64], ALU.add, r=[K_("P%d" % pi), qk(5)], w=[K_("P%d" % (1 - pi))])
            pi = 1 - pi
        Pk = K_("P%d" % pi)
        Pf = Pm[p][pi]
        for ch in range(4):
            P.mm(Q[6][:, ch, 0:64], AR[p][:, ch, 0:64], St[:, ch, :], start=True, stop=False, r=ARk + ["St"], w=[qk(6)])
            P.mm(Q[6][:, ch, 0:64], MK[p][:, ch, 0:64], v_tm[:, ch, :], start=False, stop=True, r=[K_("MK"), tmk(3)], w=[qk(6)])
        P.copy("dve", Z[:], Q[6][:, :, 0:64], r=[qk(6)], w=["Z"])
        for ch in range(4):
            P.mm(Q[7][:, ch, 0:64], Pf[:, ch, :], Z[:, ch, :], r=[Pk, "Z"], w=[qk(7)])
        P.copy("dve", U[:], Q[7][:, :, 0:64], r=[qk(7)], w=["U"])
        for ch in range(4):
            P.mm(Q[7][:, ch, 64:128], AR[p][:, ch, 64:128], St[:, ch, :], start=True, stop=False, r=ARk + ["St"], w=[qk(7)])
            P.mm(Q[7][:, ch, 64:128], MB[p][:, ch, 64:128], U[:, ch, :], start=False, stop=False, r=[K_("MB"), "U"], w=[qk(7)])
            P.mm(Q[7][:, ch, 64:128], MK[p][:, ch, 64:128], v_tm[:, ch, :], start=False, stop=True, r=[K_("MK"), tmk(3)], w=[qk(7)])
        for ch in range(4):
            P.mm(Q[6][:, ch, 64:128], btt[p][:, ch, :], U[:, ch, :], start=True, stop=False, r=[K_("btt"), "U"], w=[qk(6)])
            P.mm(Q[6][:, ch, 64:128], ktt[p][:, ch, :], v_tm[:, ch, :], start=False, stop=True, r=[K_("ktt"), tmk(3)], w=[qk(6)])
        P.copy("act", yB[bp][:, :, cs], Q[7][:, :, 64:128], r=[qk(7)], w=[("yB", bp, cl)])
        P.tt("dve", tmpS[:], Q[6][:, :, 64:128], St[:], ALU.add, r=[qk(6), "St"], w=["tmpS"])
        P.tt("dve", St[:], tmpS[:], Ep[p][:, :, 63:64].broadcast_to([64, 4, 64]), ALU.mult, r=["tmpS", K_("Ep")], w=["St"])
        if cl == NCB - 1 or c == nchunks - 1:
            bs = slice(blk * W, (blk + 1) * W)
            P.dma("sp", y_d[:, :, bs], yB[bp][:], r=[("yB", bp, i) for i in range(NCB)], w=[("y", blk)], is_out=True)
    return P.build()


def scan_consts():
    s = np.arange(64)[:, None]
    t = np.arange(64)[None, :]
    tri = (s <= t).astype(np.float32)
    m2 = np.concatenate([(s < t).astype(np.float32), (s <= t).astype(np.float32)], 1)
    mT = (t < s).astype(np.float32)
    return {"tri": tri, "m2": m2, "mT": mT, "id64": np.eye(64, dtype=np.float32)}


def tm_layout(x):
    return x.reshape(NCH, 64, 64).transpose(1, 0, 2).reshape(64, S)


def tm_unlayout(y):
    return y.reshape(64, NCH, 64).transpose(1, 0, 2).reshape(S, 64)


SCAN_ARRS = ["r", "v", "a", "lw0", "lw1", "kd0", "kd1", "b0", "b1", "bonus", "g"]
EXPM05 = float(np.exp(-0.5))
GELU_C = 1.5957691216057308


def gelu_tanh(P, out, x_ps, xkey, okey, t1, k1, t2, k2):
    P.copy("act", t1[:], x_ps, r=[xkey], w=[k1])
    P.tt("dve", t2[:], t1[:], t1[:], ALU.mult, r=[k1], w=[k2])
    P.ts("dve", t2[:], t2[:], 0.044715, 1.0, ALU.mult, ALU.add, r=[k2], w=[k2])
    P.tt("dve", t2[:], t2[:], t1[:], ALU.mult, r=[k2, k1], w=[k2])
    P.act(t2[:], t2[:], AF.Sigmoid, r=[k2], w=[k2], scale=GELU_C)
    P.tt("dve", out, t2[:], t1[:], ALU.mult, r=[k2, k1], w=[okey])


def build_O1():
    P = Prog()
    x2_d = P.dram("x2", [TOK, D], F32)
    pt_d = P.dram("parts", [8, TOK, D], F32)
    x2h_d = P.dram("x2h", [2, D], F32)
    pth_d = P.dram("partsh", [8, 2, D], F32)
    g_d = P.dram("g", [1, D], F32)
    w_d = P.dram("w", [D, 2944], F32)
    lng_d = P.dram("lng", [1, 512], F32)
    lnb_d = P.dram("lnb", [1, 512], F32)
    wsT_d = P.dram("wsT", [128, 4, 128], F32)
    bsT_d = P.dram("bsT", [128, 4], F32)
    mu_d = P.dram("mu", [128, 15, 2], F32)
    w2p_d = P.dram("w2p", [128, 2, 512], F32)
    a2p_d = P.dram("a2p", [128, 2, 512], F32)
    g2_d = P.dram("g2", [128, 512], F32)
    pv_d = P.dram("pvec", [128, 4, 8], F32)
    bo_d = P.dram("bones", [128, 128], F32)
    ident_d = P.dram("ident", [128, 128], F32)
    x3_o = P.dram("x3_o", [TOK, D], F32, kind="ExternalOutput")
    yc_o = P.dram("yc_o", [TOK, 512], BF16, kind="ExternalOutput")
    sc_o = P.dram("sc_o", [len(SCAN_ARRS), 4, 128, TOK], F32, kind="ExternalOutput")

    gb = P.sb([128, D], F32)
    wc = P.sb([128, 8, 2944], BF16)
    lng = P.sb([128, 512], F32)
    lnb = P.sb([128, 512], F32)
    wsTf = P.sb([128, 4, 128], F32)
    wsT = P.sb([128, 4, 128], BF16)
    bsT = P.sb([128, 4], F32)
    mu = P.sb([128, 15, 2], F32)
    c0 = P.sb([128, 15], F32)
    w2pf = P.sb([128, 2, 512], F32)
    a2pf = P.sb([128, 2, 512], F32)
    g2f = P.sb([128, 512], F32)
    w2p = P.sb([128, 2, 512], BF16)
    a2p = P.sb([128, 2, 512], BF16)
    g2 = P.sb([128, 512], BF16)
    pv = P.sb([128, 4, 8], F32)
    bones = P.sb([128, 128], F32)
    identf = P.sb([128, 128], F32)
    ident = P.sb([128, 128], BF16)
    scr = norm_scratch(P)
    acc = [P.sb([128, D], F32) for _ in range(2)]
    prt = [P.sb([128, D], F32) for _ in range(2)]
    h = P.sb([128, D], BF16)
    hT = P.sb([128, 8, TOK + 2], BF16)
    T = [P.sb([128, 512], F32) for _ in range(22)]
    tk = lambda i: ("T", i)
    ub = P.sb([128, 512], BF16)
    vnb = P.sb([128, 512], BF16)
    ycb = [P.sb([128, 512], BF16) for _ in range(2)]
    mean = P.sb([128, 1], F32)
    var = P.sb([128, 1], F32)
    fd = P.sb([128, 514], F32)
    thb = P.sb([128, 512], BF16)
    hab = P.sb([128, 512], BF16)
    sgb = P.sb([128, 512], BF16)
    Bt = P.ps([128, 8, 128], BF16)
    Bc = [P.ps([128, 512], F32) for _ in range(2)]
    Bs = P.ps([128, 4, 128], F32)
    Bm = P.ps([128, 512], F32)
    Be = P.ps([128, 512], F32)
    Bl = [P.ps([128, 512], F32) for _ in range(2)]

    P.dma("sp", gb[:], g_d.partition_broadcast(128), w=["gb"])
    P.dma("sp", lng[:], lng_d.partition_broadcast(128), w=["lng"])
    P.dma("sp", lnb[:], lnb_d.partition_broadcast(128), w=["lnb"])
    P.dma("sp", identf[:], ident_d, w=["identf"])
    P.copy("dve", ident[:], identf[:], r=["identf"], w=["ident"])
    P.dma("sp", wsTf[:], wsT_d, w=["wsTf"])
    P.copy("dve", wsT[:], wsTf[:], r=["wsTf"], w=["wsT"])
    P.dma("sp", bsT[:], bsT_d, w=["bsT"])
    P.dma("sp", mu[:], mu_d, w=["mu"])
    P.dma("sp", w2pf[:], w2p_d, w=["w2pf"])
    P.dma("sp", a2pf[:], a2p_d, w=["a2pf"])
    P.dma("sp", g2f[:], g2_d, w=["g2f"])
    P.copy("dve", w2p[:], w2pf[:], r=["w2pf"], w=["w2p"])
    P.copy("dve", a2p[:], a2pf[:], r=["a2pf"], w=["a2p"])
    P.copy("dve", g2[:], g2f[:], r=["g2f"], w=["g2"])
    P.dma("sp", pv[:], pv_d, w=["pv"])
    P.ts("dve", pv[:, :, 7], pv[:, :, 5], -1.0, 1.0, ALU.mult, ALU.add, r=["pv"], w=["pv"])
    P.dma("sp", bones[:], bo_d, w=["bones"])
    P.tt("dve", c0[:], mu[:, :, 0], mu[:, :, 1], ALU.add, r=["mu"], w=["c0"])
    P.ts("dve", c0[:], c0[:], -1.0, 1.0, ALU.mult, ALU.add, r=["c0"], w=["c0"])
    for k in range(8):
        P.dma("pool", wc[:, k, 0:1472], w_d[k * 128:(k + 1) * 128, 0:1472], w=[("wc", k)])
        P.dma("pool", wc[:, k, 1472:2944], w_d[k * 128:(k + 1) * 128, 1472:2944], w=[("wc", k)])
    wck = [("wc", k) for k in range(8)]
    P.dma("sp", acc[0][0:2, :], x2h_d, w=[("acc", 0)])
    for c in range(8):
        P.dma("sp", prt[c % 2][0:2, :], pth_d[c], w=[("prt", c % 2)])
        P.tt("dve", acc[0][0:2, :], acc[0][0:2, :], prt[c % 2][0:2, :], ALU.add, r=[("acc", 0), ("prt", c % 2)], w=[("acc", 0)])
    sq, ss, rstd = scr["sq"], scr["ss"], scr["rstd"]
    P.act(sq[0:2, :], acc[0][0:2, :], AF.Square, r=[("acc", 0)], w=["sq", "ss"], accum_out=ss[0:2, :])
    P.ts("dve", rstd[0:2, :], ss[0:2, :], 1.0 / D, 1e-6, ALU.mult, ALU.add, r=["ss"], w=["rstd"])
    P.act(rstd[0:2, :], rstd[0:2, :], AF.Sqrt, r=["rstd"], w=["rstd"])
    P.op("dve", lambda e: e.reciprocal(out=rstd[0:2, :], in_=rstd[0:2, :]), ["rstd"], ["rstd"])
    P.stt("dve", h[0:2, :], acc[0][0:2, :], rstd[0:2, :], gb[0:2, :], ALU.mult, ALU.mult, r=[("acc", 0), "rstd", "gb"], w=["h"])
    for k in range(8):
        P.tr(Bt[:, k, 0:2], h[0:2, k * 128:(k + 1) * 128], ident[0:2, 0:2], r=["h", "ident"], w=["Bt"])
    P.copy("act", hT[:, :, 0:1], Bt[:, :, 0:1], r=["Bt"], w=[("hT", "h0")])
    P.copy("act", hT[:, :, TOK + 1:TOK + 2], Bt[:, :, 1:2], r=["Bt"], w=[("hT", "h1")])
    for t in range(NT):
        a = acc[t % 2]
        ak = ("acc", t % 2)
        ts_ = slice(t * 128, (t + 1) * 128)
        P.dma("sp", a[:], x2_d[ts_, :], w=[ak])
        for c in range(8):
            pb = prt[c % 2]
            P.dma("act" if c % 2 else "sp", pb[:], pt_d[c, ts_, :], w=[("prt", c % 2)])
            P.tt("dve" if c % 2 else "pool", a[:], a[:], pb[:], ALU.add, r=[ak, ("prt", c % 2)], w=[ak])
        P.dma("sp", x3_o[ts_, :], a[:], r=[ak], w=[("x3_o", t)], is_out=True)
        rmsnorm_tile(P, a[:], ak, gb[:], h[:], "h", scr)
        for k in range(8):
            P.tr(Bt[:, k, :], h[:, k * 128:(k + 1) * 128], ident[:], r=["h", "ident"], w=["Bt"])
        P.copy("act", hT[:, :, 1 + t * 128:1 + (t + 1) * 128], Bt[:], r=["Bt"], w=[("hT", t)])
        for k in range(8):
            P.mm(Bc[0][:], hT[:, k, 1 + t * 128:1 + (t + 1) * 128], wc[:, k, 0:512], start=(k == 0), stop=(k == 7), r=[("hT", t)] + wck, w=["Bc0"])
        for k in range(8):
            P.mm(Bc[1][:], hT[:, k, 1 + t * 128:1 + (t + 1) * 128], wc[:, k, 512:1024], start=(k == 0), stop=(k == 7), r=[("hT", t)] + wck, w=["Bc1"])
        gelu_tanh(P, ub[:], Bc[0][:], "Bc0", "ub", T[0], tk(0), T[1], tk(1))
        gelu_tanh(P, T[4][:], Bc[1][:], "Bc1", tk(4), T[2], tk(2), T[3], tk(3))
        P.op("dve", lambda e: e.reduce_sum(out=mean[:], in_=T[4][:], axis=AX.X), [tk(4)], ["mean"])
        P.ts("dve", mean[:], mean[:], -1.0 / 512, None, ALU.mult, r=["mean"], w=["mean"])
        P.ts("dve", T[5][:], T[4][:], mean[:], None, ALU.add, r=[tk(4), "mean"], w=[tk(5)])
        P.act(T[2][:], T[5][:], AF.Square, r=[tk(5)], w=[tk(2), "var"], accum_out=var[:])
        P.ts("dve", var[:], var[:], 1.0 / 512, 1e-5, ALU.mult, ALU.add, r=["var"], w=["var"])
        P.act(var[:], var[:], AF.Sqrt, r=["var"], w=["var"])
        P.op("dve", lambda e: e.reciprocal(out=var[:], in_=var[:]), ["var"], ["var"])
        P.stt("dve", T[5][:], T[5][:], var[:], lng[:], ALU.mult, ALU.mult, r=[tk(5), "var", "lng"], w=[tk(5)])
        P.tt("dve", vnb[:], T[5][:], lnb[:], ALU.add, r=[tk(5), "lnb"], w=["vnb"])
        for g in range(4):
            P.mm(Bs[:, g, :], wsT[:, g, :], vnb[:, g * 128:(g + 1) * 128], r=["wsT", "vnb"], w=["Bs"])
        P.tt("dve", T[5][:].rearrange("p (g c) -> p g c", c=128), Bs[:], bsT[:].unsqueeze(2).broadcast_to([128, 4, 128]), ALU.add,
             r=["Bs", "bsT"], w=[tk(5)])
        yb = ycb[t % 2]
        P.tt("dve", yb[:], T[5][:], ub[:], ALU.mult, r=[tk(5), "ub"], w=[("ycb", t % 2)])
        P.dma("sp", yc_o[ts_, :], yb[:], r=[("ycb", t % 2)], w=[("yc_o", t)], is_out=True)
    hTk = [("hT", t) for t in range(NT)] + [("hT", "h0"), ("hT", "h1")]

    def shifted(blk, chunk, out_tile, okey):
        c0s = slice(blk * 512, blk * 512 + 514)
        col = 1024 + chunk * 128
        for k in range(8):
            P.mm(Bm[:], wc[:, k, col:col + 128], hT[:, k, 1 + blk * 512:1 + (blk + 1) * 512], start=(k == 0), stop=(k == 7), r=hTk + wck, w=["Bm"])
        for k in range(8):
            P.mm(Be[:, 0:2], wc[:, k, col:col + 128], hT[:, k, blk * 512:blk * 512 + 514:513], start=(k == 0), stop=(k == 7), r=hTk + wck, w=["Be"])
        P.copy("act", fd[:, 1:513], Bm[:], r=["Bm"], w=["fd_m"])
        P.copy("act", fd[:, 0:514:513], Be[:, 0:2], r=["Be"], w=["fd_e"])
        fk = ["fd_m", "fd_e"]
        P.ts("dve", out_tile[:], fd[:, 1:513], c0[:, chunk:chunk + 1], None, ALU.mult, r=fk + ["c0"], w=[okey])
        P.stt("dve", out_tile[:], fd[:, 0:512], mu[:, chunk, 0:1], out_tile[:], ALU.mult, ALU.add, r=fk + ["mu", okey], w=[okey])
        P.stt("dve", out_tile[:], fd[:, 2:514], mu[:, chunk, 1:2], out_tile[:], ALU.mult, ALU.add, r=fk + ["mu", okey], w=[okey])

    nb = 0
    for blk in range(TOK // 512):
        bs_ = slice(blk * 512, (blk + 1) * 512)
        shifted(blk, 12, T[0], tk(0))
        P.act(thb[:], T[0][:], AF.Tanh, r=[tk(0)], w=["thb"])
        shifted(blk, 13, T[0], tk(0))
        P.copy("act", hab[:], T[0][:], r=[tk(0)], w=["hab"])
        shifted(blk, 14, T[0], tk(0))
        P.act(sgb[:], T[0][:], AF.Sigmoid, r=[tk(0)], w=["sgb"])
        for pc in range(4):
            cs_ = slice(pc * 128, (pc + 1) * 128)
            PV = lambda i: pv[:, pc, i:i + 1]
            o = lambda name, tile, key: P.dma("sp", sc_o[SCAN_ARRS.index(name), pc, :, bs_], tile[:], r=[key], w=[("sc_o", name, pc, blk)], is_out=True)
            shifted(blk, pc, T[1], tk(1))
            shifted(blk, 4 + pc, T[2], tk(2))
            shifted(blk, 8 + pc, T[3], tk(3))
            o("r", T[1], tk(1))
            o("v", T[3], tk(3))
            for dr in range(2):
                bl = Bl[nb % 2]
                blk_ = ("Bl", nb % 2)
                nb += 1
                P.mm(bl[:], w2p[:, dr, cs_], thb[:], r=["w2p", "thb"], w=[blk_])
                P.act(T[4 + dr][:], bl[:], AF.Sigmoid, r=[blk_, "pv"], w=[tk(4 + dr)], bias=PV(dr))
                P.ts("pool", T[4 + dr][:], T[4 + dr][:], -EXPM05, None, ALU.mult, r=[tk(4 + dr)], w=[tk(4 + dr)])
                o("lw%d" % dr, T[4 + dr], tk(4 + dr))
                bl = Bl[nb % 2]
                blk_ = ("Bl", nb % 2)
                nb += 1
                P.mm(bl[:], a2p[:, dr, cs_], hab[:], r=["a2p", "hab"], w=[blk_])
                P.act(T[6 + dr][:], bl[:], AF.Sigmoid, r=[blk_, "pv"], w=[tk(6 + dr)], bias=PV(2 + dr))
            bl = Bl[nb % 2]
            blk_ = ("Bl", nb % 2)
            nb += 1
            P.mm(bl[:], g2[:, cs_], sgb[:], r=["g2", "sgb"], w=[blk_])
            P.copy("act", T[8][:], bl[:], r=[blk_], w=[tk(8)])
            o("g", T[8], tk(8))
            P.ts("dve", T[9][:], T[2][:], PV(4), None, ALU.mult, r=[tk(2), "pv"], w=[tk(9)])
            P.tt("dve", T[10][:], T[9][:], T[9][:], ALU.mult, r=[tk(9)], w=[tk(10)])
            bl = Bl[nb % 2]
            blk_ = ("Bl", nb % 2)
            nb += 1
            P.mm(bl[:], bones[:], T[10][:], r=["bones", tk(10)], w=[blk_])
            P.act(T[10][:], bl[:], AF.Sqrt, r=[blk_], w=[tk(10)])
            P.ts("dve", T[10][:], T[10][:], 1e-12, None, ALU.max, r=[tk(10)], w=[tk(10)])
            P.op("dve", lambda e: e.reciprocal(out=T[10][:], in_=T[10][:]), [tk(10)], [tk(10)])
            P.tt("dve", T[9][:], T[9][:], T[10][:], ALU.mult, r=[tk(9), tk(10)], w=[tk(9)])
            P.ts("pool", T[11][:], T[9][:], -1.0, None, ALU.mult, r=[tk(9)], w=[tk(11)])
            o("a", T[11], tk(11))
            for dr in range(2):
                P.ts("dve", T[12 + dr][:], T[6 + dr][:], PV(5), PV(7), ALU.mult, ALU.add, r=[tk(6 + dr), "pv"], w=[tk(12 + dr)])
                P.tt("dve", T[12 + dr][:], T[12 + dr][:], T[2][:], ALU.mult, r=[tk(12 + dr), tk(2)], w=[tk(12 + dr)])
                o("kd%d" % dr, T[12 + dr], tk(12 + dr))
                P.tt("pool", T[14 + dr][:], T[9][:], T[6 + dr][:], ALU.mult, r=[tk(9), tk(6 + dr)], w=[tk(14 + dr)])
                o("b%d" % dr, T[14 + dr], tk(14 + dr))
            P.tt("dve", T[16][:], T[12][:], T[13][:], ALU.add, r=[tk(12), tk(13)], w=[tk(16)])
            P.tt("dve", T[16][:], T[16][:], T[1][:], ALU.mult, r=[tk(16), tk(1)], w=[tk(16)])
            P.ts("dve", T[16][:], T[16][:], PV(6), None, ALU.mult, r=[tk(16), "pv"], w=[tk(16)])
            bl = Bl[nb % 2]
            blk_ = ("Bl", nb % 2)
            nb += 1
            P.mm(bl[:], bones[:], T[16][:], r=["bones", tk(16)], w=[blk_])
            P.tt("dve", T[17][:], bl[:], T[3][:], ALU.mult, r=[blk_, tk(3)], w=[tk(17)])
            o("bonus", T[17], tk(17))
    return P.build()


def run_O1(x2, parts, prm):
    if "O1" not in _cache:
        _cache["O1"] = build_O1()
    nc = _cache["O1"]
    wsT = np.ascontiguousarray(prm["c_w_s"].transpose(2, 0, 1))
    bsT = np.ascontiguousarray(prm["c_b_s"].T)
    shift = prm["d_shift"]
    mu = np.ascontiguousarray(shift.reshape(2, 15, 128).transpose(2, 1, 0))
    w2p = np.zeros((128, 2, 512), np.float32)
    a2p = np.zeros((128, 2, 512), np.float32)
    for dr in range(2):
        w2p[dr * 64:(dr + 1) * 64, dr, :] = prm["d_w2"][dr]
        a2p[dr * 64:(dr + 1) * 64, dr, :] = prm["d_a2"][dr]
    pvec = np.zeros((128, 4, 8), np.float32)
    col = lambda v: v.reshape(4, 128).T
    pvec[:, :, 0] = col(prm["d_w0"][0]); pvec[:, :, 1] = col(prm["d_w0"][1])
    pvec[:, :, 2] = col(prm["d_a0"][0]); pvec[:, :, 3] = col(prm["d_a0"][1])
    pvec[:, :, 4] = col(prm["d_k_k"]); pvec[:, :, 5] = col(prm["d_k_a"]); pvec[:, :, 6] = col(prm["d_r_k"].reshape(512))
    bones = np.kron(np.eye(2, dtype=np.float32), np.ones((64, 64), np.float32))
    xfull = [np.concatenate([np.asarray(x2[b * 4 + i]) for i in range(4)], 0) for b in range(NB)]
    maps = []
    for c in range(8):
        b, q = c // 4, c % 4
        lo, hi = q * TOK, (q + 1) * TOK
        x2h = np.zeros((2, D), np.float32)
        pth = np.zeros((8, 2, D), np.float32)
        if lo > 0:
            x2h[0] = xfull[b][lo - 1]
            for cc in range(8):
                pth[cc, 0] = parts[cc][b, lo - 1]
        if hi < S:
            x2h[1] = xfull[b][hi]
            for cc in range(8):
                pth[cc, 1] = parts[cc][b, hi]
        maps.append({"x2": np.ascontiguousarray(x2[c]), "parts": np.ascontiguousarray(np.stack([parts[cc][b, lo:hi] for cc in range(8)], 0)),
                     "x2h": x2h, "partsh": pth, "g": prm["o_norm"].reshape(1, D), "w": prm["o_w_in"],
                     "lng": prm["c_ln_g"].reshape(1, 512), "lnb": prm["c_ln_b"].reshape(1, 512), "wsT": wsT, "bsT": bsT, "mu": mu,
                     "w2p": w2p, "a2p": a2p, "g2": prm["d_g2"], "pvec": pvec, "bones": bones, "ident": _ident_np()})
    res = run(nc, maps)
    return ([res.results[c]["x3_o"] for c in range(8)], [res.results[c]["yc_o"] for c in range(8)],
            [res.results[c]["sc_o"] for c in range(8)])


def build_O2():
    P = Prog()
    yf_d = P.dram("yf", [TOK, 512], F32)
    yb_d = P.dram("yb", [TOK, 512], F32)
    bo_d = P.dram("bonus", [TOK, 512], F32)
    gt_d = P.dram("gt", [TOK, 512], F32)
    yc_d = P.dram("yc", [TOK, 512], BF16)
    x3_d = P.dram("x3", [TOK, D], F32)
    lng_d = P.dram("lng", [1, 512], F32)
    lnb_d = P.dram("lnb", [1, 512], F32)
    wo_d = P.dram("wo", [D, D], F32)
    ident_d = P.dram("ident", [128, 128], F32)
    x4_o = P.dram("x4_o", [TOK, D], F32, kind="ExternalOutput")

    lng = P.sb([128, 512], F32)
    lnb = P.sb([128, 512], F32)
    wo = P.sb([128, 8, D], BF16)
    identf = P.sb([128, 128], F32)
    ident = P.sb([128, 128], BF16)
    yf = [P.sb([128, 512], F32) for _ in range(2)]
    yb = [P.sb([128, 512], F32) for _ in range(2)]
    bo = [P.sb([128, 512], F32) for _ in range(2)]
    gt = [P.sb([128, 512], F32) for _ in range(2)]
    x3 = [P.sb([128, D], F32) for _ in range(2)]
    cat = [P.sb([128, D], BF16) for _ in range(2)]
    y = P.sb([128, 512], F32)
    sq = P.sb([128, 512], F32)
    st = P.sb([128, 8], F32)
    catT = P.sb([128, 8, 128], BF16)
    xo = [P.sb([128, D], F32) for _ in range(2)]
    ptr = P.ps([128, 8, 128], BF16)
    pso = [P.ps([128, 512], F32) for _ in range(2)]

    P.dma("sp", lng[:], lng_d.partition_broadcast(128), w=["lng"])
    P.dma("sp", lnb[:], lnb_d.partition_broadcast(128), w=["lnb"])
    P.dma("sp", identf[:], ident_d, w=["identf"])
    P.copy("dve", ident[:], identf[:], r=["identf"], w=["ident"])
    for k in range(8):
        P.dma("pool", wo[:, k, :], wo_d[k * 128:(k + 1) * 128, :], w=[("wo", k)])
    wok = [("wo", k) for k in range(8)]
    v3 = lambda ap: ap.rearrange("p (h c) -> p h c", c=64)
    bc = lambda ap: ap.unsqueeze(2).broadcast_to([128, 8, 64])
    for t in range(NT):
        i = t % 2
        ts_ = slice(t * 128, (t + 1) * 128)
        P.dma("sp", yf[i][:], yf_d[ts_, :], w=[("yf", i)])
        P.dma("act", yb[i][:], yb_d[ts_, :], w=[("yb", i)])
        P.dma("sp", bo[i][:], bo_d[ts_, :], w=[("bo", i)])
        P.dma("act", gt[i][:], gt_d[ts_, :], w=[("gt", i)])
        P.dma("sp", x3[i][:], x3_d[ts_, :], w=[("x3", i)])
        P.dma("act", cat[i][:, 0:512], yc_d[ts_, :], w=[("cat", i, 0)])
        P.tt("dve", y[:], yf[i][:], yb[i][:], ALU.add, r=[("yf", i), ("yb", i)], w=["y"])
        P.op("dve", lambda e: e.tensor_reduce(out=st[:], in_=v3(y[:]), axis=AX.X, op=ALU.add), ["y"], ["st"])
        P.ts("dve", st[:], st[:], -1.0 / 64, None, ALU.mult, r=["st"], w=["st"])
        P.tt("dve", v3(y[:]), v3(y[:]), bc(st[:]), ALU.add, r=["y", "st"], w=["y"])
        P.tt("pool", sq[:], y[:], y[:], ALU.mult, r=["y"], w=["sq"])
        P.op("dve", lambda e: e.tensor_reduce(out=st[:], in_=v3(sq[:]), axis=AX.X, op=ALU.add), ["sq"], ["st"])
        P.ts("dve", st[:], st[:], 1.0 / 64, 64e-5, ALU.mult, ALU.add, r=["st"], w=["st"])
        P.act(st[:], st[:], AF.Sqrt, r=["st"], w=["st"])
        P.op("dve", lambda e: e.reciprocal(out=st[:], in_=st[:]), ["st"], ["st"])
        P.tt("dve", v3(y[:]), v3(y[:]), bc(st[:]), ALU.mult, r=["y", "st"], w=["y"])
        P.tt("dve", y[:], y[:], lng[:], ALU.mult, r=["y", "lng"], w=["y"])
        P.tt("dve", y[:], y[:], lnb[:], ALU.add, r=["y", "lnb"], w=["y"])
        P.tt("dve", y[:], y[:], bo[i][:], ALU.add, r=["y", ("bo", i)], w=["y"])
        P.tt("dve", cat[i][:, 512:1024], y[:], gt[i][:], ALU.mult, r=["y", ("gt", i)], w=[("cat", i, 1)])
        ck = [("cat", i, 0), ("cat", i, 1)]
        for k in range(8):
            P.tr(ptr[:, k, :], cat[i][:, k * 128:(k + 1) * 128], ident[:], r=ck + ["ident"], w=["ptr"])
        P.copy("act", catT[:], ptr[:], r=["ptr"], w=["catT"])
        for hf_ in range(2):
            for k in range(8):
                P.mm(pso[hf_][:], catT[:, k, :], wo[:, k, hf_ * 512:(hf_ + 1) * 512], start=(k == 0), stop=(k == 7), r=["catT"] + wok, w=[("pso", hf_)])
            P.tt("dve", xo[i][:, hf_ * 512:(hf_ + 1) * 512], pso[hf_][:], x3[i][:, hf_ * 512:(hf_ + 1) * 512], ALU.add,
                 r=[("pso", hf_), ("x3", i)], w=[("xo", i, hf_)])
        P.dma("sp", x4_o[ts_, :], xo[i][:], r=[("xo", i, 0), ("xo", i, 1)], w=[("x4_o", t)], is_out=True)
    return P.build()


def build_F():
    P = Prog()
    x_d = P.dram("x", [TOK, D], F32)
    pt_d = P.dram("parts", [8, TOK, D], F32)
    g_d = P.dram("g", [1, D], F32)
    o_d = P.dram("o", [TOK, D], F32, kind="ExternalOutput")
    gb = P.sb([128, D], F32)
    scr = norm_scratch(P)
    acc = [P.sb([128, D], F32) for _ in range(2)]
    prt = [P.sb([128, D], F32) for _ in range(2)]
    ob = [P.sb([128, D], F32) for _ in range(2)]
    P.dma("sp", gb[:], g_d.partition_broadcast(128), w=["gb"])
    for t in range(NT):
        a = acc[t % 2]
        ak = ("acc", t % 2)
        ts_ = slice(t * 128, (t + 1) * 128)
        P.dma("sp", a[:], x_d[ts_, :], w=[ak])
        for c in range(8):
            pb = prt[c % 2]
            P.dma("act" if c % 2 else "sp", pb[:], pt_d[c, ts_, :], w=[("prt", c % 2)])
            P.tt("dve" if c % 2 else "pool", a[:], a[:], pb[:], ALU.add, r=[ak, ("prt", c % 2)], w=[ak])
        rmsnorm_tile(P, a[:], ak, gb[:], ob[t % 2][:], ("ob", t % 2), scr)
        P.dma("sp", o_d[ts_, :], ob[t % 2][:], r=[("ob", t % 2)], w=[("o", t)], is_out=True)
    return P.build()


def run_O2(yf, yb, bonus, gt, yc, x3, prm):
    if "O2" not in _cache:
        _cache["O2"] = build_O2()
    nc = _cache["O2"]
    maps = []
    for c in range(8):
        maps.append({"yf": yf[c], "yb": yb[c], "bonus": bonus[c], "gt": gt[c], "yc": np.ascontiguousarray(yc[c]), "x3": np.ascontiguousarray(x3[c]),
                     "lng": prm["d_ln_g"].reshape(1, 512), "lnb": prm["d_ln_b"].reshape(1, 512), "wo": prm["o_w_out"], "ident": _ident_np()})
    res = run(nc, maps)
    return [res.results[c]["x4_o"] for c in range(8)]


def run_F(x5, parts, final_norm):
    if "F" not in _cache:
        _cache["F"] = build_F()
    nc = _cache["F"]
    maps = []
    for c in range(8):
        b, q = c // 4, c % 4
        maps.append({"x": np.ascontiguousarray(x5[c]), "parts": np.ascontiguousarray(np.stack([parts[cc][b, q * TOK:(q + 1) * TOK] for cc in range(8)], 0)),
                     "g": final_norm.reshape(1, D)})
    res = run(nc, maps)
    return [res.results[c]["o"] for c in range(8)]


def run_S(sc):
    if "S" not in _cache:
        _cache["S"] = build_S()
    nc = _cache["S"]
    ai = {n: i for i, n in enumerate(SCAN_ARRS)}
    full = [np.concatenate([np.asarray(sc[b * 4 + i]).reshape(len(SCAN_ARRS), 512, TOK) for i in range(4)], axis=2) for b in range(NB)]
    consts = scan_consts()
    maps = []
    for h in range(8):
        tm = np.zeros((4, 64, 4, S), np.float32)
        fm = np.zeros((5, 64, 4, S), np.float32)
        hs = slice(h * 64, (h + 1) * 64)
        for b in range(NB):
            for dr in range(2):
                ci = b * 2 + dr
                fl = (lambda a: a) if dr == 0 else (lambda a: a[:, ::-1])
                get = lambda n: fl(full[b][ai[n], hs, :])
                lw, r, a, k, bb, v = get("lw%d" % dr), get("r"), get("a"), get("kd%d" % dr), get("b%d" % dr), get("v")
                for j, arr in enumerate([lw, r, a, k, bb]):
                    fm[j, :, ci, :] = arr
                for j, arr in enumerate([lw, k, bb, v]):
                    tm[j, :, ci, :] = tm_layout(np.ascontiguousarray(arr.T))
        m = {"tm": tm, "fm": fm}
        m.update(consts)
        maps.append(m)
    res = run(nc, maps)
    yfull = np.zeros((NB, 2, S, 512), np.float32)
    for h in range(8):
        yo = np.asarray(res.results[h]["y"])
        for b in range(NB):
            for dr in range(2):
                yy = tm_unlayout(yo[:, b * 2 + dr, :])
                yfull[b, dr, :, h * 64:(h + 1) * 64] = yy if dr == 0 else yy[::-1]
    yf = [np.ascontiguousarray(yfull[c // 4, 0, (c % 4) * TOK:(c % 4 + 1) * TOK]) for c in range(8)]
    yb = [np.ascontiguousarray(yfull[c // 4, 1, (c % 4) * TOK:(c % 4 + 1) * TOK]) for c in range(8)]
    return yf, yb


def kernel(x, mem, e_norm, e_w_in, e_sink, e_w_out, o_norm, o_w_in, c_ln_g, c_ln_b, c_w_s, c_b_s, d_shift, d_w0, d_w2, d_a0, d_a2,
           d_g2, d_k_k, d_k_a, d_r_k, d_ln_g, d_ln_b, o_w_out, x_norm, m_norm, x_wq, x_wkv, x_wo, f_norm, f_router, f_w_gate,
           f_w_up, f_w_down, final_norm):
    f = lambda a: np.asarray(a, dtype=np.float32)
    x, mem = f(x), f(mem)
    qk, v = run_A1(x, f(e_norm)[0], f(e_w_in)[0])
    x1 = run_A2(qk, v, x, f(e_sink)[0], f(e_w_out)[0])
    x2, hf, aff = run_A3(x1, mem, f(x_norm)[0], f(m_norm)[0], f(x_wq)[0], f(x_wkv)[0], f(x_wo)[0], f(f_norm)[0], f(f_router)[0])
    parts = run_M(hf, aff, f(f_w_gate)[0], f(f_w_up)[0], f(f_w_down)[0])
    prm = {"o_norm": f(o_norm)[0], "o_w_in": f(o_w_in)[0], "c_ln_g": f(c_ln_g)[0], "c_ln_b": f(c_ln_b)[0], "c_w_s": f(c_w_s)[0],
           "c_b_s": f(c_b_s)[0], "d_shift": f(d_shift)[0], "d_w0": f(d_w0)[0], "d_w2": f(d_w2)[0], "d_a0": f(d_a0)[0], "d_a2": f(d_a2)[0],
           "d_g2": f(d_g2)[0], "d_k_k": f(d_k_k)[0], "d_k_a": f(d_k_a)[0], "d_r_k": f(d_r_k)[0], "d_ln_g": f(d_ln_g)[0],
           "d_ln_b": f(d_ln_b)[0], "o_w_out": f(o_w_out)[0]}
    x3, yc, sc = run_O1(x2, parts, prm)
    yf, yb = run_S(sc)
    ai = {n: i for i, n in enumerate(SCAN_ARRS)}
    tmaj = lambda c, n: np.ascontiguousarray(np.asarray(sc[c])[ai[n]].reshape(512, TOK).T)
    bonus = [tmaj(c, "bonus") for c in range(8)]
    gt = [tmaj(c, "g") for c in range(8)]
    x4 = run_O2(yf, yb, bonus, gt, yc, x3, prm)
    x5, hf, aff = run_A3(x4, mem, f(x_norm)[1], f(m_norm)[1], f(x_wq)[1], f(x_wkv)[1], f(x_wo)[1], f(f_norm)[1], f(f_router)[1])
    parts = run_M(hf, aff, f(f_w_gate)[1], f(f_w_up)[1], f(f_w_down)[1])
    o = run_F(x5, parts, f(final_norm))
    out = np.zeros((NB, S, D), np.float32)
    for c in range(8):
        out[c // 4, (c % 4) * TOK:(c % 4 + 1) * TOK] = np.asarray(o[c])
    return out
```

```python
import ml_dtypes
import numpy as np
from contextlib import ExitStack
import concourse.bass as bass
import concourse.mybir as mybir
from concourse.bass_utils import run_bass_kernel_spmd

F32 = mybir.dt.float32
BF16 = mybir.dt.bfloat16
I32 = mybir.dt.int32
AF = mybir.ActivationFunctionType
ALU = mybir.AluOpType
AX = mybir.AxisListType

ENGS = ("pe", "act", "dve", "pool", "sp")
NPOOL = 6


class Op:
    __slots__ = ("eng", "fn", "idx", "deps", "dma", "dsem", "dval", "gidx")


class Prog:
    def __init__(self):
        self.nc = bass.Bass("TRN2", target_bir_lowering=False)
        self.stack = ExitStack()
        self.ops = {e: [] for e in ENGS}
        self.lastw = {}
        self.readers = {}
        self.n = 0
        self.out_dmas = []
        self.dma_count = {e: 0 for e in ENGS}
        self.same_engine_sync = True
        self._uid = 0

    def dram(self, name, shape, dt, kind="ExternalInput"):
        return self.nc.dram_tensor(name, list(shape), dt, kind=kind).ap()

    def sb(self, shape, dt, name=None):
        self._uid += 1
        name = name or f"sb{self._uid}"
        return self.stack.enter_context(self.nc.sbuf_tensor(name, list(shape), dt))

    def ps(self, shape, dt=F32, name=None):
        self._uid += 1
        name = name or f"ps{self._uid}"
        return self.stack.enter_context(self.nc.psum_tensor(name, list(shape), dt))

    def op(self, eng, fn, r=(), w=(), dma=False, out=False):
        o = Op()
        o.eng = eng
        o.fn = fn
        o.dma = dma
        o.idx = len(self.ops[eng])
        o.gidx = self.n
        self.n += 1
        deps = set()
        for k in r:
            lw = self.lastw.get(k)
            if lw is not None:
                deps.add(lw)
        for k in w:
            lw = self.lastw.get(k)
            if lw is not None:
                deps.add(lw)
            for rd in self.readers.get(k, ()):
                deps.add(rd)
        for k in r:
            self.readers.setdefault(k, []).append(o)
        for k in w:
            self.lastw[k] = o
            self.readers[k] = []
        deps.discard(o)
        o.deps = deps
        self.ops[eng].append(o)
        if out:
            self.out_dmas.append(o)
        return o

    def dma(self, eng, out, in_, r=(), w=(), is_out=False, **kw):
        return self.op(eng, lambda e: e.dma_start(out=out, in_=in_, **kw), r, w, dma=True, out=is_out)

    def mm(self, out, lhsT, rhs, start=True, stop=True, r=(), w=()):
        return self.op("pe", lambda e: e.matmul(out, lhsT, rhs, start=start, stop=stop), r, w)

    def tr(self, out, in_, ident, r=(), w=()):
        return self.op("pe", lambda e: e.transpose(out, in_, ident), r, w)

    def act(self, out, in_, func, r=(), w=(), eng="act", **kw):
        return self.op(eng, lambda e: e.activation(out=out, in_=in_, func=func, **kw), r, w)

    def tt(self, eng, out, in0, in1, op, r=(), w=()):
        return self.op(eng, lambda e: e.tensor_tensor(out=out, in0=in0, in1=in1, op=op), r, w)

    def ts(self, eng, out, in0, s1, s2, op0, op1=None, r=(), w=(), **kw):
        if op1 is None:
            return self.op(eng, lambda e: e.tensor_scalar(out=out, in0=in0, scalar1=s1, scalar2=None, op0=op0, **kw), r, w)
        return self.op(eng, lambda e: e.tensor_scalar(out=out, in0=in0, scalar1=s1, scalar2=s2, op0=op0, op1=op1, **kw), r, w)

    def stt(self, eng, out, in0, scalar, in1, op0, op1, r=(), w=(), **kw):
        return self.op(eng, lambda e: e.scalar_tensor_tensor(out=out, in0=in0, scalar=scalar, in1=in1, op0=op0, op1=op1, **kw), r, w)

    def copy(self, eng, out, in_, r=(), w=()):
        if eng == "act":
            return self.op(eng, lambda e: e.copy(out=out, in_=in_), r, w)
        return self.op(eng, lambda e: e.tensor_copy(out=out, in_=in_), r, w)

    def memset(self, eng, ap, val, w=()):
        return self.op(eng, lambda e: e.memset(ap, val), (), w)

    def build(self):
        nc = self.nc
        st = self.stack
        csem = {e: st.enter_context(nc.semaphore(f"c_{e}")) for e in ENGS}
        dsem = {e: [st.enter_context(nc.semaphore(f"d_{e}{i}")) for i in range(NPOOL)] for e in ("sp", "act", "pool")}
        duse = {e: [0] * NPOOL for e in dsem}
        for e in dsem:
            k = 0
            for o in self.ops[e]:
                if o.dma:
                    i = k % NPOOL
                    duse[e][i] += 1
                    o.dsem = (e, i)
                    o.dval = 16 * duse[e][i]
                    k += 1
        ccount = {}
        for e in ENGS:
            c = 0
            for o in self.ops[e]:
                if not o.dma:
                    c += 1
                    o.dval = c
                    o.dsem = None
        out_dmas = self.out_dmas
        prog = self

        def emit(ename, eng):
            seen = {}

            def wait(key, sem, val):
                if seen.get(key, 0) >= val:
                    return
                eng.wait_ge(sem, val)
                seen[key] = val

            for o in prog.ops[ename]:
                for d in sorted(o.deps, key=lambda x: x.gidx):
                    if d.dma:
                        wait(("d",) + d.dsem, dsem[d.dsem[0]][d.dsem[1]], d.dval)
                    else:
                        if d.eng == ename and not prog.same_engine_sync:
                            continue
                        if d.eng == ename and ename == "pe":
                            continue
                        wait(("c", d.eng), csem[d.eng], d.dval)
                if o.dma:
                    if o.dval > 16:
                        wait(("d",) + o.dsem, dsem[o.dsem[0]][o.dsem[1]], o.dval - 16)
                    ins = o.fn(eng)
                    ins.then_inc(dsem[o.dsem[0]][o.dsem[1]], 16)
                else:
                    ins = o.fn(eng)
                    ins.then_inc(csem[ename], 1)
            if ename == "sp":
                for d in out_dmas:
                    wait(("d",) + d.dsem, dsem[d.dsem[0]][d.dsem[1]], d.dval)

        with nc.Block() as block:
            @block.sync
            def _(e):
                emit("sp", e)

            @block.scalar
            def _(e):
                emit("act", e)

            @block.vector
            def _(e):
                emit("dve", e)

            @block.gpsimd
            def _(e):
                emit("pool", e)

            @block.tensor
            def _(e):
                emit("pe", e)
        self.stack.close()
        return nc


def run(nc, in_maps, trace=False):
    res = run_bass_kernel_spmd(nc, in_maps, core_ids=list(range(len(in_maps))), trace=trace)
    return res


D = 1024
S = 8192
NB = 2
TOK = 2048
NT = TOK // 128
NPBF = ml_dtypes.bfloat16
_cache = {}


def _ident_np():
    return np.eye(128, dtype=np.float32)


def rmsnorm_tile(P, xb, xkey, gb, h, hkey, scr, eps=1e-6, d=1024):
    sq, ss, rstd = scr["sq"], scr["ss"], scr["rstd"]
    P.act(sq[:, :d], xb, AF.Square, r=[xkey], w=["sq", "ss"], accum_out=ss[:])
    P.ts("dve", rstd[:], ss[:], 1.0 / d, eps, ALU.mult, ALU.add, r=["ss"], w=["rstd"])
    P.act(rstd[:], rstd[:], AF.Sqrt, r=["rstd"], w=["rstd"])
    P.op("dve", lambda e: e.reciprocal(out=rstd[:], in_=rstd[:]), ["rstd"], ["rstd"])
    P.stt("dve", h, xb, rstd[:], gb, ALU.mult, ALU.mult, r=[xkey, "rstd", "gb"], w=[hkey])


def norm_scratch(P):
    return {"sq": P.sb([128, 1024], F32), "ss": P.sb([128, 1], F32), "rstd": P.sb([128, 1], F32)}


def build_A1(var=0):
    P = Prog()
    x = P.dram("x", [TOK, D], F32)
    g = P.dram("g", [1, D], F32)
    wqk_d = P.dram("wqk", [D, 1664], F32)
    wsw_d = P.dram("wsw", [D, 1664], F32)
    wv_d = P.dram("wv", [D, 640], F32)
    cos_d = P.dram("cos", [128, TOK], F32)
    sin_d = P.dram("sin", [128, TOK], F32)
    ident_d = P.dram("ident", [128, 128], F32)
    qk_o = P.dram("qk_o", [13, 128, TOK], BF16, kind="ExternalOutput")
    v_o = P.dram("v_o", [TOK, 640], BF16, kind="ExternalOutput")

    gb = P.sb([128, D], F32)
    wqk = P.sb([128, 8, 1664], BF16)
    wsw = P.sb([128, 8, 1664], BF16)
    wv = P.sb([128, 8, 640], BF16)
    cos = P.sb([128, TOK], F32)
    sin = P.sb([128, TOK], F32)
    identf = P.sb([128, 128], F32)
    ident = P.sb([128, 128], BF16)
    scr = norm_scratch(P)
    xt = [P.sb([128, D], F32) for _ in range(2)]
    h = P.sb([128, D], BF16)
    hT4 = [P.sb([128, 8, 512], BF16) for _ in range(2)]
    ptr = [P.ps([128, 8, 128], BF16) for _ in range(2)]
    ps1 = [P.ps([128, 512], F32) for _ in range(2)]
    ps2 = [P.ps([128, 512], F32) for _ in range(2)]
    psv = P.ps([128, 512], F32)
    psv2 = P.ps([128, 128], F32)
    t1 = [P.sb([128, 512], F32) for _ in range(2)]
    t2 = [P.sb([128, 512], F32) for _ in range(2)]
    ro = [P.sb([128, 512], BF16) for _ in range(2)]
    vo = [P.sb([128, 640], BF16) for _ in range(2)]

    P.dma("sp", gb[:], g.partition_broadcast(128), w=["gb"])
    P.dma("sp", identf[:], ident_d, w=["identf"])
    P.copy("dve", ident[:], identf[:], r=["identf"], w=["ident"])
    for k in range(8):
        P.dma("pool", wqk[:, k, :], wqk_d[k * 128:(k + 1) * 128, :], w=[("wqk", k)])
        P.dma("pool", wsw[:, k, :], wsw_d[k * 128:(k + 1) * 128, :], w=[("wsw", k)])
        P.dma("pool", wv[:, k, :], wv_d[k * 128:(k + 1) * 128, :], w=[("wv", k)])
    P.dma("sp", cos[:], cos_d, w=["cos"])
    P.dma("sp", sin[:], sin_d, w=["sin"])
    nblk = 0
    for grp in range(NT // 4):
        hb = hT4[grp % 2]
        hk = ("hT4", grp % 2)
        for j in range(4):
            t = grp * 4 + j
            xb = xt[t % 2]
            P.dma("sp", xb[:], x[t * 128:(t + 1) * 128, :], w=[("x", t % 2)])
            rmsnorm_tile(P, xb[:], ("x", t % 2), gb[:], h[:], "h", scr)
            pt = ptr[t % 2]
            for k in range(8):
                P.tr(pt[:, k, :], h[:, k * 128:(k + 1) * 128], ident[:], r=["h", "ident"], w=[("ptr", t % 2)])
            P.copy("act", hb[:, :, j * 128:(j + 1) * 128], pt[:], r=[("ptr", t % 2)], w=[hk + (j,)])
        hkeys = [hk + (j,) for j in range(4)]
        for cb in range(13):
            i = nblk % 2
            nblk += 1
            for k in range(8):
                P.mm(ps1[i][:], wqk[:, k, cb * 128:(cb + 1) * 128], hb[:, k, :], start=(k == 0), stop=(k == 7),
                     r=hkeys + [("wqk", k)], w=[("ps1", i)])
            for k in range(8):
                P.mm(ps2[i][:], wsw[:, k, cb * 128:(cb + 1) * 128], hb[:, k, :], start=(k == 0), stop=(k == 7),
                     r=hkeys + [("wsw", k)], w=[("ps2", i)])
            cs = slice(grp * 512, (grp + 1) * 512)
            P.tt("dve", t1[i][:], ps1[i][:], cos[:, cs], ALU.mult, r=[("ps1", i), "cos"], w=[("t1", i)])
            P.tt("dve", t2[i][:], ps2[i][:], sin[:, cs], ALU.mult, r=[("ps2", i), "sin"], w=[("t2", i)])
            P.tt("dve" if var == 1 else "pool", ro[i][:], t1[i][:], t2[i][:], ALU.add, r=[("t1", i), ("t2", i)], w=[("ro", i)])
            P.dma("sp", qk_o[cb, :, cs], ro[i][:], r=[("ro", i)], w=[("qk_o", cb, grp)], is_out=True)
        for j in range(4):
            t = grp * 4 + j
            i = t % 2
            for k in range(8):
                P.mm(psv[:], hb[:, k, j * 128:(j + 1) * 128], wv[:, k, 0:512], start=(k == 0), stop=(k == 7),
                     r=hkeys + [("wv", k)], w=["psv"])
            for k in range(8):
                P.mm(psv2[:], hb[:, k, j * 128:(j + 1) * 128], wv[:, k, 512:640], start=(k == 0), stop=(k == 7),
                     r=hkeys + [("wv", k)], w=["psv2"])
            P.copy("act", vo[i][:, 0:512], psv[:], r=["psv"], w=[("vo", i, 0)])
            P.copy("act", vo[i][:, 512:640], psv2[:], r=["psv2"], w=[("vo", i, 1)])
            P.dma("sp", v_o[t * 128:(t + 1) * 128, :], vo[i][:], r=[("vo", i, 0), ("vo", i, 1)], w=[("v_o", t)], is_out=True)
    return P.build()


def rope_tables_np(pos):
    inv = (1.0 / (np.float32(10000.0) ** (np.arange(0, 64, 2, dtype=np.float32) / np.float32(64)))).astype(np.float32)
    ang = pos.astype(np.float32)[:, None] * inv[None, :]
    ang = np.concatenate([ang, ang], axis=-1)
    c = np.cos(ang).astype(np.float32)
    s = np.sin(ang).astype(np.float32)
    s[:, :32] *= -1.0
    cT = np.concatenate([c.T, c.T], axis=0)
    sT = np.concatenate([s.T, s.T], axis=0)
    return np.ascontiguousarray(cT), np.ascontiguousarray(sT)


QK_COLS = np.concatenate([np.arange(0, 512), np.arange(512, 1024), np.arange(1536, 2048), np.arange(2048, 2176)])
V_COLS = np.concatenate([np.arange(1024, 1536), np.arange(2176, 2304)])


def _swap_halves_cols(n):
    idx = np.arange(n)
    hd = idx // 64
    d = idx % 64
    return hd * 64 + (d + 32) % 64


def run_A1(x, e_norm, e_w_in):
    if "A1" not in _cache:
        _cache["A1"] = build_A1()
    nc = _cache["A1"]
    wqk = np.ascontiguousarray(e_w_in[:, QK_COLS])
    wsw = np.ascontiguousarray(wqk[:, _swap_halves_cols(1664)])
    wv = np.ascontiguousarray(e_w_in[:, V_COLS])
    maps = []
    for c in range(8):
        b, q = c // 4, c % 4
        cT, sT = rope_tables_np(np.arange(q * TOK, (q + 1) * TOK))
        maps.append({"x": np.ascontiguousarray(x[b, q * TOK:(q + 1) * TOK]), "g": e_norm.reshape(1, D), "wqk": wqk, "wsw": wsw,
                     "wv": wv, "cos": cT, "sin": sT, "ident": _ident_np()})
    res = run(nc, maps)
    qk = [res.results[c]["qk_o"] for c in range(8)]
    v = [res.results[c]["v_o"] for c in range(8)]
    return qk, v


NKA = 32
NKB = 18


def build_A2(var=0):
    P = Prog()
    ACTQ = "sp" if var & 1 else "act"
    POOLE = "dve" if var & 4 else "pool"
    qa_d = P.dram("qa", [4, 128, TOK], BF16)
    qb_d = P.dram("qb", [4, 128, TOK], BF16)
    ka_d = P.dram("ka", [4, 128, NKA * 128], BF16)
    kb_d = P.dram("kb", [2, 128, NKB * 128], BF16)
    va_d = P.dram("va", [NKA * 128, 8 * 65], BF16)
    vb_d = P.dram("vb", [NKB * 128, 2 * 65], BF16)
    kva_d = P.dram("kva", [128, NKA], F32)
    kvb_d = P.dram("kvb", [128, NKB], F32)
    ma_d = P.dram("ma", [128, 17, 128], BF16)
    mb_d = P.dram("mb", [128, 3, 128], BF16)
    x_d = P.dram("x", [TOK, D], F32)
    wo_d = P.dram("wo", [D, D], F32)
    sink_d = P.dram("sink", [1, 8], F32)
    ident_d = P.dram("ident", [128, 128], F32)
    x1_o = P.dram("x1_o", [TOK, D], F32, kind="ExternalOutput")

    qa = P.sb([128, 4, TOK], BF16)
    qb = P.sb([128, 4, TOK], BF16)
    ka = P.sb([128, 4, NKA * 128], BF16)
    kb = P.sb([128, 2, NKB * 128], BF16)
    va = P.sb([128, NKA, 8 * 65], BF16)
    vb = P.sb([128, NKB, 2 * 65], BF16)
    kva = P.sb([128, NKA], F32)
    kvb = P.sb([128, NKB], F32)
    ma = P.sb([128, 17, 128], BF16)
    mb = P.sb([128, 3, 128], BF16)
    wo = P.sb([128, 8, D], BF16)
    sink = P.sb([128, 8], F32)
    esink = P.sb([128, 8], F32)
    identf = P.sb([128, 128], F32)
    ident = P.sb([128, 128], BF16)
    xt = [P.sb([128, D], F32) for _ in range(2)]
    y = P.sb([128, D], BF16)
    yT = P.sb([128, 8, 128], BF16)
    pT = [P.sb([128, 4, 128], BF16) for _ in range(3)]
    pm = [P.sb([128, 4, 128], BF16) for _ in range(3)]
    den = P.sb([128, 4], F32)
    rden = P.sb([128, 4], F32)
    xo = [P.sb([128, D], F32) for _ in range(2)]
    ps_s = [P.ps([128, 4, 128], F32) for _ in range(2)]
    ps_o = [P.ps([128, 512], F32) for _ in range(4)]
    ptr = P.ps([128, 8, 128], BF16)
    ps_out1 = P.ps([128, 512], F32)
    ps_out = [ps_out1, ps_out1]

    P.dma("sp", identf[:], ident_d, w=["identf"])
    P.copy("dve", ident[:], identf[:], r=["identf"], w=["ident"])
    P.dma("sp", sink[:], sink_d.partition_broadcast(128), w=["sink"])
    P.act(esink[:], sink[:], AF.Exp, r=["sink"], w=["esink"])
    P.dma("sp", kva[:], kva_d, w=["kva"])
    P.dma("sp", kvb[:], kvb_d, w=["kvb"])
    P.dma("sp", ma[:], ma_d, w=["ma"])
    P.dma("sp", mb[:], mb_d, w=["mb"])
    for i in range(4):
        P.dma("sp", qa[:, i, :], qa_d[i], w=[("qa", i)])
        P.dma(ACTQ, qb[:, i, :], qb_d[i], w=[("qb", i)])
        if var & 2:
            for j in range(4):
                P.dma("sp", ka[:, i, j * 1024:(j + 1) * 1024], ka_d[i, :, j * 1024:(j + 1) * 1024], w=[("ka", i)])
        else:
            P.dma("sp", ka[:, i, :], ka_d[i], w=[("ka", i)])
    for i in range(2):
        P.dma(ACTQ, kb[:, i, :], kb_d[i], w=[("kb", i)])
    for kt in range(NKA):
        P.dma("sp" if kt % 2 else ACTQ, va[:, kt, :], va_d[kt * 128:(kt + 1) * 128, :], w=[("va", kt)])
    for kt in range(NKB):
        P.dma("sp" if kt % 2 else ACTQ, vb[:, kt, :], vb_d[kt * 128:(kt + 1) * 128, :], w=[("vb", kt)])
    for k in range(8):
        P.dma("pool", wo[:, k, :], wo_d[k * 128:(k + 1) * 128, :], w=[("wo", k)])

    cnt = 0
    for qt in range(NT):
        qs = slice(qt * 128, (qt + 1) * 128)
        xb = xt[qt % 2]
        P.dma("sp", xb[:], x_d[qs, :], w=[("x", qt % 2)])
        its = []
        for part in ("A", "B"):
            deltas = list(range(-8, 9)) if part == "A" else list(range(-1, 2))
            for g in range(2):
                for di, dl in enumerate(deltas):
                    its.append((part, g, di, len(deltas), dl))

        def emit_qk(it, idx):
            part, g, di, nd, dl = it
            i2 = idx % 2
            i3 = idx % 3
            kt = qt + 8 + dl if part == "A" else qt + 1 + dl
            ks = slice(kt * 128, (kt + 1) * 128)
            for hh in range(4):
                hd = 2 * hh + g
                pr, pb = hh, g * 64
                if part == "A":
                    P.mm(ps_s[i2][:, hh, :], ka[pb:pb + 64, pr, ks], qa[pb:pb + 64, pr, qs],
                         r=[("ka", pr), ("qa", pr)], w=[("ps_s", i2)])
                else:
                    P.mm(ps_s[i2][:, hh, :], kb[pb:pb + 64, hd // 4, ks], qb[pb:pb + 64, pr, qs],
                         r=[("kb", hd // 4), ("qb", pr)], w=[("ps_s", i2)])
            P.act(pT[i3][:], ps_s[i2][:], AF.Exp, r=[("ps_s", i2)], w=[("pT", i3)], scale=0.125)
            if part == "A":
                msk = ma[:, dl + 8, :].unsqueeze(1).broadcast_to([128, 4, 128])
                mkeys = ["ma"]
            else:
                msk = mb[:, dl + 1, :].unsqueeze(1).broadcast_to([128, 4, 128])
                mkeys = ["mb"]
            P.tt("dve" if idx % 2 else POOLE, pm[i3][:], pT[i3][:], msk, ALU.mult,
                 r=[("pT", i3)] + mkeys, w=[("pm", i3)])

        def emit_pv(it, idx):
            part, g, di, nd, dl = it
            i3 = idx % 3
            kt = qt + 8 + dl if part == "A" else qt + 1 + dl
            for hh in range(4):
                hd = 2 * hh + g
                if part == "A":
                    rhs = va[:, kt, hd * 65:(hd + 1) * 65]
                    rk = ("va", kt)
                else:
                    rhs = vb[:, kt, (hd // 4) * 65:(hd // 4 + 1) * 65]
                    rk = ("vb", kt)
                P.mm(ps_o[hh][:, 0:65], pm[i3][:, hh, :], rhs, start=(di == 0), stop=(di == nd - 1),
                     r=[("pm", i3), rk], w=[("ps_o", hh)])
            if di == nd - 1:
                base = (0 if part == "A" else 512)
                for hh in range(4):
                    hd = 2 * hh + g
                    pok = ("ps_o", hh)
                    if part == "A":
                        P.copy("dve", den[:, hh:hh + 1], ps_o[hh][:, 64:65], r=[pok], w=[("den", hh)])
                    else:
                        P.tt("dve", den[:, hh:hh + 1], ps_o[hh][:, 64:65], esink[:, hd:hd + 1], ALU.add,
                             r=[pok, "esink"], w=[("den", hh)])
                    P.op("dve", lambda e, hh=hh: e.reciprocal(out=rden[:, hh:hh + 1], in_=den[:, hh:hh + 1]), [("den", hh)], [("rden", hh)])
                    P.ts("dve", y[:, base + hd * 64:base + (hd + 1) * 64], ps_o[hh][:, 0:64], rden[:, hh:hh + 1], None, ALU.mult,
                         r=[pok, ("rden", hh)], w=[("y", part, g, hh)])

        emit_qk(its[0], cnt)
        for n_, it in enumerate(its):
            if n_ + 1 < len(its):
                emit_qk(its[n_ + 1], cnt + n_ + 1)
            emit_pv(it, cnt + n_)
        cnt += len(its)
        ykeys = [("y", p_, g_, h_) for p_ in "AB" for g_ in range(2) for h_ in range(4)]
        for k in range(8):
            P.tr(ptr[:, k, :], y[:, k * 128:(k + 1) * 128], ident[:], r=ykeys + ["ident"], w=["ptr"])
        P.copy("act", yT[:], ptr[:], r=["ptr"], w=["yT"])
        for hf in range(2):
            for k in range(8):
                P.mm(ps_out[hf][:], yT[:, k, :], wo[:, k, hf * 512:(hf + 1) * 512], start=(k == 0), stop=(k == 7),
                     r=["yT", ("wo", k)], w=[("ps_out", 0)])
            P.tt("dve", xo[qt % 2][:, hf * 512:(hf + 1) * 512], ps_out[hf][:], xb[:, hf * 512:(hf + 1) * 512], ALU.add,
                 r=[("ps_out", 0), ("x", qt % 2)], w=[("xo", qt % 2, hf)])
        P.dma("sp", x1_o[qs, :], xo[qt % 2][:], r=[("xo", qt % 2, 0), ("xo", qt % 2, 1)], w=[("x1_o", qt)], is_out=True)
    return P.build()


def _masks_np():
    ql = np.arange(128)[None, :]
    kl = np.arange(128)[:, None]
    ma = np.zeros((128, 17, 128), np.float32)
    for dl in range(-8, 9):
        diff = ql - kl - 128 * dl
        ad = np.abs(diff)
        ma[:, dl + 8, :] = (ad <= 64).astype(np.float32) + ((ad <= 256) & (diff % 4 == 0)) + ((ad <= 1024) & (diff % 16 == 0))
    mb = np.zeros((128, 3, 128), np.float32)
    for dl in range(-1, 2):
        diff = ql - kl - 128 * dl
        mb[:, dl + 1, :] = (np.abs(diff) <= 128)
    return ma.astype(NPBF), mb.astype(NPBF)


def _halo(arr_list, b, q, axis, lo, hi, zero_shape_fn):
    full = np.concatenate([arr_list[b * 4 + i] for i in range(4)], axis=axis)
    padw = [(0, 0)] * full.ndim
    padw[axis] = (lo, hi)
    full = np.pad(full, padw)
    sl = [slice(None)] * full.ndim
    sl[axis] = slice(q * TOK, (q + 1) * TOK + lo + hi)
    return np.ascontiguousarray(full[tuple(sl)])


def run_A2(qk, v, x, e_sink, e_w_out, var=0):
    if ("A2", var) not in _cache:
        _cache[("A2", var)] = build_A2(var)
    nc = _cache[("A2", var)]
    ma, mb = _masks_np()
    ones = np.ones((TOK, 1), NPBF)
    vaug_a, vaug_b, ka_l, kb_l = [], [], [], []
    for c in range(8):
        vv = np.asarray(v[c])
        va = np.concatenate([np.concatenate([vv[:, h * 64:(h + 1) * 64], ones], 1) for h in range(8)], 1)
        vb = np.concatenate([np.concatenate([vv[:, 512 + h * 64:512 + (h + 1) * 64], ones], 1) for h in range(2)], 1)
        vaug_a.append(va)
        vaug_b.append(vb)
        qq = np.asarray(qk[c])
        ka_l.append(qq[4:8])
        kbt = qq[12]
        kb_l.append(np.stack([np.concatenate([kbt[0:64], kbt[0:64]], 0), np.concatenate([kbt[64:128], kbt[64:128]], 0)], 0))
    maps = []
    for c in range(8):
        b, q = c // 4, c % 4
        qq = np.asarray(qk[c])
        pos_a = (q * TOK - 1024) + np.arange(NKA) * 128
        pos_b = (q * TOK - 128) + np.arange(NKB) * 128
        kva = np.broadcast_to(((pos_a >= 0) & (pos_a < S)).astype(np.float32)[None, :], (128, NKA))
        kvb = np.broadcast_to(((pos_b >= 0) & (pos_b < S)).astype(np.float32)[None, :], (128, NKB))
        maps.append({
            "qa": np.ascontiguousarray(qq[0:4]), "qb": np.ascontiguousarray(qq[8:12]),
            "ka": _halo(ka_l, b, q, 2, 1024, 1024, None), "kb": _halo(kb_l, b, q, 2, 128, 128, None),
            "va": _halo(vaug_a, b, q, 0, 1024, 1024, None), "vb": _halo(vaug_b, b, q, 0, 128, 128, None),
            "kva": np.ascontiguousarray(kva), "kvb": np.ascontiguousarray(kvb), "ma": ma, "mb": mb,
            "x": np.ascontiguousarray(x[b, q * TOK:(q + 1) * TOK]), "wo": e_w_out, "sink": e_sink.reshape(1, 8),
            "ident": _ident_np()})
    res = run(nc, maps)
    return [res.results[c]["x1_o"] for c in range(8)]


def build_A3():
    P = Prog()
    x_d = P.dram("x", [TOK, D], F32)
    mem_d = P.dram("mem", [256, D], F32)
    xn_d = P.dram("xn", [1, D], F32)
    mn_d = P.dram("mn", [1, D], F32)
    fn_d = P.dram("fn", [1, D], F32)
    wq_d = P.dram("wq", [D, D], F32)
    wkv_d = P.dram("wkv", [D, 2 * D], F32)
    wo_d = P.dram("wo", [D, D], F32)
    wr_d = P.dram("wr", [D, 16], F32)
    ident_d = P.dram("ident", [128, 128], F32)
    x2_o = P.dram("x2_o", [TOK, D], F32, kind="ExternalOutput")
    hf_o = P.dram("hf_o", [TOK, D], BF16, kind="ExternalOutput")
    aff_o = P.dram("aff_o", [TOK, 16], F32, kind="ExternalOutput")

    gx = P.sb([128, D], F32)
    gm = P.sb([128, D], F32)
    gf = P.sb([128, D], F32)
    wq = P.sb([128, 8, D], BF16)
    wkv = P.sb([128, 8, 2 * D], BF16)
    wo = P.sb([128, 8, D], BF16)
    wr = P.sb([128, 8, 16], F32)
    identf = P.sb([128, 128], F32)
    ident = P.sb([128, 128], BF16)
    scr = norm_scratch(P)
    mt_ = P.sb([128, D], F32)
    h = P.sb([128, D], BF16)
    memT = P.sb([128, 8, 256], BF16)
    KT = P.sb([128, 8, 256], BF16)
    Vaug = P.sb([128, 2, 4 * 257], BF16)
    xt4 = P.sb([128, 4, D], F32)
    hT4 = P.sb([128, 8, 512], BF16)
    QT = P.sb([128, 8, 512], BF16)
    pT = [P.sb([128, 4, 128], BF16) for _ in range(2)]
    o = P.sb([128, D], BF16)
    oT = P.sb([128, 8, 128], BF16)
    x2 = [P.sb([128, D], F32) for _ in range(2)]
    hf = P.sb([128, D], F32)
    hfb = [P.sb([128, D], BF16) for _ in range(2)]
    hfT = P.sb([128, 8, 128], F32)
    den = P.sb([128, 4], F32)
    rden = P.sb([128, 4], F32)
    lg = P.sb([128, 16], F32)
    mx = P.sb([128, 1], F32)
    ex = P.sb([128, 16], F32)
    sm = P.sb([128, 1], F32)
    aff = [P.sb([128, 16], F32) for _ in range(2)]
    ps_s = [P.ps([128, 4, 128], F32) for _ in range(2)]
    ps_o = [P.ps([128, 512], F32) for _ in range(4)]
    ptr = P.ps([128, 8, 128], BF16)
    psA = P.ps([128, 512], F32)

    P.dma("sp", identf[:], ident_d, w=["identf"])
    P.copy("dve", ident[:], identf[:], r=["identf"], w=["ident"])
    P.dma("sp", gx[:], xn_d.partition_broadcast(128), w=["gx"])
    P.dma("sp", gm[:], mn_d.partition_broadcast(128), w=["gm"])
    P.dma("sp", gf[:], fn_d.partition_broadcast(128), w=["gf"])
    P.dma("sp", wr[:], wr_d.rearrange("(k p) n -> p k n", p=128), w=["wr"])
    for k in range(8):
        P.dma("pool", wkv[:, k, 0:1024], wkv_d[k * 128:(k + 1) * 128, 0:1024], w=[("wkv", k)])
        P.dma("pool", wkv[:, k, 1024:2048], wkv_d[k * 128:(k + 1) * 128, 1024:2048], w=[("wkv", k)])
    for k in range(8):
        P.dma("pool", wq[:, k, :], wq_d[k * 128:(k + 1) * 128, :], w=[("wq", k)])
    for k in range(8):
        P.dma("pool", wo[:, k, :], wo_d[k * 128:(k + 1) * 128, :], w=[("wo", k)])
    wkvk = [("wkv", k) for k in range(8)]
    P.memset("dve", Vaug[:], 1.0, w=["Vaug"])
    for m in range(2):
        P.dma("sp", mt_[:], mem_d[m * 128:(m + 1) * 128, :], w=["mt"])
        rmsnorm_tile(P, mt_[:], "mt", gm[:], h[:], "h", scr)
        for k in range(8):
            P.tr(ptr[:, k, :], h[:, k * 128:(k + 1) * 128], ident[:], r=["h", "ident"], w=["ptr"])
        P.copy("act", memT[:, :, m * 128:(m + 1) * 128], ptr[:], r=["ptr"], w=[("memT", m)])
    mk = [("memT", 0), ("memT", 1)]
    for c in range(8):
        for k in range(8):
            P.mm(psA[:, 0:256], wkv[:, k, c * 128:(c + 1) * 128], memT[:, k, :], start=(k == 0), stop=(k == 7),
                 r=mk + wkvk, w=["psA"])
        P.copy("act", KT[:, c, :], psA[:, 0:256], r=["psA"], w=["KT"])
    for m in range(2):
        for hf_ in range(2):
            for k in range(8):
                P.mm(psA[:], memT[:, k, m * 128:(m + 1) * 128], wkv[:, k, 1024 + hf_ * 512:1024 + (hf_ + 1) * 512],
                     start=(k == 0), stop=(k == 7), r=mk + wkvk, w=["psA"])
            for hh in range(2):
                hd = hf_ * 2 + hh
                P.copy("act", Vaug[:, m, hd * 257:hd * 257 + 256], psA[:, hh * 256:(hh + 1) * 256], r=["psA"], w=["Vaug"])
    wqk = [("wq", k) for k in range(8)]
    wok = [("wo", k) for k in range(8)]
    for grp in range(NT // 4):
        for j in range(4):
            t = grp * 4 + j
            P.dma("sp", xt4[:, j, :], x_d[t * 128:(t + 1) * 128, :], w=[("x", j)])
            rmsnorm_tile(P, xt4[:, j, :], ("x", j), gx[:], h[:], "h", scr)
            for k in range(8):
                P.tr(ptr[:, k, :], h[:, k * 128:(k + 1) * 128], ident[:], r=["h", "ident"], w=["ptr"])
            P.copy("act", hT4[:, :, j * 128:(j + 1) * 128], ptr[:], r=["ptr"], w=[("hT4", j)])
        hk = [("hT4", j) for j in range(4)]
        for c in range(8):
            for k in range(8):
                P.mm(psA[:], wq[:, k, c * 128:(c + 1) * 128], hT4[:, k, :], start=(k == 0), stop=(k == 7), r=hk + wqk, w=["psA"])
            P.copy("dve" if c % 2 else "act", QT[:, c, :], psA[:], r=["psA"], w=[("QT", c)])
        qk_ = [("QT", c) for c in range(8)]
        for j in range(4):
            t = grp * 4 + j
            js = slice(j * 128, (j + 1) * 128)
            for m in range(2):
                for hd in range(4):
                    for half in range(2):
                        P.mm(ps_s[m][:, hd, :], KT[:, 2 * hd + half, m * 128:(m + 1) * 128], QT[:, 2 * hd + half, js],
                             start=(half == 0), stop=(half == 1), r=["KT"] + qk_, w=[("ps_s", m)])
                P.act(pT[m][:], ps_s[m][:], AF.Exp, r=[("ps_s", m)], w=[("pT", m)], scale=1.0 / 16)
            for hd in range(4):
                for m in range(2):
                    P.mm(ps_o[hd][:, 0:257], pT[m][:, hd, :], Vaug[:, m, hd * 257:(hd + 1) * 257], start=(m == 0), stop=(m == 1),
                         r=[("pT", m), "Vaug"], w=[("ps_o", hd)])
                P.op("dve", lambda e, hd=hd: e.reciprocal(out=rden[:, hd:hd + 1], in_=ps_o[hd][:, 256:257]), [("ps_o", hd)], [("rden", hd)])
                P.ts("dve", o[:, hd * 256:(hd + 1) * 256], ps_o[hd][:, 0:256], rden[:, hd:hd + 1], None, ALU.mult,
                     r=[("ps_o", hd), ("rden", hd)], w=[("o", hd)])
            ok_ = [("o", hd) for hd in range(4)]
            for k in range(8):
                P.tr(ptr[:, k, :], o[:, k * 128:(k + 1) * 128], ident[:], r=ok_ + ["ident"], w=["ptr"])
            P.copy("act", oT[:], ptr[:], r=["ptr"], w=["oT"])
            xb = x2[t % 2]
            for hf_ in range(2):
                for k in range(8):
                    P.mm(psA[:], oT[:, k, :], wo[:, k, hf_ * 512:(hf_ + 1) * 512], start=(k == 0), stop=(k == 7), r=["oT"] + wok, w=["psA"])
                P.tt("dve", xb[:, hf_ * 512:(hf_ + 1) * 512], psA[:], xt4[:, j, hf_ * 512:(hf_ + 1) * 512], ALU.add,
                     r=["psA", ("x", j)], w=[("x2", t % 2, hf_)])
            x2k = [("x2", t % 2, 0), ("x2", t % 2, 1)]
            P.dma("sp", x2_o[t * 128:(t + 1) * 128, :], xb[:], r=x2k, w=[("x2_o", t)], is_out=True)
            sq, ss, rstd = scr["sq"], scr["ss"], scr["rstd"]
            P.act(sq[:], xb[:], AF.Square, r=x2k, w=["sq", "ss"], accum_out=ss[:])
            P.ts("dve", rstd[:], ss[:], 1.0 / D, 1e-6, ALU.mult, ALU.add, r=["ss"], w=["rstd"])
            P.act(rstd[:], rstd[:], AF.Sqrt, r=["rstd"], w=["rstd"])
            P.op("dve", lambda e: e.reciprocal(out=rstd[:], in_=rstd[:]), ["rstd"], ["rstd"])
            P.stt("dve", hf[:], xb[:], rstd[:], gf[:], ALU.mult, ALU.mult, r=x2k + ["rstd", "gf"], w=["hf"])
            P.copy("pool", hfb[t % 2][:], hf[:], r=["hf"], w=[("hfb", t % 2)])
            P.dma("sp", hf_o[t * 128:(t + 1) * 128, :], hfb[t % 2][:], r=[("hfb", t % 2)], w=[("hf_o", t)], is_out=True)
            psAv = psA[:].rearrange("p (a b) -> p a b", b=128)
            for hf_ in range(2):
                for k in range(4):
                    kk = hf_ * 4 + k
                    P.tr(psAv[:, k, :], hf[:, kk * 128:(kk + 1) * 128], identf[:], r=["hf", "identf"], w=["psA"])
                P.copy("act", hfT[:, hf_ * 4:(hf_ + 1) * 4, :], psAv, r=["psA"], w=[("hfT", hf_)])
            for k in range(8):
                P.mm(psA[:, 0:16], hfT[:, k, :], wr[:, k, :], start=(k == 0), stop=(k == 7), r=[("hfT", 0), ("hfT", 1), "wr"], w=["psA"])
            P.copy("dve", lg[:], psA[:, 0:16], r=["psA"], w=["lg"])
            P.op("dve", lambda e: e.reduce_max(out=mx[:], in_=lg[:], axis=AX.X), ["lg"], ["mx"])
            P.ts("dve", mx[:], mx[:], -1.0, None, ALU.mult, r=["mx"], w=["mx"])
            P.act(ex[:], lg[:], AF.Exp, r=["lg", "mx"], w=["ex", "sm"], bias=mx[:], accum_out=sm[:])
            P.op("dve", lambda e: e.reciprocal(out=sm[:], in_=sm[:]), ["sm"], ["sm"])
            P.ts("dve", aff[t % 2][:], ex[:], sm[:], None, ALU.mult, r=["ex", "sm"], w=[("aff", t % 2)])
            P.dma("sp", aff_o[t * 128:(t + 1) * 128, :], aff[t % 2][:], r=[("aff", t % 2)], w=[("aff_o", t)], is_out=True)
    return P.build()


def run_A3(x1, mem, x_norm, m_norm, wq, wkv, wo, f_norm, f_router):
    if "A3" not in _cache:
        _cache["A3"] = build_A3()
    nc = _cache["A3"]
    maps = []
    for c in range(8):
        b = c // 4
        maps.append({"x": np.ascontiguousarray(x1[c]), "mem": np.ascontiguousarray(mem[b]), "xn": x_norm.reshape(1, D), "mn": m_norm.reshape(1, D),
                     "fn": f_norm.reshape(1, D), "wq": wq, "wkv": wkv, "wo": wo, "wr": f_router, "ident": _ident_np()})
    res = run(nc, maps)
    return ([res.results[c]["x2_o"] for c in range(8)], [res.results[c]["hf_o"] for c in range(8)],
            [res.results[c]["aff_o"] for c in range(8)])


CAP = 1024
FE = 2816
NFC = FE // 128


def build_M(nbis=32):
    P = Prog()
    aff_d = P.dram("aff", [128, 4, 64], F32)
    hf_d = P.dram("hf", [NB * S, D], BF16)
    wg_d = P.dram("wg", [2, D, FE], F32)
    wu_d = P.dram("wu", [2, D, FE], F32)
    wd_d = P.dram("wd", [2, FE, D], F32)
    ltri_d = P.dram("ltri", [128, 128], F32)
    iota_d = P.dram("iota", [128, 1024], F32)
    iotap_d = P.dram("iotap", [128, 8], F32)
    ident_d = P.dram("ident", [128, 128], F32)
    out_d = P.dram("out", [NB, S, D], F32, kind="ExternalOutput")

    affc = P.sb([128, 4, 64], F32)
    ltri = P.sb([128, 128], F32)
    iota = P.sb([128, 1024], F32)
    iotap = P.sb([128, 8], F32)
    identf = P.sb([128, 128], F32)
    onesf = P.sb([128, 128], F32)
    lo = P.sb([128, 4], F32)
    hi = P.sb([128, 4], F32)
    mid = P.sb([128, 4], F32)
    cnt = P.sb([128, 4], F32)
    sel = P.sb([128, 4], F32)
    d1 = P.sb([128, 4], F32)
    d2 = P.sb([128, 4], F32)
    cmp_ = P.sb([128, 4, 64], F32)
    msk = P.sb([128, 4, 64], F32)
    posf = P.sb([128, 4, 64], F32)
    tmpm = P.sb([128, 4, 64], F32)
    cs = P.sb([64, 1], F32)
    csb = P.sb([64, 128], F32)
    hft = [P.sb([128, D], BF16) for _ in range(3)]
    selt = [P.sb([128, 512], BF16) for _ in range(3)]
    xeT = P.sb([128, 8, CAP], BF16)
    wgs = [P.sb([128, 8, 256], BF16) for _ in range(2)]
    wus = [P.sb([128, 8, 256], BF16) for _ in range(2)]
    wgf = P.sb([128, 8, 256], F32)
    sg = [P.sb([128, 512], BF16) for _ in range(2)]
    hidT = P.sb([128, NFC, CAP], BF16)
    wdq = [P.sb([128, NFC, 256], BF16) for _ in range(2)]
    wdf = P.sb([128, NFC // 2, 256], F32)
    ye = [P.sb([128, 8, D], BF16) for _ in range(2)]
    diag = [P.sb([128, 128], F32) for _ in range(2)]
    selT = [P.sb([128, 8, 128], BF16) for _ in range(2)]
    otile = [P.sb([128, D], F32) for _ in range(2)]
    B = [P.ps([128, 512], F32) for _ in range(8)]
    bk = lambda i: ("B", i)

    P.dma("sp", affc[:], aff_d, w=["affc"])
    P.dma("sp", ltri[:], ltri_d, w=["ltri"])
    P.dma("sp", iota[:], iota_d, w=["iota"])
    P.dma("sp", iotap[:], iotap_d, w=["iotap"])
    P.dma("sp", identf[:], ident_d, w=["identf"])
    P.memset("dve", onesf[:], 1.0, w=["onesf"])
    P.memset("dve", lo[:], 0.0, w=["lo"])
    P.memset("dve", hi[:], 1.0, w=["hi"])
    for it in range(nbis):
        P.tt("dve", mid[:], lo[:], hi[:], ALU.add, r=["lo", "hi"], w=["mid"])
        P.ts("dve", mid[:], mid[:], 0.5, None, ALU.mult, r=["mid"], w=["mid"])
        P.tt("dve", cmp_[:], affc[:], mid[:].unsqueeze(2).broadcast_to([128, 4, 64]), ALU.is_ge, r=["affc", "mid"], w=["cmp"])
        P.op("dve", lambda e: e.tensor_reduce(out=cnt[:], in_=cmp_[:], axis=AX.X, op=ALU.add), ["cmp"], ["cnt"])
        P.mm(B[0][:, 0:4], onesf[:], cnt[:], r=["onesf", "cnt"], w=[bk(0)])
        P.ts("dve", sel[:], B[0][:, 0:4], float(CAP), None, ALU.is_ge, r=[bk(0)], w=["sel"])
        P.tt("dve", d1[:], mid[:], lo[:], ALU.subtract, r=["mid", "lo"], w=["d1"])
        P.tt("dve", d1[:], d1[:], sel[:], ALU.mult, r=["d1", "sel"], w=["d1"])
        P.tt("dve", d2[:], hi[:], mid[:], ALU.subtract, r=["hi", "mid"], w=["d2"])
        P.tt("dve", d2[:], d2[:], sel[:], ALU.mult, r=["d2", "sel"], w=["d2"])
        P.tt("dve", lo[:], lo[:], d1[:], ALU.add, r=["lo", "d1"], w=["lo"])
        P.tt("dve", hi[:], mid[:], d2[:], ALU.add, r=["mid", "d2"], w=["hi"])
    P.tt("dve", msk[:], affc[:], lo[:].unsqueeze(2).broadcast_to([128, 4, 64]), ALU.is_ge, r=["affc", "lo"], w=["msk"])
    for g in range(4):
        P.mm(B[1][0:64, 0:1], msk[:, g, :], onesf[:, 0:1], r=["msk", "onesf"], w=[bk(1)])
        P.copy("dve", cs[:], B[1][0:64, 0:1], r=[bk(1)], w=["cs"])
        P.ts("dve", csb[:], onesf[0:64, :], cs[:], None, ALU.mult, r=["onesf", "cs"], w=["csb"])
        P.mm(B[2][:, 0:64], csb[:], ltri[0:64, 0:64], start=True, stop=False, r=["csb", "ltri"], w=[bk(2)])
        P.mm(B[2][:, 0:64], ltri[:], msk[:, g, :], start=False, stop=True, r=["ltri", "msk"], w=[bk(2)])
        P.ts("dve", tmpm[:, g, :], msk[:, g, :], -4096.0, 4096.0, ALU.mult, ALU.add, r=["msk"], w=[("tmpm", g)])
        P.tt("dve", posf[:, g, :], B[2][:, 0:64], tmpm[:, g, :], ALU.add, r=[bk(2), ("tmpm", g)], w=[("posf", g)])

    ncnt = {"hf": 0, "w": 0, "sg": 0, "ev": 0, "sc": 0, "wd": 0}
    for b in range(NB):
        for el in range(2):
            g = b * 2 + el
            for sh in range(2):
                for j in range(64):
                    i3 = ncnt["hf"] % 3
                    ncnt["hf"] += 1
                    P.dma("sp" if j % 2 else "act", hft[i3][:], hf_d[b * S + j * 128: b * S + (j + 1) * 128, :], w=[("hft", i3)])
                    P.ts("dve", selt[i3][:], iota[:, sh * 512:(sh + 1) * 512], posf[:, g, j:j + 1], None, ALU.is_equal,
                         r=["iota", ("posf", g)], w=[("selt", i3)])
                    for k in range(8):
                        P.mm(B[k][:], hft[i3][:, k * 128:(k + 1) * 128], selt[i3][:], start=(j == 0), stop=(j == 63),
                             r=[("hft", i3), ("selt", i3)], w=[bk(k)])
                for k in range(8):
                    P.copy("act" if k % 2 else "dve", xeT[:, k, sh * 512:(sh + 1) * 512], B[k][:], r=[bk(k)], w=[("xeT", k, sh)])
            xk = [("xeT", k, sh) for k in range(8) for sh in range(2)]
            for fb in range(NFC // 2):
                wi = ncnt["w"] % 2
                ncnt["w"] += 1
                for k in range(8):
                    P.dma("sp" if k % 2 else "act", wgf[:, k, :], wg_d[el, k * 128:(k + 1) * 128, fb * 256:(fb + 1) * 256], w=[("wgf", k)])
                    P.dma("pool", wus[wi][:, k, :], wu_d[el, k * 128:(k + 1) * 128, fb * 256:(fb + 1) * 256], w=[("wus", wi)])
                P.copy("act", wgs[wi][:], wgf[:], r=[("wgf", k) for k in range(8)], w=[("wgs", wi)])
                for fcl in range(2):
                    fc = fb * 2 + fcl
                    for sh in range(2):
                        bg = (fc % 2) * 4 + sh
                        bu = (fc % 2) * 4 + 2 + sh
                        for k in range(8):
                            P.mm(B[bg][:], wgs[wi][:, k, fcl * 128:(fcl + 1) * 128], xeT[:, k, sh * 512:(sh + 1) * 512],
                                 start=(k == 0), stop=(k == 7), r=xk + [("wgs", wi)], w=[bk(bg)])
                        for k in range(8):
                            P.mm(B[bu][:], wus[wi][:, k, fcl * 128:(fcl + 1) * 128], xeT[:, k, sh * 512:(sh + 1) * 512],
                                 start=(k == 0), stop=(k == 7), r=xk + [("wus", wi)], w=[bk(bu)])
                        si = ncnt["sg"] % 2
                        ncnt["sg"] += 1
                        P.act(sg[si][:], B[bg][:], AF.Silu, r=[bk(bg)], w=[("sg", si)])
                        P.tt("dve", hidT[:, fc, sh * 512:(sh + 1) * 512], sg[si][:], B[bu][:], ALU.mult,
                             r=[("sg", si), bk(bu)], w=[("hidT", fc, sh)])
            for q4 in range(4):
                wq_ = ncnt["wd"] % 2
                ncnt["wd"] += 1
                for hh_ in range(2):
                    f0 = hh_ * (NFC // 2)
                    src = wd_d[el, f0 * 128:(f0 + NFC // 2) * 128, q4 * 256:(q4 + 1) * 256].rearrange("(f p) n -> p f n", p=128)
                    P.dma("sp" if hh_ else "act", wdf[:], src, w=["wdf"])
                    P.copy("act", wdq[wq_][:, f0:f0 + NFC // 2, :], wdf[:], r=["wdf"], w=[("wdq", wq_, hh_)])
                for st in range(8):
                    bi = ncnt["ev"] % 8
                    ncnt["ev"] += 1
                    for fc in range(NFC):
                        P.mm(B[bi][:, 0:256], hidT[:, fc, st * 128:(st + 1) * 128], wdq[wq_][:, fc, :],
                             start=(fc == 0), stop=(fc == NFC - 1), r=[("hidT", fc, st // 4), ("wdq", wq_, fc // (NFC // 2))], w=[bk(bi)])
                    P.copy("act" if bi % 2 else "dve", ye[el][:, st, q4 * 256:(q4 + 1) * 256], B[bi][:, 0:256], r=[bk(bi)], w=[("ye", el, st, q4)])
        def mk_sel(j, el):
            g = b * 2 + el
            si = ncnt["sc"] % 2
            ncnt["sc"] += 1
            P.ts("dve", diag[si][:], identf[:], posf[:, g, j:j + 1], None, ALU.mult, r=["identf", ("posf", g)], w=[("diag", si)])
            pb = 4 + si
            P.mm(B[pb][:, 0:128], onesf[:], diag[si][:], r=["onesf", ("diag", si)], w=[bk(pb)])
            P.tt("dve", selT[si][:], B[pb][:, 0:128].unsqueeze(1).broadcast_to([128, 8, 128]),
                 iotap[:].unsqueeze(2).broadcast_to([128, 8, 128]), ALU.is_equal, r=[bk(pb), "iotap"], w=[("selT", si)])
            return si

        steps = [(j, el) for j in range(64) for el in range(2)]
        nxt = mk_sel(*steps[0])
        for n_, (j, el) in enumerate(steps):
            si = nxt
            if n_ + 1 < len(steps):
                nxt = mk_sel(*steps[n_ + 1])
            ot = otile[j % 2]
            for dh in range(2):
                bi = el * 2 + dh
                for st in range(8):
                    P.mm(B[bi][:], selT[si][:, st, :], ye[el][:, st, dh * 512:(dh + 1) * 512], start=(st == 0), stop=(st == 7),
                         r=[("selT", si)] + [("ye", el, st, q4) for q4 in (2 * dh, 2 * dh + 1)], w=[bk(bi)])
            if el == 1:
                for dh in range(2):
                    ds_ = slice(dh * 512, (dh + 1) * 512)
                    P.ts("dve", ot[:, ds_], B[dh][:], affc[:, b * 2, j:j + 1], None, ALU.mult, r=[bk(dh), "affc"], w=[("ot", j % 2, dh)])
                    P.stt("dve", ot[:, ds_], B[2 + dh][:], affc[:, b * 2 + 1, j:j + 1], ot[:, ds_], ALU.mult, ALU.add,
                          r=[bk(2 + dh), "affc", ("ot", j % 2, dh)], w=[("ot", j % 2, dh)])
                P.dma("sp", out_d[b, j * 128:(j + 1) * 128, :], ot[:], r=[("ot", j % 2, 0), ("ot", j % 2, 1)], w=[("out", b, j)], is_out=True)
    return P.build()


def run_M(hf, aff, wg, wu, wd):
    if "M" not in _cache:
        _cache["M"] = build_M()
    nc = _cache["M"]
    hf_all = np.ascontiguousarray(np.concatenate([np.asarray(h) for h in hf], 0))
    aff_all = np.concatenate([np.asarray(a) for a in aff], 0).reshape(NB, S, 16)
    ltri = np.triu(np.ones((128, 128), np.float32), 1)
    iota = np.ascontiguousarray(np.broadcast_to(np.arange(1024, dtype=np.float32)[None, :], (128, 1024)))
    iotap = (np.arange(128, dtype=np.float32)[:, None] + 128.0 * np.arange(8, dtype=np.float32)[None, :]).astype(np.float32)
    maps = []
    for c in range(8):
        a = np.zeros((128, 4, 64), np.float32)
        for b in range(NB):
            for el in range(2):
                a[:, b * 2 + el, :] = aff_all[b, :, 2 * c + el].reshape(64, 128).T
        maps.append({"aff": a, "hf": hf_all, "wg": np.ascontiguousarray(wg[2 * c:2 * c + 2]), "wu": np.ascontiguousarray(wu[2 * c:2 * c + 2]),
                     "wd": np.ascontiguousarray(wd[2 * c:2 * c + 2]), "ltri": ltri, "iota": iota, "iotap": iotap, "ident": _ident_np()})
    res = run(nc, maps)
    return [res.results[c]["out"] for c in range(8)]


CH = 64
NCH = S // CH
NCB = 4


def build_S(nchunks=NCH):
    P = Prog()
    tm_d = P.dram("tm", [4, 64, 4, S], F32)
    fm_d = P.dram("fm", [5, 64, 4, S], F32)
    tri_d = P.dram("tri", [64, 64], F32)
    m2_d = P.dram("m2", [64, 128], F32)
    mT_d = P.dram("mT", [64, 64], F32)
    id_d = P.dram("id64", [64, 64], F32)
    y_d = P.dram("y", [64, 4, S], F32, kind="ExternalOutput")

    tri = P.sb([64, 64], F32)
    m2 = P.sb([64, 128], F32)
    mT = P.sb([64, 64], F32)
    id64 = P.sb([64, 64], F32)
    W = NCB * 64
    tmB = [[P.sb([64, 4, W], F32) for _ in range(4)] for _ in range(2)]
    fmB = [[P.sb([64, 4, W], F32) for _ in range(5)] for _ in range(2)]
    vB = [P.sb([64, 4, W], BF16) for _ in range(2)]
    yB = [P.sb([64, 4, W], F32) for _ in range(2)]
    St = P.sb([64, 4, 64], F32)
    Sb = P.sb([64, 4, 64], BF16)
    two = lambda shape, dt=F32: [P.sb(shape, dt) for _ in range(2)]
    Ep, Emm, dd, Eprev = two([64, 4, 64]), two([64, 4, 128]), two([64, 4, 64]), two([64, 4, 64])
    AR = two([64, 4, 128], BF16)
    ktf, btf, ktt, btt = two([64, 4, 64], BF16), two([64, 4, 64], BF16), two([64, 4, 64], BF16), two([64, 4, 64], BF16)
    MB, MK, Am = two([64, 4, 128], BF16), two([64, 4, 128], BF16), two([64, 4, 64], BF16)
    MA2 = [two([64, 4, 128], BF16) for _ in range(2)]
    Pm = [two([64, 4, 64], BF16) for _ in range(2)]
    Z, U, tmpS = P.sb([64, 4, 64], BF16), P.sb([64, 4, 64], BF16), P.sb([64, 4, 64], F32)
    Q = [P.ps([64, 4, 128], F32) for _ in range(8)]
    qk = lambda i: ("Q", i)

    P.dma("sp", tri[:], tri_d, w=["tri"])
    P.dma("sp", m2[:], m2_d, w=["m2"])
    P.dma("sp", mT[:], mT_d, w=["mT"])
    P.dma("sp", id64[:], id_d, w=["id64"])
    P.memset("dve", St[:], 0.0, w=["St"])
    P.memset("dve", Sb[:], 0.0, w=["Sb"])
    HS = [slice(0, 2), slice(2, 4)]
    bcm = lambda t, n, hs: t[:].unsqueeze(1).broadcast_to([64, n, t.shape[1]])
    final = {}

    def pre(c):
        blk, cl = c // NCB, c % NCB
        bp = blk % 2
        if cl == 0:
            bs = slice(blk * W, (blk + 1) * W)
            for a in range(4):
                P.dma("sp", tmB[bp][a][:], tm_d[a, :, :, bs], w=[("tmB", bp, a)])
            for a in range(5):
                P.dma("act", fmB[bp][a][:], fm_d[a, :, :, bs], w=[("fmB", bp, a)])
            P.copy("pool", vB[bp][:], tmB[bp][3][:], r=[("tmB", bp, 3)], w=[("vB", bp)])
        cs = slice(cl * 64, (cl + 1) * 64)
        lw_tm, k_tm, b_tm, v_tm = [tmB[bp][a][:, :, cs] for a in range(4)]
        lw_fm, r_fm, a_fm, k_fm, b_fm = [fmB[bp][a][:, :, cs] for a in range(5)]
        tmk = lambda a: ("tmB", bp, a)
        fmk = lambda a: ("fmB", bp, a)
        p = c % 2
        K_ = lambda name: (name, p)
        for ch in range(4):
            P.mm(Q[0][:, ch, 0:64], lw_tm[:, ch, :], tri[:], r=[tmk(0), "tri"], w=[qk(0)])
            P.mm(Q[0][:, ch, 64:128], tri[:], lw_tm[:, ch, :], r=[tmk(0), "tri"], w=[qk(0)])
        P.act(Ep[p][:], Q[0][:, :, 0:64], AF.Exp, r=[qk(0)], w=[K_("Ep"), qk(0)])
        P.act(Emm[p][:], Q[0][:], AF.Exp, r=[qk(0)], w=[K_("Emm"), qk(0)], scale=-1.0)
        P.tt("dve", dd[p][:], Q[0][:, :, 0:64], lw_fm, ALU.subtract, r=[qk(0), fmk(0)], w=[K_("dd"), qk(0)])
        P.act(Eprev[p][:], dd[p][:], AF.Exp, r=[K_("dd")], w=[K_("Eprev")])
        P.tt("dve", AR[p][:, :, 0:64], a_fm, Eprev[p][:], ALU.mult, r=[fmk(2), K_("Eprev")], w=[K_("ARa")])
        P.tt("pool", AR[p][:, :, 64:128], r_fm, Ep[p][:], ALU.mult, r=[fmk(1), K_("Ep")], w=[K_("ARr")])
        P.tt("dve", ktf[p][:], k_fm, Emm[p][:, :, 0:64], ALU.mult, r=[fmk(3), K_("Emm")], w=[K_("ktf")])
        P.tt("pool", btf[p][:], b_fm, Emm[p][:, :, 0:64], ALU.mult, r=[fmk(4), K_("Emm")], w=[K_("btf")])
        P.tt("dve", ktt[p][:], k_tm, Emm[p][:, :, 64:128], ALU.mult, r=[tmk(1), K_("Emm")], w=[K_("ktt")])
        P.tt("pool", btt[p][:], b_tm, Emm[p][:, :, 64:128], ALU.mult, r=[tmk(2), K_("Emm")], w=[K_("btt")])
        yield
        ARk = [K_("ARa"), K_("ARr")]
        for ch in range(4):
            P.mm(Q[1][:, ch, :], btf[p][:, ch, :], AR[p][:, ch, :], r=[K_("btf")] + ARk, w=[qk(1)])
            P.mm(Q[2][:, ch, :], ktf[p][:, ch, :], AR[p][:, ch, :], r=[K_("ktf")] + ARk, w=[qk(2)])
            P.mm(Q[3][:, ch, 0:64], AR[p][:, ch, 0:64], btf[p][:, ch, :], r=[K_("btf")] + ARk, w=[qk(3)])
        P.tt("dve", MB[p][:], Q[1][:], bcm(m2, 4, None), ALU.mult, r=[qk(1), "m2"], w=[K_("MB")])
        P.tt("dve", MK[p][:], Q[2][:], bcm(m2, 4, None), ALU.mult, r=[qk(2), "m2"], w=[K_("MK")])
        P.tt("dve", Am[p][:], Q[3][:, :, 0:64], bcm(mT, 4, None), ALU.mult, r=[qk(3), "mT"], w=[K_("Am")])
        P.tt("pool", Pm[p][0][:], MB[p][:, :, 0:64], bcm(id64, 4, None), ALU.add, r=[K_("MB"), "id64"], w=[K_("P0_0"), K_("P0_1")])
        yield
        curM = lambda ch: MB[p][:, ch, 0:64]
        curA = lambda ch: Am[p][:, ch, :]
        curk = lambda hf: [K_("MB"), K_("Am")]
        pi = 0
        for lvl in range(5):
            mi = lvl % 2
            for hf in range(2):
                qs_ = 4 + hf
                for ch in range(2 * hf, 2 * hf + 2):
                    P.mm(Q[qs_][:, ch, 0:64], curA(ch), curM(ch), r=curk(hf), w=[qk(qs_)])
                    P.mm(Q[qs_][:, ch, 64:128], curM(ch), curA(ch), r=curk(hf), w=[qk(qs_)])
                P.copy("act", MA2[p][mi][:, HS[hf], :], Q[qs_][:, HS[hf], :], r=[qk(qs_)], w=[K_("MA2_%d_%d" % (mi, hf))])
            curM = lambda ch, mi=mi: MA2[p][mi][:, ch, 0:64]
            curA = lambda ch, mi=mi: MA2[p][mi][:, ch, 64:128]
            curk = lambda hf, mi=mi: [K_("MA2_%d_%d" % (mi, hf))]
            yield
            for hf in range(2):
                qs_ = 4 + hf
                for ch in range(2 * hf, 2 * hf + 2):
                    cc = ch - 2 * hf + (2 - 2 * hf)
                    P.mm(Q[qs_][:, cc, 0:64], curA(ch), Pm[p][pi][:, ch, :], r=curk(hf) + [K_("P%d_%d" % (pi, hf))], w=[qk(qs_)])
                osl = slice(2 - 2 * hf, 4 - 2 * hf)
                P.tt("dve", Pm[p][1 - pi][:, HS[hf], :], Pm[p][pi][:, HS[hf], :], Q[qs_][:, osl, 0:64], ALU.add,
                     r=[K_("P%d_%d" % (pi, hf)), qk(qs_)], w=[K_("P%d_%d" % (1 - pi, hf))])
            pi = 1 - pi
            yield
        final[c] = pi

    def state(c):
        blk, cl = c // NCB, c % NCB
        bp = blk % 2
        cs = slice(cl * 64, (cl + 1) * 64)
        v_b = vB[bp][:, :, cs]
        vk = ("vB", bp)
        p = c % 2
        K_ = lambda name: (name, p)
        ARk = [K_("ARa"), K_("ARr")]
        pi = final[c]
        Pk = [K_("P%d_0" % pi), K_("P%d_1" % pi)]
        Pf = Pm[p][pi]
        for ch in range(4):
            P.mm(Q[6][:, ch, 0:64], AR[p][:, ch, 0:64], Sb[:, ch, :], start=True, stop=False, r=ARk + ["Sb"], w=[qk(6)])
            P.mm(Q[6][:, ch, 0:64], MK[p][:, ch, 0:64], v_b[:, ch, :], start=False, stop=True, r=[K_("MK"), vk], w=[qk(6)])
        P.copy("dve", Z[:], Q[6][:, :, 0:64], r=[qk(6)], w=["Z"])
        yield
        for ch in range(4):
            P.mm(Q[7][:, ch, 0:64], Pf[:, ch, :], Z[:, ch, :], r=Pk + ["Z"], w=[qk(7)])
        P.copy("dve", U[:], Q[7][:, :, 0:64], r=[qk(7)], w=["U"])
        yield
        for ch in range(4):
            P.mm(Q[6][:, ch, 64:128], btt[p][:, ch, :], U[:, ch, :], start=True, stop=False, r=[K_("btt"), "U"], w=[qk(6)])
            P.mm(Q[6][:, ch, 64:128], ktt[p][:, ch, :], v_b[:, ch, :], start=False, stop=True, r=[K_("ktt"), vk], w=[qk(6)])
        for ch in range(4):
            P.mm(Q[7][:, ch, 64:128], AR[p][:, ch, 64:128], Sb[:, ch, :], start=True, stop=False, r=ARk + ["Sb"], w=[qk(7)])
            P.mm(Q[7][:, ch, 64:128], MB[p][:, ch, 64:128], U[:, ch, :], start=False, stop=False, r=[K_("MB"), "U"], w=[qk(7)])
            P.mm(Q[7][:, ch, 64:128], MK[p][:, ch, 64:128], v_b[:, ch, :], start=False, stop=True, r=[K_("MK"), vk], w=[qk(7)])
        P.tt("dve", tmpS[:], Q[6][:, :, 64:128], St[:], ALU.add, r=[qk(6), "St"], w=["tmpS"])
        P.tt("dve", St[:], tmpS[:], Ep[p][:, :, 63:64].broadcast_to([64, 4, 64]), ALU.mult, r=["tmpS", K_("Ep")], w=["St"])
        P.copy("pool", Sb[:], St[:], r=["St"], w=["Sb"])
        P.copy("act", yB[bp][:, :, cs], Q[7][:, :, 64:128], r=[qk(7)], w=[("yB", bp, cl)])
        if cl == NCB - 1 or c == nchunks - 1:
            bs = slice(blk * W, (blk + 1) * W)
            P.dma("sp", y_d[:, :, bs], yB[bp][:], r=[("yB", bp, i) for i in range(NCB)], w=[("y", blk)], is_out=True)
        yield

    def drain(g):
        for _ in g:
            pass

    def step(g):
        try:
            next(g)
            return True
        except StopIteration:
            return False

    drain(pre(0))
    for c in range(nchunks):
        gs = state(c)
        if c + 1 < nchunks:
            gp = pre(c + 1)
            step(gp); step(gp)
            step(gs)
            step(gp)
            step(gs)
            step(gp)
            step(gs)
            drain(gp)
        else:
            drain(gs)
    return P.build()


def scan_consts():
    s = np.arange(64)[:, None]
    t = np.arange(64)[None, :]
    tri = (s <= t).astype(np.float32)
    m2 = np.concatenate([(s < t).astype(np.float32), (s <= t).astype(np.float32)], 1)
    mT = (t < s).astype(np.float32)
    return {"tri": tri, "m2": m2, "mT": mT, "id64": np.eye(64, dtype=np.float32)}


def tm_layout(x):
    return x.reshape(NCH, 64, 64).transpose(1, 0, 2).reshape(64, S)


def tm_unlayout(y):
    return y.reshape(64, NCH, 64).transpose(1, 0, 2).reshape(S, 64)


SCAN_ARRS = ["r", "v", "a", "lw0", "lw1", "kd0", "kd1", "b0", "b1", "bonus", "g"]
EXPM05 = float(np.exp(-0.5))
GELU_C = 1.5957691216057308


def gelu_tanh(P, out, x_ps, xkey, okey, t1, k1, t2, k2):
    P.copy("act", t1[:], x_ps, r=[xkey], w=[k1])
    P.tt("dve", t2[:], t1[:], t1[:], ALU.mult, r=[k1], w=[k2])
    P.ts("dve", t2[:], t2[:], 0.044715, 1.0, ALU.mult, ALU.add, r=[k2], w=[k2])
    P.tt("dve", t2[:], t2[:], t1[:], ALU.mult, r=[k2, k1], w=[k2])
    P.act(t2[:], t2[:], AF.Sigmoid, r=[k2], w=[k2], scale=GELU_C)
    P.tt("dve", out, t2[:], t1[:], ALU.mult, r=[k2, k1], w=[okey])


def build_O1():
    P = Prog()
    x2_d = P.dram("x2", [TOK, D], F32)
    pt_d = P.dram("parts", [8, TOK, D], F32)
    x2h_d = P.dram("x2h", [2, D], F32)
    pth_d = P.dram("partsh", [8, 2, D], F32)
    g_d = P.dram("g", [1, D], F32)
    w_d = P.dram("w", [D, 2944], F32)
    lng_d = P.dram("lng", [1, 512], F32)
    lnb_d = P.dram("lnb", [1, 512], F32)
    wsT_d = P.dram("wsT", [128, 4, 128], F32)
    bsT_d = P.dram("bsT", [128, 4], F32)
    mu_d = P.dram("mu", [128, 15, 2], F32)
    w2p_d = P.dram("w2p", [128, 2, 512], F32)
    a2p_d = P.dram("a2p", [128, 2, 512], F32)
    g2_d = P.dram("g2", [128, 512], F32)
    pv_d = P.dram("pvec", [128, 4, 8], F32)
    bo_d = P.dram("bones", [128, 128], F32)
    ident_d = P.dram("ident", [128, 128], F32)
    x3_o = P.dram("x3_o", [TOK, D], F32, kind="ExternalOutput")
    yc_o = P.dram("yc_o", [TOK, 512], BF16, kind="ExternalOutput")
    sc_o = P.dram("sc_o", [len(SCAN_ARRS), 4, 128, TOK], F32, kind="ExternalOutput")

    gb = P.sb([128, D], F32)
    wc = P.sb([128, 8, 2944], BF16)
    lng = P.sb([128, 512], F32)
    lnb = P.sb([128, 512], F32)
    wsTf = P.sb([128, 4, 128], F32)
    wsT = P.sb([128, 4, 128], BF16)
    bsT = P.sb([128, 4], F32)
    mu = P.sb([128, 15, 2], F32)
    c0 = P.sb([128, 15], F32)
    w2pf = P.sb([128, 2, 512], F32)
    a2pf = P.sb([128, 2, 512], F32)
    g2f = P.sb([128, 512], F32)
    w2p = P.sb([128, 2, 512], BF16)
    a2p = P.sb([128, 2, 512], BF16)
    g2 = P.sb([128, 512], BF16)
    pv = P.sb([128, 4, 8], F32)
    bones = P.sb([128, 128], F32)
    identf = P.sb([128, 128], F32)
    ident = P.sb([128, 128], BF16)
    scr = norm_scratch(P)
    acc = [P.sb([128, D], F32) for _ in range(2)]
    prt = [P.sb([128, D], F32) for _ in range(2)]
    h = P.sb([128, D], BF16)
    hT = P.sb([128, 8, TOK + 2], BF16)
    T = [P.sb([128, 512], F32) for _ in range(22)]
    tk = lambda i: ("T", i)
    ub = P.sb([128, 512], BF16)
    vnb = P.sb([128, 512], BF16)
    ycb = [P.sb([128, 512], BF16) for _ in range(2)]
    mean = P.sb([128, 1], F32)
    var = P.sb([128, 1], F32)
    fd = P.sb([128, 514], F32)
    thb = P.sb([128, 512], BF16)
    hab = P.sb([128, 512], BF16)
    sgb = P.sb([128, 512], BF16)
    Bt = P.ps([128, 8, 128], BF16)
    Bc = [P.ps([128, 512], F32) for _ in range(2)]
    Bs = P.ps([128, 4, 128], F32)
    Bm = P.ps([128, 512], F32)
    Be = P.ps([128, 512], F32)
    Bl = [P.ps([128, 512], F32) for _ in range(2)]

    P.dma("sp", gb[:], g_d.partition_broadcast(128), w=["gb"])
    P.dma("sp", lng[:], lng_d.partition_broadcast(128), w=["lng"])
    P.dma("sp", lnb[:], lnb_d.partition_broadcast(128), w=["lnb"])
    P.dma("sp", identf[:], ident_d, w=["identf"])
    P.copy("dve", ident[:], identf[:], r=["identf"], w=["ident"])
    P.dma("sp", wsTf[:], wsT_d, w=["wsTf"])
    P.copy("dve", wsT[:], wsTf[:], r=["wsTf"], w=["wsT"])
    P.dma("sp", bsT[:], bsT_d, w=["bsT"])
    P.dma("sp", mu[:], mu_d, w=["mu"])
    P.dma("sp", w2pf[:], w2p_d, w=["w2pf"])
    P.dma("sp", a2pf[:], a2p_d, w=["a2pf"])
    P.dma("sp", g2f[:], g2_d, w=["g2f"])
    P.copy("dve", w2p[:], w2pf[:], r=["w2pf"], w=["w2p"])
    P.copy("dve", a2p[:], a2pf[:], r=["a2pf"], w=["a2p"])
    P.copy("dve", g2[:], g2f[:], r=["g2f"], w=["g2"])
    P.dma("sp", pv[:], pv_d, w=["pv"])
    P.ts("dve", pv[:, :, 7], pv[:, :, 5], -1.0, 1.0, ALU.mult, ALU.add, r=["pv"], w=["pv"])
    P.dma("sp", bones[:], bo_d, w=["bones"])
    P.tt("dve", c0[:], mu[:, :, 0], mu[:, :, 1], ALU.add, r=["mu"], w=["c0"])
    P.ts("dve", c0[:], c0[:], -1.0, 1.0, ALU.mult, ALU.add, r=["c0"], w=["c0"])
    for k in range(8):
        P.dma("pool", wc[:, k, 0:1472], w_d[k * 128:(k + 1) * 128, 0:1472], w=[("wc", k)])
        P.dma("pool", wc[:, k, 1472:2944], w_d[k * 128:(k + 1) * 128, 1472:2944], w=[("wc", k)])
    wck = [("wc", k) for k in range(8)]
    P.dma("sp", acc[0][0:2, :], x2h_d, w=[("acc", 0)])
    for c in range(8):
        P.dma("sp", prt[c % 2][0:2, :], pth_d[c], w=[("prt", c % 2)])
        P.tt("dve", acc[0][0:2, :], acc[0][0:2, :], prt[c % 2][0:2, :], ALU.add, r=[("acc", 0), ("prt", c % 2)], w=[("acc", 0)])
    sq, ss, rstd = scr["sq"], scr["ss"], scr["rstd"]
    P.act(sq[0:2, :], acc[0][0:2, :], AF.Square, r=[("acc", 0)], w=["sq", "ss"], accum_out=ss[0:2, :])
    P.ts("dve", rstd[0:2, :], ss[0:2, :], 1.0 / D, 1e-6, ALU.mult, ALU.add, r=["ss"], w=["rstd"])
    P.act(rstd[0:2, :], rstd[0:2, :], AF.Sqrt, r=["rstd"], w=["rstd"])
    P.op("dve", lambda e: e.reciprocal(out=rstd[0:2, :], in_=rstd[0:2, :]), ["rstd"], ["rstd"])
    P.stt("dve", h[0:2, :], acc[0][0:2, :], rstd[0:2, :], gb[0:2, :], ALU.mult, ALU.mult, r=[("acc", 0), "rstd", "gb"], w=["h"])
    for k in range(8):
        P.tr(Bt[:, k, 0:2], h[0:2, k * 128:(k + 1) * 128], ident[0:2, 0:2], r=["h", "ident"], w=["Bt"])
    P.copy("act", hT[:, :, 0:1], Bt[:, :, 0:1], r=["Bt"], w=[("hT", "h0")])
    P.copy("act", hT[:, :, TOK + 1:TOK + 2], Bt[:, :, 1:2], r=["Bt"], w=[("hT", "h1")])
    for t in range(NT):
        a = acc[t % 2]
        ak = ("acc", t % 2)
        ts_ = slice(t * 128, (t + 1) * 128)
        P.dma("sp", a[:], x2_d[ts_, :], w=[ak])
        for c in range(8):
            pb = prt[c % 2]
            P.dma("act" if c % 2 else "sp", pb[:], pt_d[c, ts_, :], w=[("prt", c % 2)])
            P.tt("dve" if c % 2 else "pool", a[:], a[:], pb[:], ALU.add, r=[ak, ("prt", c % 2)], w=[ak])
        P.dma("sp", x3_o[ts_, :], a[:], r=[ak], w=[("x3_o", t)], is_out=True)
        rmsnorm_tile(P, a[:], ak, gb[:], h[:], "h", scr)
        for k in range(8):
            P.tr(Bt[:, k, :], h[:, k * 128:(k + 1) * 128], ident[:], r=["h", "ident"], w=["Bt"])
        P.copy("act", hT[:, :, 1 + t * 128:1 + (t + 1) * 128], Bt[:], r=["Bt"], w=[("hT", t)])
        for k in range(8):
            P.mm(Bc[0][:], hT[:, k, 1 + t * 128:1 + (t + 1) * 128], wc[:, k, 0:512], start=(k == 0), stop=(k == 7), r=[("hT", t)] + wck, w=["Bc0"])
        for k in range(8):
            P.mm(Bc[1][:], hT[:, k, 1 + t * 128:1 + (t + 1) * 128], wc[:, k, 512:1024], start=(k == 0), stop=(k == 7), r=[("hT", t)] + wck, w=["Bc1"])
        gelu_tanh(P, ub[:], Bc[0][:], "Bc0", "ub", T[0], tk(0), T[1], tk(1))
        gelu_tanh(P, T[4][:], Bc[1][:], "Bc1", tk(4), T[2], tk(2), T[3], tk(3))
        P.op("dve", lambda e: e.reduce_sum(out=mean[:], in_=T[4][:], axis=AX.X), [tk(4)], ["mean"])
        P.ts("dve", mean[:], mean[:], -1.0 / 512, None, ALU.mult, r=["mean"], w=["mean"])
        P.ts("dve", T[5][:], T[4][:], mean[:], None, ALU.add, r=[tk(4), "mean"], w=[tk(5)])
        P.act(T[2][:], T[5][:], AF.Square, r=[tk(5)], w=[tk(2), "var"], accum_out=var[:])
        P.ts("dve", var[:], var[:], 1.0 / 512, 1e-5, ALU.mult, ALU.add, r=["var"], w=["var"])
        P.act(var[:], var[:], AF.Sqrt, r=["var"], w=["var"])
        P.op("dve", lambda e: e.reciprocal(out=var[:], in_=var[:]), ["var"], ["var"])
        P.stt("dve", T[5][:], T[5][:], var[:], lng[:], ALU.mult, ALU.mult, r=[tk(5), "var", "lng"], w=[tk(5)])
        P.tt("dve", vnb[:], T[5][:], lnb[:], ALU.add, r=[tk(5), "lnb"], w=["vnb"])
        for g in range(4):
            P.mm(Bs[:, g, :], wsT[:, g, :], vnb[:, g * 128:(g + 1) * 128], r=["wsT", "vnb"], w=["Bs"])
        P.tt("dve", T[5][:].rearrange("p (g c) -> p g c", c=128), Bs[:], bsT[:].unsqueeze(2).broadcast_to([128, 4, 128]), ALU.add,
             r=["Bs", "bsT"], w=[tk(5)])
        yb = ycb[t % 2]
        P.tt("dve", yb[:], T[5][:], ub[:], ALU.mult, r=[tk(5), "ub"], w=[("ycb", t % 2)])
        P.dma("sp", yc_o[ts_, :], yb[:], r=[("ycb", t % 2)], w=[("yc_o", t)], is_out=True)
    hTk = [("hT", t) for t in range(NT)] + [("hT", "h0"), ("hT", "h1")]

    def shifted(blk, chunk, out_tile, okey):
        c0s = slice(blk * 512, blk * 512 + 514)
        col = 1024 + chunk * 128
        for k in range(8):
            P.mm(Bm[:], wc[:, k, col:col + 128], hT[:, k, 1 + blk * 512:1 + (blk + 1) * 512], start=(k == 0), stop=(k == 7), r=hTk + wck, w=["Bm"])
        for k in range(8):
            P.mm(Be[:, 0:2], wc[:, k, col:col + 128], hT[:, k, blk * 512:blk * 512 + 514:513], start=(k == 0), stop=(k == 7), r=hTk + wck, w=["Be"])
        P.copy("act", fd[:, 1:513], Bm[:], r=["Bm"], w=["fd_m"])
        P.copy("act", fd[:, 0:514:513], Be[:, 0:2], r=["Be"], w=["fd_e"])
        fk = ["fd_m", "fd_e"]
        P.ts("dve", out_tile[:], fd[:, 1:513], c0[:, chunk:chunk + 1], None, ALU.mult, r=fk + ["c0"], w=[okey])
        P.stt("dve", out_tile[:], fd[:, 0:512], mu[:, chunk, 0:1], out_tile[:], ALU.mult, ALU.add, r=fk + ["mu", okey], w=[okey])
        P.stt("dve", out_tile[:], fd[:, 2:514], mu[:, chunk, 1:2], out_tile[:], ALU.mult, ALU.add, r=fk + ["mu", okey], w=[okey])

    nb = 0
    for blk in range(TOK // 512):
        bs_ = slice(blk * 512, (blk + 1) * 512)
        shifted(blk, 12, T[0], tk(0))
        P.act(thb[:], T[0][:], AF.Tanh, r=[tk(0)], w=["thb"])
        shifted(blk, 13, T[0], tk(0))
        P.copy("act", hab[:], T[0][:], r=[tk(0)], w=["hab"])
        shifted(blk, 14, T[0], tk(0))
        P.act(sgb[:], T[0][:], AF.Sigmoid, r=[tk(0)], w=["sgb"])
        for pc in range(4):
            cs_ = slice(pc * 128, (pc + 1) * 128)
            PV = lambda i: pv[:, pc, i:i + 1]
            o = lambda name, tile, key: P.dma("sp", sc_o[SCAN_ARRS.index(name), pc, :, bs_], tile[:], r=[key], w=[("sc_o", name, pc, blk)], is_out=True)
            shifted(blk, pc, T[1], tk(1))
            shifted(blk, 4 + pc, T[2], tk(2))
            shifted(blk, 8 + pc, T[3], tk(3))
            o("r", T[1], tk(1))
            o("v", T[3], tk(3))
            for dr in range(2):
                bl = Bl[nb % 2]
                blk_ = ("Bl", nb % 2)
                nb += 1
                P.mm(bl[:], w2p[:, dr, cs_], thb[:], r=["w2p", "thb"], w=[blk_])
                P.act(T[4 + dr][:], bl[:], AF.Sigmoid, r=[blk_, "pv"], w=[tk(4 + dr)], bias=PV(dr))
                P.ts("pool", T[4 + dr][:], T[4 + dr][:], -EXPM05, None, ALU.mult, r=[tk(4 + dr)], w=[tk(4 + dr)])
                o("lw%d" % dr, T[4 + dr], tk(4 + dr))
                bl = Bl[nb % 2]
                blk_ = ("Bl", nb % 2)
                nb += 1
                P.mm(bl[:], a2p[:, dr, cs_], hab[:], r=["a2p", "hab"], w=[blk_])
                P.act(T[6 + dr][:], bl[:], AF.Sigmoid, r=[blk_, "pv"], w=[tk(6 + dr)], bias=PV(2 + dr))
            bl = Bl[nb % 2]
            blk_ = ("Bl", nb % 2)
            nb += 1
            P.mm(bl[:], g2[:, cs_], sgb[:], r=["g2", "sgb"], w=[blk_])
            P.copy("act", T[8][:], bl[:], r=[blk_], w=[tk(8)])
            o("g", T[8], tk(8))
            P.ts("dve", T[9][:], T[2][:], PV(4), None, ALU.mult, r=[tk(2), "pv"], w=[tk(9)])
            P.tt("dve", T[10][:], T[9][:], T[9][:], ALU.mult, r=[tk(9)], w=[tk(10)])
            bl = Bl[nb % 2]
            blk_ = ("Bl", nb % 2)
            nb += 1
            P.mm(bl[:], bones[:], T[10][:], r=["bones", tk(10)], w=[blk_])
            P.act(T[10][:], bl[:], AF.Sqrt, r=[blk_], w=[tk(10)])
            P.ts("dve", T[10][:], T[10][:], 1e-12, None, ALU.max, r=[tk(10)], w=[tk(10)])
            P.op("dve", lambda e: e.reciprocal(out=T[10][:], in_=T[10][:]), [tk(10)], [tk(10)])
            P.tt("dve", T[9][:], T[9][:], T[10][:], ALU.mult, r=[tk(9), tk(10)], w=[tk(9)])
            P.ts("pool", T[11][:], T[9][:], -1.0, None, ALU.mult, r=[tk(9)], w=[tk(11)])
            o("a", T[11], tk(11))
            for dr in range(2):
                P.ts("dve", T[12 + dr][:], T[6 + dr][:], PV(5), PV(7), ALU.mult, ALU.add, r=[tk(6 + dr), "pv"], w=[tk(12 + dr)])
                P.tt("dve", T[12 + dr][:], T[12 + dr][:], T[2][:], ALU.mult, r=[tk(12 + dr), tk(2)], w=[tk(12 + dr)])
                o("kd%d" % dr, T[12 + dr], tk(12 + dr))
                P.tt("pool", T[14 + dr][:], T[9][:], T[6 + dr][:], ALU.mult, r=[tk(9), tk(6 + dr)], w=[tk(14 + dr)])
                o("b%d" % dr, T[14 + dr], tk(14 + dr))
            P.tt("dve", T[16][:], T[12][:], T[13][:], ALU.add, r=[tk(12), tk(13)], w=[tk(16)])
            P.tt("dve", T[16][:], T[16][:], T[1][:], ALU.mult, r=[tk(16), tk(1)], w=[tk(16)])
            P.ts("dve", T[16][:], T[16][:], PV(6), None, ALU.mult, r=[tk(16), "pv"], w=[tk(16)])
            bl = Bl[nb % 2]
            blk_ = ("Bl", nb % 2)
            nb += 1
            P.mm(bl[:], bones[:], T[16][:], r=["bones", tk(16)], w=[blk_])
            P.tt("dve", T[17][:], bl[:], T[3][:], ALU.mult, r=[blk_, tk(3)], w=[tk(17)])
            o("bonus", T[17], tk(17))
    return P.build()


def run_O1(x2, parts, prm):
    if "O1" not in _cache:
        _cache["O1"] = build_O1()
    nc = _cache["O1"]
    wsT = np.ascontiguousarray(prm["c_w_s"].transpose(2, 0, 1))
    bsT = np.ascontiguousarray(prm["c_b_s"].T)
    shift = prm["d_shift"]
    mu = np.ascontiguousarray(shift.reshape(2, 15, 128).transpose(2, 1, 0))
    w2p = np.zeros((128, 2, 512), np.float32)
    a2p = np.zeros((128, 2, 512), np.float32)
    for dr in range(2):
        w2p[dr * 64:(dr + 1) * 64, dr, :] = prm["d_w2"][dr]
        a2p[dr * 64:(dr + 1) * 64, dr, :] = prm["d_a2"][dr]
    pvec = np.zeros((128, 4, 8), np.float32)
    col = lambda v: v.reshape(4, 128).T
    pvec[:, :, 0] = col(prm["d_w0"][0]); pvec[:, :, 1] = col(prm["d_w0"][1])
    pvec[:, :, 2] = col(prm["d_a0"][0]); pvec[:, :, 3] = col(prm["d_a0"][1])
    pvec[:, :, 4] = col(prm["d_k_k"]); pvec[:, :, 5] = col(prm["d_k_a"]); pvec[:, :, 6] = col(prm["d_r_k"].reshape(512))
    bones = np.kron(np.eye(2, dtype=np.float32), np.ones((64, 64), np.float32))
    xfull = [np.concatenate([np.asarray(x2[b * 4 + i]) for i in range(4)], 0) for b in range(NB)]
    maps = []
    for c in range(8):
        b, q = c // 4, c % 4
        lo, hi = q * TOK, (q + 1) * TOK
        x2h = np.zeros((2, D), np.float32)
        pth = np.zeros((8, 2, D), np.float32)
        if lo > 0:
            x2h[0] = xfull[b][lo - 1]
            for cc in range(8):
                pth[cc, 0] = parts[cc][b, lo - 1]
        if hi < S:
            x2h[1] = xfull[b][hi]
            for cc in range(8):
                pth[cc, 1] = parts[cc][b, hi]
        maps.append({"x2": np.ascontiguousarray(x2[c]), "parts": np.ascontiguousarray(np.stack([parts[cc][b, lo:hi] for cc in range(8)], 0)),
                     "x2h": x2h, "partsh": pth, "g": prm["o_norm"].reshape(1, D), "w": prm["o_w_in"],
                     "lng": prm["c_ln_g"].reshape(1, 512), "lnb": prm["c_ln_b"].reshape(1, 512), "wsT": wsT, "bsT": bsT, "mu": mu,
                     "w2p": w2p, "a2p": a2p, "g2": prm["d_g2"], "pvec": pvec, "bones": bones, "ident": _ident_np()})
    res = run(nc, maps)
    return ([res.results[c]["x3_o"] for c in range(8)], [res.results[c]["yc_o"] for c in range(8)],
            [res.results[c]["sc_o"] for c in range(8)])


def build_O2():
    P = Prog()
    yf_d = P.dram("yf", [TOK, 512], F32)
    yb_d = P.dram("yb", [TOK, 512], F32)
    bo_d = P.dram("bonus", [TOK, 512], F32)
    gt_d = P.dram("gt", [TOK, 512], F32)
    yc_d = P.dram("yc", [TOK, 512], BF16)
    x3_d = P.dram("x3", [TOK, D], F32)
    lng_d = P.dram("lng", [1, 512], F32)
    lnb_d = P.dram("lnb", [1, 512], F32)
    wo_d = P.dram("wo", [D, D], F32)
    ident_d = P.dram("ident", [128, 128], F32)
    x4_o = P.dram("x4_o", [TOK, D], F32, kind="ExternalOutput")

    lng = P.sb([128, 512], F32)
    lnb = P.sb([128, 512], F32)
    wo = P.sb([128, 8, D], BF16)
    identf = P.sb([128, 128], F32)
    ident = P.sb([128, 128], BF16)
    yf = [P.sb([128, 512], F32) for _ in range(2)]
    yb = [P.sb([128, 512], F32) for _ in range(2)]
    bo = [P.sb([128, 512], F32) for _ in range(2)]
    gt = [P.sb([128, 512], F32) for _ in range(2)]
    x3 = [P.sb([128, D], F32) for _ in range(2)]
    cat = [P.sb([128, D], BF16) for _ in range(2)]
    y = P.sb([128, 512], F32)
    sq = P.sb([128, 512], F32)
    st = P.sb([128, 8], F32)
    catT = P.sb([128, 8, 128], BF16)
    xo = [P.sb([128, D], F32) for _ in range(2)]
    ptr = P.ps([128, 8, 128], BF16)
    pso = [P.ps([128, 512], F32) for _ in range(2)]

    P.dma("sp", lng[:], lng_d.partition_broadcast(128), w=["lng"])
    P.dma("sp", lnb[:], lnb_d.partition_broadcast(128), w=["lnb"])
    P.dma("sp", identf[:], ident_d, w=["identf"])
    P.copy("dve", ident[:], identf[:], r=["identf"], w=["ident"])
    for k in range(8):
        P.dma("pool", wo[:, k, :], wo_d[k * 128:(k + 1) * 128, :], w=[("wo", k)])
    wok = [("wo", k) for k in range(8)]
    v3 = lambda ap: ap.rearrange("p (h c) -> p h c", c=64)
    bc = lambda ap: ap.unsqueeze(2).broadcast_to([128, 8, 64])
    for t in range(NT):
        i = t % 2
        ts_ = slice(t * 128, (t + 1) * 128)
        P.dma("sp", yf[i][:], yf_d[ts_, :], w=[("yf", i)])
        P.dma("act", yb[i][:], yb_d[ts_, :], w=[("yb", i)])
        P.dma("sp", bo[i][:], bo_d[ts_, :], w=[("bo", i)])
        P.dma("act", gt[i][:], gt_d[ts_, :], w=[("gt", i)])
        P.dma("sp", x3[i][:], x3_d[ts_, :], w=[("x3", i)])
        P.dma("act", cat[i][:, 0:512], yc_d[ts_, :], w=[("cat", i, 0)])
        P.tt("dve", y[:], yf[i][:], yb[i][:], ALU.add, r=[("yf", i), ("yb", i)], w=["y"])
        P.op("dve", lambda e: e.tensor_reduce(out=st[:], in_=v3(y[:]), axis=AX.X, op=ALU.add), ["y"], ["st"])
        P.ts("dve", st[:], st[:], -1.0 / 64, None, ALU.mult, r=["st"], w=["st"])
        P.tt("dve", v3(y[:]), v3(y[:]), bc(st[:]), ALU.add, r=["y", "st"], w=["y"])
        P.tt("pool", sq[:], y[:], y[:], ALU.mult, r=["y"], w=["sq"])
        P.op("dve", lambda e: e.tensor_reduce(out=st[:], in_=v3(sq[:]), axis=AX.X, op=ALU.add), ["sq"], ["st"])
        P.ts("dve", st[:], st[:], 1.0 / 64, 64e-5, ALU.mult, ALU.add, r=["st"], w=["st"])
        P.act(st[:], st[:], AF.Sqrt, r=["st"], w=["st"])
        P.op("dve", lambda e: e.reciprocal(out=st[:], in_=st[:]), ["st"], ["st"])
        P.tt("dve", v3(y[:]), v3(y[:]), bc(st[:]), ALU.mult, r=["y", "st"], w=["y"])
        P.tt("dve", y[:], y[:], lng[:], ALU.mult, r=["y", "lng"], w=["y"])
        P.tt("dve", y[:], y[:], lnb[:], ALU.add, r=["y", "lnb"], w=["y"])
        P.tt("dve", y[:], y[:], bo[i][:], ALU.add, r=["y", ("bo", i)], w=["y"])
        P.tt("dve", cat[i][:, 512:1024], y[:], gt[i][:], ALU.mult, r=["y", ("gt", i)], w=[("cat", i, 1)])
        ck = [("cat", i, 0), ("cat", i, 1)]
        for k in range(8):
            P.tr(ptr[:, k, :], cat[i][:, k * 128:(k + 1) * 128], ident[:], r=ck + ["ident"], w=["ptr"])
        P.copy("act", catT[:], ptr[:], r=["ptr"], w=["catT"])
        for hf_ in range(2):
            for k in range(8):
                P.mm(pso[hf_][:], catT[:, k, :], wo[:, k, hf_ * 512:(hf_ + 1) * 512], start=(k == 0), stop=(k == 7), r=["catT"] + wok, w=[("pso", hf_)])
            P.tt("dve", xo[i][:, hf_ * 512:(hf_ + 1) * 512], pso[hf_][:], x3[i][:, hf_ * 512:(hf_ + 1) * 512], ALU.add,
                 r=[("pso", hf_), ("x3", i)], w=[("xo", i, hf_)])
        P.dma("sp", x4_o[ts_, :], xo[i][:], r=[("xo", i, 0), ("xo", i, 1)], w=[("x4_o", t)], is_out=True)
    return P.build()


def build_F():
    P = Prog()
    x_d = P.dram("x", [TOK, D], F32)
    pt_d = P.dram("parts", [8, TOK, D], F32)
    g_d = P.dram("g", [1, D], F32)
    o_d = P.dram("o", [TOK, D], F32, kind="ExternalOutput")
    gb = P.sb([128, D], F32)
    scr = norm_scratch(P)
    acc = [P.sb([128, D], F32) for _ in range(2)]
    prt = [P.sb([128, D], F32) for _ in range(2)]
    ob = [P.sb([128, D], F32) for _ in range(2)]
    P.dma("sp", gb[:], g_d.partition_broadcast(128), w=["gb"])
    for t in range(NT):
        a = acc[t % 2]
        ak = ("acc", t % 2)
        ts_ = slice(t * 128, (t + 1) * 128)
        P.dma("sp", a[:], x_d[ts_, :], w=[ak])
        for c in range(8):
            pb = prt[c % 2]
            P.dma("act" if c % 2 else "sp", pb[:], pt_d[c, ts_, :], w=[("prt", c % 2)])
            P.tt("dve" if c % 2 else "pool", a[:], a[:], pb[:], ALU.add, r=[ak, ("prt", c % 2)], w=[ak])
        rmsnorm_tile(P, a[:], ak, gb[:], ob[t % 2][:], ("ob", t % 2), scr)
        P.dma("sp", o_d[ts_, :], ob[t % 2][:], r=[("ob", t % 2)], w=[("o", t)], is_out=True)
    return P.build()


def run_O2(yf, yb, bonus, gt, yc, x3, prm):
    if "O2" not in _cache:
        _cache["O2"] = build_O2()
    nc = _cache["O2"]
    maps = []
    for c in range(8):
        maps.append({"yf": yf[c], "yb": yb[c], "bonus": bonus[c], "gt": gt[c], "yc": np.ascontiguousarray(yc[c]), "x3": np.ascontiguousarray(x3[c]),
                     "lng": prm["d_ln_g"].reshape(1, 512), "lnb": prm["d_ln_b"].reshape(1, 512), "wo": prm["o_w_out"], "ident": _ident_np()})
    res = run(nc, maps)
    return [res.results[c]["x4_o"] for c in range(8)]


def run_F(x5, parts, final_norm):
    if "F" not in _cache:
        _cache["F"] = build_F()
    nc = _cache["F"]
    maps = []
    for c in range(8):
        b, q = c // 4, c % 4
        maps.append({"x": np.ascontiguousarray(x5[c]), "parts": np.ascontiguousarray(np.stack([parts[cc][b, q * TOK:(q + 1) * TOK] for cc in range(8)], 0)),
                     "g": final_norm.reshape(1, D)})
    res = run(nc, maps)
    return [res.results[c]["o"] for c in range(8)]


def run_S(sc):
    if "S" not in _cache:
        _cache["S"] = build_S()
    nc = _cache["S"]
    ai = {n: i for i, n in enumerate(SCAN_ARRS)}
    full = [np.concatenate([np.asarray(sc[b * 4 + i]).reshape(len(SCAN_ARRS), 512, TOK) for i in range(4)], axis=2) for b in range(NB)]
    consts = scan_consts()
    maps = []
    for h in range(8):
        tm = np.zeros((4, 64, 4, S), np.float32)
        fm = np.zeros((5, 64, 4, S), np.float32)
        hs = slice(h * 64, (h + 1) * 64)
        for b in range(NB):
            for dr in range(2):
                ci = b * 2 + dr
                fl = (lambda a: a) if dr == 0 else (lambda a: a[:, ::-1])
                get = lambda n: fl(full[b][ai[n], hs, :])
                lw, r, a, k, bb, v = get("lw%d" % dr), get("r"), get("a"), get("kd%d" % dr), get("b%d" % dr), get("v")
                for j, arr in enumerate([lw, r, a, k, bb]):
                    fm[j, :, ci, :] = arr
                for j, arr in enumerate([lw, k, bb, v]):
                    tm[j, :, ci, :] = tm_layout(np.ascontiguousarray(arr.T))
        m = {"tm": tm, "fm": fm}
        m.update(consts)
        maps.append(m)
    res = run(nc, maps)
    yfull = np.zeros((NB, 2, S, 512), np.float32)
    for h in range(8):
        yo = np.asarray(res.results[h]["y"])
        for b in range(NB):
            for dr in range(2):
                yy = tm_unlayout(yo[:, b * 2 + dr, :])
                yfull[b, dr, :, h * 64:(h + 1) * 64] = yy if dr == 0 else yy[::-1]
    yf = [np.ascontiguousarray(yfull[c // 4, 0, (c % 4) * TOK:(c % 4 + 1) * TOK]) for c in range(8)]
    yb = [np.ascontiguousarray(yfull[c // 4, 1, (c % 4) * TOK:(c % 4 + 1) * TOK]) for c in range(8)]
    return yf, yb


def kernel(x, mem, e_norm, e_w_in, e_sink, e_w_out, o_norm, o_w_in, c_ln_g, c_ln_b, c_w_s, c_b_s, d_shift, d_w0, d_w2, d_a0, d_a2,
           d_g2, d_k_k, d_k_a, d_r_k, d_ln_g, d_ln_b, o_w_out, x_norm, m_norm, x_wq, x_wkv, x_wo, f_norm, f_router, f_w_gate,
           f_w_up, f_w_down, final_norm):
    f = lambda a: np.asarray(a, dtype=np.float32)
    x, mem = f(x), f(mem)
    qk, v = run_A1(x, f(e_norm)[0], f(e_w_in)[0])
    x1 = run_A2(qk, v, x, f(e_sink)[0], f(e_w_out)[0])
    x2, hf, aff = run_A3(x1, mem, f(x_norm)[0], f(m_norm)[0], f(x_wq)[0], f(x_wkv)[0], f(x_wo)[0], f(f_norm)[0], f(f_router)[0])
    parts = run_M(hf, aff, f(f_w_gate)[0], f(f_w_up)[0], f(f_w_down)[0])
    prm = {"o_norm": f(o_norm)[0], "o_w_in": f(o_w_in)[0], "c_ln_g": f(c_ln_g)[0], "c_ln_b": f(c_ln_b)[0], "c_w_s": f(c_w_s)[0],
           "c_b_s": f(c_b_s)[0], "d_shift": f(d_shift)[0], "d_w0": f(d_w0)[0], "d_w2": f(d_w2)[0], "d_a0": f(d_a0)[0], "d_a2": f(d_a2)[0],
           "d_g2": f(d_g2)[0], "d_k_k": f(d_k_k)[0], "d_k_a": f(d_k_a)[0], "d_r_k": f(d_r_k)[0], "d_ln_g": f(d_ln_g)[0],
           "d_ln_b": f(d_ln_b)[0], "o_w_out": f(o_w_out)[0]}
    x3, yc, sc = run_O1(x2, parts, prm)
    yf, yb = run_S(sc)
    ai = {n: i for i, n in enumerate(SCAN_ARRS)}
    tmaj = lambda c, n: np.ascontiguousarray(np.asarray(sc[c])[ai[n]].reshape(512, TOK).T)
    bonus = [tmaj(c, "bonus") for c in range(8)]
    gt = [tmaj(c, "g") for c in range(8)]
    x4 = run_O2(yf, yb, bonus, gt, yc, x3, prm)
    x5, hf, aff = run_A3(x4, mem, f(x_norm)[1], f(m_norm)[1], f(x_wq)[1], f(x_wkv)[1], f(x_wo)[1], f(f_norm)[1], f(f_router)[1])
    parts = run_M(hf, aff, f(f_w_gate)[1], f(f_w_up)[1], f(f_w_down)[1])
    o = run_F(x5, parts, f(final_norm))
    out = np.zeros((NB, S, D), np.float32)
    for c in range(8):
        out[c // 4, (c % 4) * TOK:(c % 4 + 1) * TOK] = np.asarray(o[c])
    return out
```

```python
import ml_dtypes
import numpy as np
from contextlib import ExitStack
import concourse.bass as bass
import concourse.mybir as mybir
from concourse.bass_utils import run_bass_kernel_spmd

F32 = mybir.dt.float32
BF16 = mybir.dt.bfloat16
I32 = mybir.dt.int32
AF = mybir.ActivationFunctionType
ALU = mybir.AluOpType
AX = mybir.AxisListType

ENGS = ("pe", "act", "dve", "pool", "sp")
NPOOL = 6


class Op:
    __slots__ = ("eng", "fn", "idx", "deps", "dma", "dsem", "dval", "gidx")


class Prog:
    def __init__(self):
        self.nc = bass.Bass("TRN2", target_bir_lowering=False)
        self.stack = ExitStack()
        self.ops = {e: [] for e in ENGS}
        self.lastw = {}
        self.readers = {}
        self.n = 0
        self.out_dmas = []
        self.dma_count = {e: 0 for e in ENGS}
        self.same_engine_sync = True
        self._uid = 0

    def dram(self, name, shape, dt, kind="ExternalInput"):
        return self.nc.dram_tensor(name, list(shape), dt, kind=kind).ap()

    def sb(self, shape, dt, name=None):
        self._uid += 1
        name = name or f"sb{self._uid}"
        return self.stack.enter_context(self.nc.sbuf_tensor(name, list(shape), dt))

    def ps(self, shape, dt=F32, name=None):
        self._uid += 1
        name = name or f"ps{self._uid}"
        return self.stack.enter_context(self.nc.psum_tensor(name, list(shape), dt))

    def op(self, eng, fn, r=(), w=(), dma=False, out=False):
        o = Op()
        o.eng = eng
        o.fn = fn
        o.dma = dma
        o.idx = len(self.ops[eng])
        o.gidx = self.n
        self.n += 1
        deps = set()
        for k in r:
            lw = self.lastw.get(k)
            if lw is not None:
                deps.add(lw)
        for k in w:
            lw = self.lastw.get(k)
            if lw is not None:
                deps.add(lw)
            for rd in self.readers.get(k, ()):
                deps.add(rd)
        for k in r:
            self.readers.setdefault(k, []).append(o)
        for k in w:
            self.lastw[k] = o
            self.readers[k] = []
        deps.discard(o)
        o.deps = deps
        self.ops[eng].append(o)
        if out:
            self.out_dmas.append(o)
        return o

    def dma(self, eng, out, in_, r=(), w=(), is_out=False, **kw):
        return self.op(eng, lambda e: e.dma_start(out=out, in_=in_, **kw), r, w, dma=True, out=is_out)

    def mm(self, out, lhsT, rhs, start=True, stop=True, r=(), w=()):
        return self.op("pe", lambda e: e.matmul(out, lhsT, rhs, start=start, stop=stop), r, w)

    def tr(self, out, in_, ident, r=(), w=()):
        return self.op("pe", lambda e: e.transpose(out, in_, ident), r, w)

    def act(self, out, in_, func, r=(), w=(), eng="act", **kw):
        return self.op(eng, lambda e: e.activation(out=out, in_=in_, func=func, **kw), r, w)

    def tt(self, eng, out, in0, in1, op, r=(), w=()):
        return self.op(eng, lambda e: e.tensor_tensor(out=out, in0=in0, in1=in1, op=op), r, w)

    def ts(self, eng, out, in0, s1, s2, op0, op1=None, r=(), w=(), **kw):
        if op1 is None:
            return self.op(eng, lambda e: e.tensor_scalar(out=out, in0=in0, scalar1=s1, scalar2=None, op0=op0, **kw), r, w)
        return self.op(eng, lambda e: e.tensor_scalar(out=out, in0=in0, scalar1=s1, scalar2=s2, op0=op0, op1=op1, **kw), r, w)

    def stt(self, eng, out, in0, scalar, in1, op0, op1, r=(), w=(), **kw):
        return self.op(eng, lambda e: e.scalar_tensor_tensor(out=out, in0=in0, scalar=scalar, in1=in1, op0=op0, op1=op1, **kw), r, w)

    def copy(self, eng, out, in_, r=(), w=()):
        if eng == "act":
            return self.op(eng, lambda e: e.copy(out=out, in_=in_), r, w)
        return self.op(eng, lambda e: e.tensor_copy(out=out, in_=in_), r, w)

    def memset(self, eng, ap, val, w=()):
        return self.op(eng, lambda e: e.memset(ap, val), (), w)

    def build(self):
        nc = self.nc
        st = self.stack
        csem = {e: st.enter_context(nc.semaphore(f"c_{e}")) for e in ENGS}
        dsem = {e: [st.enter_context(nc.semaphore(f"d_{e}{i}")) for i in range(NPOOL)] for e in ("sp", "act", "pool")}
        duse = {e: [0] * NPOOL for e in dsem}
        for e in dsem:
            k = 0
            for o in self.ops[e]:
                if o.dma:
                    i = k % NPOOL
                    duse[e][i] += 1
                    o.dsem = (e, i)
                    o.dval = 16 * duse[e][i]
                    k += 1
        ccount = {}
        for e in ENGS:
            c = 0
            for o in self.ops[e]:
                if not o.dma:
                    c += 1
                    o.dval = c
                    o.dsem = None
        out_dmas = self.out_dmas
        prog = self

        def emit(ename, eng):
            seen = {}

            def wait(key, sem, val):
                if seen.get(key, 0) >= val:
                    return
                eng.wait_ge(sem, val)
                seen[key] = val

            for o in prog.ops[ename]:
                for d in sorted(o.deps, key=lambda x: x.gidx):
                    if d.dma:
                        wait(("d",) + d.dsem, dsem[d.dsem[0]][d.dsem[1]], d.dval)
                    else:
                        if d.eng == ename and not prog.same_engine_sync:
                            continue
                        if d.eng == ename and ename == "pe":
                            continue
                        wait(("c", d.eng), csem[d.eng], d.dval)
                if o.dma:
                    if o.dval > 16:
                        wait(("d",) + o.dsem, dsem[o.dsem[0]][o.dsem[1]], o.dval - 16)
                    ins = o.fn(eng)
                    ins.then_inc(dsem[o.dsem[0]][o.dsem[1]], 16)
                else:
                    ins = o.fn(eng)
                    ins.then_inc(csem[ename], 1)
            if ename == "sp":
                for d in out_dmas:
                    wait(("d",) + d.dsem, dsem[d.dsem[0]][d.dsem[1]], d.dval)

        with nc.Block() as block:
            @block.sync
            def _(e):
                emit("sp", e)

            @block.scalar
            def _(e):
                emit("act", e)

            @block.vector
            def _(e):
                emit("dve", e)

            @block.gpsimd
            def _(e):
                emit("pool", e)

            @block.tensor
            def _(e):
                emit("pe", e)
        self.stack.close()
        return nc


def run(nc, in_maps, trace=False):
    res = run_bass_kernel_spmd(nc, in_maps, core_ids=list(range(len(in_maps))), trace=trace)
    return res


D = 1024
S = 8192
NB = 2
TOK = 2048
NT = TOK // 128
NPBF = ml_dtypes.bfloat16
_cache = {}


def _ident_np():
    return np.eye(128, dtype=np.float32)


def rmsnorm_tile(P, xb, xkey, gb, h, hkey, scr, eps=1e-6, d=1024):
    sq, ss, rstd = scr["sq"], scr["ss"], scr["rstd"]
    P.act(sq[:, :d], xb, AF.Square, r=[xkey], w=["sq", "ss"], accum_out=ss[:])
    P.ts("dve", rstd[:], ss[:], 1.0 / d, eps, ALU.mult, ALU.add, r=["ss"], w=["rstd"])
    P.act(rstd[:], rstd[:], AF.Sqrt, r=["rstd"], w=["rstd"])
    P.op("dve", lambda e: e.reciprocal(out=rstd[:], in_=rstd[:]), ["rstd"], ["rstd"])
    P.stt("dve", h, xb, rstd[:], gb, ALU.mult, ALU.mult, r=[xkey, "rstd", "gb"], w=[hkey])


def norm_scratch(P):
    return {"sq": P.sb([128, 1024], F32), "ss": P.sb([128, 1], F32), "rstd": P.sb([128, 1], F32)}


def build_A1(var=0):
    P = Prog()
    x = P.dram("x", [TOK, D], F32)
    g = P.dram("g", [1, D], F32)
    wqk_d = P.dram("wqk", [D, 1664], F32)
    wsw_d = P.dram("wsw", [D, 1664], F32)
    wv_d = P.dram("wv", [D, 640], F32)
    cos_d = P.dram("cos", [128, TOK], F32)
    sin_d = P.dram("sin", [128, TOK], F32)
    ident_d = P.dram("ident", [128, 128], F32)
    qk_o = P.dram("qk_o", [13, 128, TOK], BF16, kind="ExternalOutput")
    v_o = P.dram("v_o", [TOK, 640], BF16, kind="ExternalOutput")

    gb = P.sb([128, D], F32)
    wqk = P.sb([128, 8, 1664], BF16)
    wsw = P.sb([128, 8, 1664], BF16)
    wv = P.sb([128, 8, 640], BF16)
    cos = P.sb([128, TOK], F32)
    sin = P.sb([128, TOK], F32)
    identf = P.sb([128, 128], F32)
    ident = P.sb([128, 128], BF16)
    scr = norm_scratch(P)
    xt = [P.sb([128, D], F32) for _ in range(2)]
    h = P.sb([128, D], BF16)
    hT4 = [P.sb([128, 8, 512], BF16) for _ in range(2)]
    ptr = [P.ps([128, 8, 128], BF16) for _ in range(2)]
    ps1 = [P.ps([128, 512], F32) for _ in range(2)]
    ps2 = [P.ps([128, 512], F32) for _ in range(2)]
    psv = P.ps([128, 512], F32)
    psv2 = P.ps([128, 128], F32)
    t1 = [P.sb([128, 512], F32) for _ in range(2)]
    t2 = [P.sb([128, 512], F32) for _ in range(2)]
    ro = [P.sb([128, 512], BF16) for _ in range(2)]
    vo = [P.sb([128, 640], BF16) for _ in range(2)]

    P.dma("sp", gb[:], g.partition_broadcast(128), w=["gb"])
    P.dma("sp", identf[:], ident_d, w=["identf"])
    P.copy("dve", ident[:], identf[:], r=["identf"], w=["ident"])
    for k in range(8):
        P.dma("pool", wqk[:, k, :], wqk_d[k * 128:(k + 1) * 128, :], w=[("wqk", k)])
        P.dma("pool", wsw[:, k, :], wsw_d[k * 128:(k + 1) * 128, :], w=[("wsw", k)])
        P.dma("pool", wv[:, k, :], wv_d[k * 128:(k + 1) * 128, :], w=[("wv", k)])
    P.dma("sp", cos[:], cos_d, w=["cos"])
    P.dma("sp", sin[:], sin_d, w=["sin"])
    nblk = 0
    for grp in range(NT // 4):
        hb = hT4[grp % 2]
        hk = ("hT4", grp % 2)
        for j in range(4):
            t = grp * 4 + j
            xb = xt[t % 2]
            P.dma("sp", xb[:], x[t * 128:(t + 1) * 128, :], w=[("x", t % 2)])
            rmsnorm_tile(P, xb[:], ("x", t % 2), gb[:], h[:], "h", scr)
            pt = ptr[t % 2]
            for k in range(8):
                P.tr(pt[:, k, :], h[:, k * 128:(k + 1) * 128], ident[:], r=["h", "ident"], w=[("ptr", t % 2)])
            P.copy("act", hb[:, :, j * 128:(j + 1) * 128], pt[:], r=[("ptr", t % 2)], w=[hk + (j,)])
        hkeys = [hk + (j,) for j in range(4)]
        for cb in range(13):
            i = nblk % 2
            nblk += 1
            for k in range(8):
                P.mm(ps1[i][:], wqk[:, k, cb * 128:(cb + 1) * 128], hb[:, k, :], start=(k == 0), stop=(k == 7),
                     r=hkeys + [("wqk", k)], w=[("ps1", i)])
            for k in range(8):
                P.mm(ps2[i][:], wsw[:, k, cb * 128:(cb + 1) * 128], hb[:, k, :], start=(k == 0), stop=(k == 7),
                     r=hkeys + [("wsw", k)], w=[("ps2", i)])
            cs = slice(grp * 512, (grp + 1) * 512)
            P.tt("dve", t1[i][:], ps1[i][:], cos[:, cs], ALU.mult, r=[("ps1", i), "cos"], w=[("t1", i)])
            P.tt("dve", t2[i][:], ps2[i][:], sin[:, cs], ALU.mult, r=[("ps2", i), "sin"], w=[("t2", i)])
            P.tt("dve" if var == 1 else "pool", ro[i][:], t1[i][:], t2[i][:], ALU.add, r=[("t1", i), ("t2", i)], w=[("ro", i)])
            P.dma("sp", qk_o[cb, :, cs], ro[i][:], r=[("ro", i)], w=[("qk_o", cb, grp)], is_out=True)
        for j in range(4):
            t = grp * 4 + j
            i = t % 2
            for k in range(8):
                P.mm(psv[:], hb[:, k, j * 128:(j + 1) * 128], wv[:, k, 0:512], start=(k == 0), stop=(k == 7),
                     r=hkeys + [("wv", k)], w=["psv"])
            for k in range(8):
                P.mm(psv2[:], hb[:, k, j * 128:(j + 1) * 128], wv[:, k, 512:640], start=(k == 0), stop=(k == 7),
                     r=hkeys + [("wv", k)], w=["psv2"])
            P.copy("act", vo[i][:, 0:512], psv[:], r=["psv"], w=[("vo", i, 0)])
            P.copy("act", vo[i][:, 512:640], psv2[:], r=["psv2"], w=[("vo", i, 1)])
            P.dma("sp", v_o[t * 128:(t + 1) * 128, :], vo[i][:], r=[("vo", i, 0), ("vo", i, 1)], w=[("v_o", t)], is_out=True)
    return P.build()


def rope_tables_np(pos):
    inv = (1.0 / (np.float32(10000.0) ** (np.arange(0, 64, 2, dtype=np.float32) / np.float32(64)))).astype(np.float32)
    ang = pos.astype(np.float32)[:, None] * inv[None, :]
    ang = np.concatenate([ang, ang], axis=-1)
    c = np.cos(ang).astype(np.float32)
    s = np.sin(ang).astype(np.float32)
    s[:, :32] *= -1.0
    cT = np.concatenate([c.T, c.T], axis=0)
    sT = np.concatenate([s.T, s.T], axis=0)
    return np.ascontiguousarray(cT), np.ascontiguousarray(sT)


QK_COLS = np.concatenate([np.arange(0, 512), np.arange(512, 1024), np.arange(1536, 2048), np.arange(2048, 2176)])
V_COLS = np.concatenate([np.arange(1024, 1536), np.arange(2176, 2304)])


def _swap_halves_cols(n):
    idx = np.arange(n)
    hd = idx // 64
    d = idx % 64
    return hd * 64 + (d + 32) % 64


def run_A1(x, e_norm, e_w_in):
    if "A1" not in _cache:
        _cache["A1"] = build_A1()
    nc = _cache["A1"]
    wqk = np.ascontiguousarray(e_w_in[:, QK_COLS])
    wsw = np.ascontiguousarray(wqk[:, _swap_halves_cols(1664)])
    wv = np.ascontiguousarray(e_w_in[:, V_COLS])
    maps = []
    for c in range(8):
        b, q = c // 4, c % 4
        cT, sT = rope_tables_np(np.arange(q * TOK, (q + 1) * TOK))
        maps.append({"x": np.ascontiguousarray(x[b, q * TOK:(q + 1) * TOK]), "g": e_norm.reshape(1, D), "wqk": wqk, "wsw": wsw,
                     "wv": wv, "cos": cT, "sin": sT, "ident": _ident_np()})
    res = run(nc, maps)
    qk = [res.results[c]["qk_o"] for c in range(8)]
    v = [res.results[c]["v_o"] for c in range(8)]
    return qk, v


NKA = 32
NKB = 18


def build_A2(var=0):
    P = Prog()
    ACTQ = "sp" if var & 1 else "act"
    POOLE = "dve" if var & 4 else "pool"
    qa_d = P.dram("qa", [4, 128, TOK], BF16)
    qb_d = P.dram("qb", [4, 128, TOK], BF16)
    ka_d = P.dram("ka", [4, 128, NKA * 128], BF16)
    kb_d = P.dram("kb", [2, 128, NKB * 128], BF16)
    va_d = P.dram("va", [NKA * 128, 8 * 65], BF16)
    vb_d = P.dram("vb", [NKB * 128, 2 * 65], BF16)
    kva_d = P.dram("kva", [128, NKA], F32)
    kvb_d = P.dram("kvb", [128, NKB], F32)
    ma_d = P.dram("ma", [128, 17, 128], BF16)
    mb_d = P.dram("mb", [128, 3, 128], BF16)
    x_d = P.dram("x", [TOK, D], F32)
    wo_d = P.dram("wo", [D, D], F32)
    sink_d = P.dram("sink", [1, 8], F32)
    ident_d = P.dram("ident", [128, 128], F32)
    x1_o = P.dram("x1_o", [TOK, D], F32, kind="ExternalOutput")

    qa = P.sb([128, 4, TOK], BF16)
    qb = P.sb([128, 4, TOK], BF16)
    ka = P.sb([128, 4, NKA * 128], BF16)
    kb = P.sb([128, 2, NKB * 128], BF16)
    va = P.sb([128, NKA, 8 * 65], BF16)
    vb = P.sb([128, NKB, 2 * 65], BF16)
    kva = P.sb([128, NKA], F32)
    kvb = P.sb([128, NKB], F32)
    ma = P.sb([128, 17, 128], BF16)
    mb = P.sb([128, 3, 128], BF16)
    wo = P.sb([128, 8, D], BF16)
    sink = P.sb([128, 8], F32)
    esink = P.sb([128, 8], F32)
    identf = P.sb([128, 128], F32)
    ident = P.sb([128, 128], BF16)
    xt = [P.sb([128, D], F32) for _ in range(2)]
    y = P.sb([128, D], BF16)
    yT = P.sb([128, 8, 128], BF16)
    pT = [P.sb([128, 4, 128], BF16) for _ in range(3)]
    pm = [P.sb([128, 4, 128], BF16) for _ in range(3)]
    den = P.sb([128, 4], F32)
    rden = P.sb([128, 4], F32)
    xo = [P.sb([128, D], F32) for _ in range(2)]
    ps_s = [P.ps([128, 4, 128], F32) for _ in range(2)]
    ps_o = [P.ps([128, 512], F32) for _ in range(4)]
    ptr = P.ps([128, 8, 128], BF16)
    ps_out1 = P.ps([128, 512], F32)
    ps_out = [ps_out1, ps_out1]

    P.dma("sp", identf[:], ident_d, w=["identf"])
    P.copy("dve", ident[:], identf[:], r=["identf"], w=["ident"])
    P.dma("sp", sink[:], sink_d.partition_broadcast(128), w=["sink"])
    P.act(esink[:], sink[:], AF.Exp, r=["sink"], w=["esink"])
    P.dma("sp", kva[:], kva_d, w=["kva"])
    P.dma("sp", kvb[:], kvb_d, w=["kvb"])
    P.dma("sp", ma[:], ma_d, w=["ma"])
    P.dma("sp", mb[:], mb_d, w=["mb"])
    for i in range(4):
        P.dma("sp", qa[:, i, :], qa_d[i], w=[("qa", i)])
        P.dma(ACTQ, qb[:, i, :], qb_d[i], w=[("qb", i)])
        if var & 2:
            for j in range(4):
                P.dma("sp", ka[:, i, j * 1024:(j + 1) * 1024], ka_d[i, :, j * 1024:(j + 1) * 1024], w=[("ka", i)])
        else:
            P.dma("sp", ka[:, i, :], ka_d[i], w=[("ka", i)])
    for i in range(2):
        P.dma(ACTQ, kb[:, i, :], kb_d[i], w=[("kb", i)])
    for kt in range(NKA):
        P.dma("sp" if kt % 2 else ACTQ, va[:, kt, :], va_d[kt * 128:(kt + 1) * 128, :], w=[("va", kt)])
    for kt in range(NKB):
        P.dma("sp" if kt % 2 else ACTQ, vb[:, kt, :], vb_d[kt * 128:(kt + 1) * 128, :], w=[("vb", kt)])
    for k in range(8):
        P.dma("pool", wo[:, k, :], wo_d[k * 128:(k + 1) * 128, :], w=[("wo", k)])

    cnt = 0
    for qt in range(NT):
        qs = slice(qt * 128, (qt + 1) * 128)
        xb = xt[qt % 2]
        P.dma("sp", xb[:], x_d[qs, :], w=[("x", qt % 2)])
        its = []
        for part in ("A", "B"):
            deltas = list(range(-8, 9)) if part == "A" else list(range(-1, 2))
            for g in range(2):
                for di, dl in enumerate(deltas):
                    its.append((part, g, di, len(deltas), dl))

        def emit_qk(it, idx):
            part, g, di, nd, dl = it
            i2 = idx % 2
            i3 = idx % 3
            kt = qt + 8 + dl if part == "A" else qt + 1 + dl
            ks = slice(kt * 128, (kt + 1) * 128)
            for hh in range(4):
                hd = 2 * hh + g
                pr, pb = hh, g * 64
                if part == "A":
                    P.mm(ps_s[i2][:, hh, :], ka[pb:pb + 64, pr, ks], qa[pb:pb + 64, pr, qs],
                         r=[("ka", pr), ("qa", pr)], w=[("ps_s", i2)])
                else:
                    P.mm(ps_s[i2][:, hh, :], kb[pb:pb + 64, hd // 4, ks], qb[pb:pb + 64, pr, qs],
                         r=[("kb", hd // 4), ("qb", pr)], w=[("ps_s", i2)])
            P.act(pT[i3][:], ps_s[i2][:], AF.Exp, r=[("ps_s", i2)], w=[("pT", i3)], scale=0.125)
            if part == "A":
                msk = ma[:, dl + 8, :].unsqueeze(1).broadcast_to([128, 4, 128])
                mkeys = ["ma"]
            else:
                msk = mb[:, dl + 1, :].unsqueeze(1).broadcast_to([128, 4, 128])
                mkeys = ["mb"]
            P.tt("dve" if idx % 2 else POOLE, pm[i3][:], pT[i3][:], msk, ALU.mult,
                 r=[("pT", i3)] + mkeys, w=[("pm", i3)])

        def emit_pv(it, idx):
            part, g, di, nd, dl = it
            i3 = idx % 3
            kt = qt + 8 + dl if part == "A" else qt + 1 + dl
            for hh in range(4):
                hd = 2 * hh + g
                if part == "A":
                    rhs = va[:, kt, hd * 65:(hd + 1) * 65]
                    rk = ("va", kt)
                else:
                    rhs = vb[:, kt, (hd // 4) * 65:(hd // 4 + 1) * 65]
                    rk = ("vb", kt)
                P.mm(ps_o[hh][:, 0:65], pm[i3][:, hh, :], rhs, start=(di == 0), stop=(di == nd - 1),
                     r=[("pm", i3), rk], w=[("ps_o", hh)])
            if di == nd - 1:
                base = (0 if part == "A" else 512)
                for hh in range(4):
                    hd = 2 * hh + g
                    pok = ("ps_o", hh)
                    if part == "A":
                        P.copy("dve", den[:, hh:hh + 1], ps_o[hh][:, 64:65], r=[pok], w=[("den", hh)])
                    else:
                        P.tt("dve", den[:, hh:hh + 1], ps_o[hh][:, 64:65], esink[:, hd:hd + 1], ALU.add,
                             r=[pok, "esink"], w=[("den", hh)])
                    P.op("dve", lambda e, hh=hh: e.reciprocal(out=rden[:, hh:hh + 1], in_=den[:, hh:hh + 1]), [("den", hh)], [("rden", hh)])
                    P.ts("dve", y[:, base + hd * 64:base + (hd + 1) * 64], ps_o[hh][:, 0:64], rden[:, hh:hh + 1], None, ALU.mult,
                         r=[pok, ("rden", hh)], w=[("y", part, g, hh)])

        emit_qk(its[0], cnt)
        for n_, it in enumerate(its):
            if n_ + 1 < len(its):
                emit_qk(its[n_ + 1], cnt + n_ + 1)
            emit_pv(it, cnt + n_)
        cnt += len(its)
        ykeys = [("y", p_, g_, h_) for p_ in "AB" for g_ in range(2) for h_ in range(4)]
        for k in range(8):
            P.tr(ptr[:, k, :], y[:, k * 128:(k + 1) * 128], ident[:], r=ykeys + ["ident"], w=["ptr"])
        P.copy("act", yT[:], ptr[:], r=["ptr"], w=["yT"])
        for hf in range(2):
            for k in range(8):
                P.mm(ps_out[hf][:], yT[:, k, :], wo[:, k, hf * 512:(hf + 1) * 512], start=(k == 0), stop=(k == 7),
                     r=["yT", ("wo", k)], w=[("ps_out", 0)])
            P.tt("dve", xo[qt % 2][:, hf * 512:(hf + 1) * 512], ps_out[hf][:], xb[:, hf * 512:(hf + 1) * 512], ALU.add,
                 r=[("ps_out", 0), ("x", qt % 2)], w=[("xo", qt % 2, hf)])
        P.dma("sp", x1_o[qs, :], xo[qt % 2][:], r=[("xo", qt % 2, 0), ("xo", qt % 2, 1)], w=[("x1_o", qt)], is_out=True)
    return P.build()


def _masks_np():
    ql = np.arange(128)[None, :]
    kl = np.arange(128)[:, None]
    ma = np.zeros((128, 17, 128), np.float32)
    for dl in range(-8, 9):
        diff = ql - kl - 128 * dl
        ad = np.abs(diff)
        ma[:, dl + 8, :] = (ad <= 64).astype(np.float32) + ((ad <= 256) & (diff % 4 == 0)) + ((ad <= 1024) & (diff % 16 == 0))
    mb = np.zeros((128, 3, 128), np.float32)
    for dl in range(-1, 2):
        diff = ql - kl - 128 * dl
        mb[:, dl + 1, :] = (np.abs(diff) <= 128)
    return ma.astype(NPBF), mb.astype(NPBF)


def _halo(arr_list, b, q, axis, lo, hi, zero_shape_fn):
    full = np.concatenate([arr_list[b * 4 + i] for i in range(4)], axis=axis)
    padw = [(0, 0)] * full.ndim
    padw[axis] = (lo, hi)
    full = np.pad(full, padw)
    sl = [slice(None)] * full.ndim
    sl[axis] = slice(q * TOK, (q + 1) * TOK + lo + hi)
    return np.ascontiguousarray(full[tuple(sl)])


def run_A2(qk, v, x, e_sink, e_w_out, var=0):
    if ("A2", var) not in _cache:
        _cache[("A2", var)] = build_A2(var)
    nc = _cache[("A2", var)]
    ma, mb = _masks_np()
    ones = np.ones((TOK, 1), NPBF)
    vaug_a, vaug_b, ka_l, kb_l = [], [], [], []
    for c in range(8):
        vv = np.asarray(v[c])
        va = np.concatenate([np.concatenate([vv[:, h * 64:(h + 1) * 64], ones], 1) for h in range(8)], 1)
        vb = np.concatenate([np.concatenate([vv[:, 512 + h * 64:512 + (h + 1) * 64], ones], 1) for h in range(2)], 1)
        vaug_a.append(va)
        vaug_b.append(vb)
        qq = np.asarray(qk[c])
        ka_l.append(qq[4:8])
        kbt = qq[12]
        kb_l.append(np.stack([np.concatenate([kbt[0:64], kbt[0:64]], 0), np.concatenate([kbt[64:128], kbt[64:128]], 0)], 0))
    maps = []
    for c in range(8):
        b, q = c // 4, c % 4
        qq = np.asarray(qk[c])
        pos_a = (q * TOK - 1024) + np.arange(NKA) * 128
        pos_b = (q * TOK - 128) + np.arange(NKB) * 128
        kva = np.broadcast_to(((pos_a >= 0) & (pos_a < S)).astype(np.float32)[None, :], (128, NKA))
        kvb = np.broadcast_to(((pos_b >= 0) & (pos_b < S)).astype(np.float32)[None, :], (128, NKB))
        maps.append({
            "qa": np.ascontiguousarray(qq[0:4]), "qb": np.ascontiguousarray(qq[8:12]),
            "ka": _halo(ka_l, b, q, 2, 1024, 1024, None), "kb": _halo(kb_l, b, q, 2, 128, 128, None),
            "va": _halo(vaug_a, b, q, 0, 1024, 1024, None), "vb": _halo(vaug_b, b, q, 0, 128, 128, None),
            "kva": np.ascontiguousarray(kva), "kvb": np.ascontiguousarray(kvb), "ma": ma, "mb": mb,
            "x": np.ascontiguousarray(x[b, q * TOK:(q + 1) * TOK]), "wo": e_w_out, "sink": e_sink.reshape(1, 8),
            "ident": _ident_np()})
    res = run(nc, maps)
    return [res.results[c]["x1_o"] for c in range(8)]


def build_A3():
    P = Prog()
    x_d = P.dram("x", [TOK, D], F32)
    mem_d = P.dram("mem", [256, D], F32)
    xn_d = P.dram("xn", [1, D], F32)
    mn_d = P.dram("mn", [1, D], F32)
    fn_d = P.dram("fn", [1, D], F32)
    wq_d = P.dram("wq", [D, D], F32)
    wkv_d = P.dram("wkv", [D, 2 * D], F32)
    wo_d = P.dram("wo", [D, D], F32)
    wr_d = P.dram("wr", [D, 16], F32)
    ident_d = P.dram("ident", [128, 128], F32)
    x2_o = P.dram("x2_o", [TOK, D], F32, kind="ExternalOutput")
    hf_o = P.dram("hf_o", [TOK, D], BF16, kind="ExternalOutput")
    aff_o = P.dram("aff_o", [TOK, 16], F32, kind="ExternalOutput")

    gx = P.sb([128, D], F32)
    gm = P.sb([128, D], F32)
    gf = P.sb([128, D], F32)
    wq = P.sb([128, 8, D], BF16)
    wkv = P.sb([128, 8, 2 * D], BF16)
    wo = P.sb([128, 8, D], BF16)
    wr = P.sb([128, 8, 16], F32)
    identf = P.sb([128, 128], F32)
    ident = P.sb([128, 128], BF16)
    scr = norm_scratch(P)
    mt_ = P.sb([128, D], F32)
    h = P.sb([128, D], BF16)
    memT = P.sb([128, 8, 256], BF16)
    KT = P.sb([128, 8, 256], BF16)
    Vaug = P.sb([128, 2, 4 * 257], BF16)
    xt4 = P.sb([128, 4, D], F32)
    hT4 = P.sb([128, 8, 512], BF16)
    QT = P.sb([128, 8, 512], BF16)
    pT = [P.sb([128, 4, 128], BF16) for _ in range(2)]
    o = P.sb([128, D], BF16)
    oT = P.sb([128, 8, 128], BF16)
    x2 = [P.sb([128, D], F32) for _ in range(2)]
    hf = P.sb([128, D], F32)
    hfb = [P.sb([128, D], BF16) for _ in range(2)]
    hfT = P.sb([128, 8, 128], F32)
    den = P.sb([128, 4], F32)
    rden = P.sb([128, 4], F32)
    lg = P.sb([128, 16], F32)
    mx = P.sb([128, 1], F32)
    ex = P.sb([128, 16], F32)
    sm = P.sb([128, 1], F32)
    aff = [P.sb([128, 16], F32) for _ in range(2)]
    ps_s = [P.ps([128, 4, 128], F32) for _ in range(2)]
    ps_o = [P.ps([128, 512], F32) for _ in range(4)]
    ptr = P.ps([128, 8, 128], BF16)
    psA = P.ps([128, 512], F32)

    P.dma("sp", identf[:], ident_d, w=["identf"])
    P.copy("dve", ident[:], identf[:], r=["identf"], w=["ident"])
    P.dma("sp", gx[:], xn_d.partition_broadcast(128), w=["gx"])
    P.dma("sp", gm[:], mn_d.partition_broadcast(128), w=["gm"])
    P.dma("sp", gf[:], fn_d.partition_broadcast(128), w=["gf"])
    P.dma("sp", wr[:], wr_d.rearrange("(k p) n -> p k n", p=128), w=["wr"])
    for k in range(8):
        P.dma("pool", wkv[:, k, 0:1024], wkv_d[k * 128:(k + 1) * 128, 0:1024], w=[("wkv", k)])
        P.dma("pool", wkv[:, k, 1024:2048], wkv_d[k * 128:(k + 1) * 128, 1024:2048], w=[("wkv", k)])
    for k in range(8):
        P.dma("pool", wq[:, k, :], wq_d[k * 128:(k + 1) * 128, :], w=[("wq", k)])
    for k in range(8):
        P.dma("pool", wo[:, k, :], wo_d[k * 128:(k + 1) * 128, :], w=[("wo", k)])
    wkvk = [("wkv", k) for k in range(8)]
    P.memset("dve", Vaug[:], 1.0, w=["Vaug"])
    for m in range(2):
        P.dma("sp", mt_[:], mem_d[m * 128:(m + 1) * 128, :], w=["mt"])
        rmsnorm_tile(P, mt_[:], "mt", gm[:], h[:], "h", scr)
        for k in range(8):
            P.tr(ptr[:, k, :], h[:, k * 128:(k + 1) * 128], ident[:], r=["h", "ident"], w=["ptr"])
        P.copy("act", memT[:, :, m * 128:(m + 1) * 128], ptr[:], r=["ptr"], w=[("memT", m)])
    mk = [("memT", 0), ("memT", 1)]
    for c in range(8):
        for k in range(8):
            P.mm(psA[:, 0:256], wkv[:, k, c * 128:(c + 1) * 128], memT[:, k, :], start=(k == 0), stop=(k == 7),
                 r=mk + wkvk, w=["psA"])
        P.copy("act", KT[:, c, :], psA[:, 0:256], r=["psA"], w=["KT"])
    for m in range(2):
        for hf_ in range(2):
            for k in range(8):
                P.mm(psA[:], memT[:, k, m * 128:(m + 1) * 128], wkv[:, k, 1024 + hf_ * 512:1024 + (hf_ + 1) * 512],
                     start=(k == 0), stop=(k == 7), r=mk + wkvk, w=["psA"])
            for hh in range(2):
                hd = hf_ * 2 + hh
                P.copy("act", Vaug[:, m, hd * 257:hd * 257 + 256], psA[:, hh * 256:(hh + 1) * 256], r=["psA"], w=["Vaug"])
    wqk = [("wq", k) for k in range(8)]
    wok = [("wo", k) for k in range(8)]
    for grp in range(NT // 4):
        for j in range(4):
            t = grp * 4 + j
            P.dma("sp", xt4[:, j, :], x_d[t * 128:(t + 1) * 128, :], w=[("x", j)])
            rmsnorm_tile(P, xt4[:, j, :], ("x", j), gx[:], h[:], "h", scr)
            for k in range(8):
                P.tr(ptr[:, k, :], h[:, k * 128:(k + 1) * 128], ident[:], r=["h", "ident"], w=["ptr"])
            P.copy("act", hT4[:, :, j * 128:(j + 1) * 128], ptr[:], r=["ptr"], w=[("hT4", j)])
        hk = [("hT4", j) for j in range(4)]
        for c in range(8):
            for k in range(8):
                P.mm(psA[:], wq[:, k, c * 128:(c + 1) * 128], hT4[:, k, :], start=(k == 0), stop=(k == 7), r=hk + wqk, w=["psA"])
            P.copy("dve" if c % 2 else "act", QT[:, c, :], psA[:], r=["psA"], w=[("QT", c)])
        qk_ = [("QT", c) for c in range(8)]
        for j in range(4):
            t = grp * 4 + j
            js = slice(j * 128, (j + 1) * 128)
            for m in range(2):
                for hd in range(4):
                    for half in range(2):
                        P.mm(ps_s[m][:, hd, :], KT[:, 2 * hd + half, m * 128:(m + 1) * 128], QT[:, 2 * hd + half, js],
                             start=(half == 0), stop=(half == 1), r=["KT"] + qk_, w=[("ps_s", m)])
                P.act(pT[m][:], ps_s[m][:], AF.Exp, r=[("ps_s", m)], w=[("pT", m)], scale=1.0 / 16)
            for hd in range(4):
                for m in range(2):
                    P.mm(ps_o[hd][:, 0:257], pT[m][:, hd, :], Vaug[:, m, hd * 257:(hd + 1) * 257], start=(m == 0), stop=(m == 1),
                         r=[("pT", m), "Vaug"], w=[("ps_o", hd)])
                P.op("dve", lambda e, hd=hd: e.reciprocal(out=rden[:, hd:hd + 1], in_=ps_o[hd][:, 256:257]), [("ps_o", hd)], [("rden", hd)])
                P.ts("dve", o[:, hd * 256:(hd + 1) * 256], ps_o[hd][:, 0:256], rden[:, hd:hd + 1], None, ALU.mult,
                     r=[("ps_o", hd), ("rden", hd)], w=[("o", hd)])
            ok_ = [("o", hd) for hd in range(4)]
            for k in range(8):
                P.tr(ptr[:, k, :], o[:, k * 128:(k + 1) * 128], ident[:], r=ok_ + ["ident"], w=["ptr"])
            P.copy("act", oT[:], ptr[:], r=["ptr"], w=["oT"])
            xb = x2[t % 2]
            for hf_ in range(2):
                for k in range(8):
                    P.mm(psA[:], oT[:, k, :], wo[:, k, hf_ * 512:(hf_ + 1) * 512], start=(k == 0), stop=(k == 7), r=["oT"] + wok, w=["psA"])
                P.tt("dve", xb[:, hf_ * 512:(hf_ + 1) * 512], psA[:], xt4[:, j, hf_ * 512:(hf_ + 1) * 512], ALU.add,
                     r=["psA", ("x", j)], w=[("x2", t % 2, hf_)])
            x2k = [("x2", t % 2, 0), ("x2", t % 2, 1)]
            P.dma("sp", x2_o[t * 128:(t + 1) * 128, :], xb[:], r=x2k, w=[("x2_o", t)], is_out=True)
            sq, ss, rstd = scr["sq"], scr["ss"], scr["rstd"]
            P.act(sq[:], xb[:], AF.Square, r=x2k, w=["sq", "ss"], accum_out=ss[:])
            P.ts("dve", rstd[:], ss[:], 1.0 / D, 1e-6, ALU.mult, ALU.add, r=["ss"], w=["rstd"])
            P.act(rstd[:], rstd[:], AF.Sqrt, r=["rstd"], w=["rstd"])
            P.op("dve", lambda e: e.reciprocal(out=rstd[:], in_=rstd[:]), ["rstd"], ["rstd"])
            P.stt("dve", hf[:], xb[:], rstd[:], gf[:], ALU.mult, ALU.mult, r=x2k + ["rstd", "gf"], w=["hf"])
            P.copy("pool", hfb[t % 2][:], hf[:], r=["hf"], w=[("hfb", t % 2)])
            P.dma("sp", hf_o[t * 128:(t + 1) * 128, :], hfb[t % 2][:], r=[("hfb", t % 2)], w=[("hf_o", t)], is_out=True)
            psAv = psA[:].rearrange("p (a b) -> p a b", b=128)
            for hf_ in range(2):
                for k in range(4):
                    kk = hf_ * 4 + k
                    P.tr(psAv[:, k, :], hf[:, kk * 128:(kk + 1) * 128], identf[:], r=["hf", "identf"], w=["psA"])
                P.copy("act", hfT[:, hf_ * 4:(hf_ + 1) * 4, :], psAv, r=["psA"], w=[("hfT", hf_)])
            for k in range(8):
                P.mm(psA[:, 0:16], hfT[:, k, :], wr[:, k, :], start=(k == 0), stop=(k == 7), r=[("hfT", 0), ("hfT", 1), "wr"], w=["psA"])
            P.copy("dve", lg[:], psA[:, 0:16], r=["psA"], w=["lg"])
            P.op("dve", lambda e: e.reduce_max(out=mx[:], in_=lg[:], axis=AX.X), ["lg"], ["mx"])
            P.ts("dve", mx[:], mx[:], -1.0, None, ALU.mult, r=["mx"], w=["mx"])
            P.act(ex[:], lg[:], AF.Exp, r=["lg", "mx"], w=["ex", "sm"], bias=mx[:], accum_out=sm[:])
            P.op("dve", lambda e: e.reciprocal(out=sm[:], in_=sm[:]), ["sm"], ["sm"])
            P.ts("dve", aff[t % 2][:], ex[:], sm[:], None, ALU.mult, r=["ex", "sm"], w=[("aff", t % 2)])
            P.dma("sp", aff_o[t * 128:(t + 1) * 128, :], aff[t % 2][:], r=[("aff", t % 2)], w=[("aff_o", t)], is_out=True)
    return P.build()


def run_A3(x1, mem, x_norm, m_norm, wq, wkv, wo, f_norm, f_router):
    if "A3" not in _cache:
        _cache["A3"] = build_A3()
    nc = _cache["A3"]
    maps = []
    for c in range(8):
        b = c // 4
        maps.append({"x": np.ascontiguousarray(x1[c]), "mem": np.ascontiguousarray(mem[b]), "xn": x_norm.reshape(1, D), "mn": m_norm.reshape(1, D),
                     "fn": f_norm.reshape(1, D), "wq": wq, "wkv": wkv, "wo": wo, "wr": f_router, "ident": _ident_np()})
    res = run(nc, maps)
    return ([res.results[c]["x2_o"] for c in range(8)], [res.results[c]["hf_o"] for c in range(8)],
            [res.results[c]["aff_o"] for c in range(8)])


CAP = 1024
FE = 2816
NFC = FE // 128


def build_M(nbis=32):
    P = Prog()
    aff_d = P.dram("aff", [128, 4, 64], F32)
    hf_d = P.dram("hf", [NB * S, D], BF16)
    wg_d = P.dram("wg", [2, D, FE], F32)
    wu_d = P.dram("wu", [2, D, FE], F32)
    wd_d = P.dram("wd", [2, FE, D], F32)
    ltri_d = P.dram("ltri", [128, 128], F32)
    iota_d = P.dram("iota", [128, 1024], F32)
    iotap_d = P.dram("iotap", [128, 8], F32)
    ident_d = P.dram("ident", [128, 128], F32)
    tokhl_d = P.dram("tokhl", [128, 2, 64], F32)
    out_d = P.dram("out", [NB, S, D], F32, kind="ExternalOutput")
    YR = CAP + 128
    ye_d = P.dram("ye_scr", [4 * YR, D], BF16, kind="ExternalOutput")

    affc = P.sb([128, 4, 64], F32)
    ltri = P.sb([128, 128], F32)
    iota = P.sb([128, 1024], F32)
    iotap = P.sb([128, 8], F32)
    identf = P.sb([128, 128], F32)
    onesf = P.sb([128, 128], F32)
    lo = P.sb([128, 4], F32)
    hi = P.sb([128, 4], F32)
    mid = P.sb([128, 4], F32)
    cnt = P.sb([128, 4], F32)
    sel = P.sb([128, 4], F32)
    d1 = P.sb([128, 4], F32)
    d2 = P.sb([128, 4], F32)
    cmp_ = P.sb([128, 4, 64], F32)
    msk = P.sb([128, 4, 64], F32)
    posf = P.sb([128, 4, 64], F32)
    tmpm = P.sb([128, 4, 64], F32)
    cs = P.sb([64, 1], F32)
    csb = P.sb([64, 128], F32)
    hft = [P.sb([128, D], BF16) for _ in range(2)]
    selt = [P.sb([128, 1024], BF16) for _ in range(3)]
    identb = P.sb([128, 128], BF16)
    tokhlf = P.sb([128, 2, 64], F32)
    tokhl = P.sb([128, 2, 64], BF16)
    idxrow = P.sb([1, 1024], F32)
    idxf = P.sb([128, 8], F32)
    idxi = P.sb([128, 8], I32)
    xeT = P.sb([128, 8, CAP], BF16)
    wgs = [P.sb([128, 8, 256], BF16) for _ in range(2)]
    wus = [P.sb([128, 8, 256], BF16) for _ in range(2)]
    wgf = P.sb([128, 8, 256], F32)
    sg = [P.sb([128, 512], BF16) for _ in range(2)]
    hidT = P.sb([128, NFC, CAP], BF16)
    wdq = [P.sb([128, NFC, 256], BF16) for _ in range(2)]
    wdf = P.sb([128, NFC // 2, 256], F32)
    ye1 = P.sb([128, 8, D], BF16)
    ye = [ye1, ye1]
    wuf = P.sb([128, 8, 256], F32)
    ztile = P.sb([128, D], BF16)
    idxsf = P.sb([128, 4, 64], F32)
    idxsi = P.sb([128, 256], I32)
    rt = [[P.sb([128, D], BF16) for _ in range(2)] for _ in range(2)]
    otile = [P.sb([128, D], F32) for _ in range(2)]
    B = [P.ps([128, 512], F32) for _ in range(8)]
    bk = lambda i: ("B", i)

    P.dma("sp", affc[:], aff_d, w=["affc"])
    P.dma("sp", ltri[:], ltri_d, w=["ltri"])
    P.dma("sp", iota[:], iota_d, w=["iota"])
    P.dma("sp", iotap[:], iotap_d, w=["iotap"])
    P.dma("sp", identf[:], ident_d, w=["identf"])
    P.memset("dve", onesf[:], 1.0, w=["onesf"])
    P.copy("dve", identb[:], identf[:], r=["identf"], w=["identb"])
    P.dma("sp", tokhlf[:], tokhl_d, w=["tokhlf"])
    P.copy("dve", tokhl[:], tokhlf[:], r=["tokhlf"], w=["tokhl"])
    P.memset("dve", lo[:], 0.0, w=["lo"])
    P.memset("dve", hi[:], 1.0, w=["hi"])
    for it in range(nbis):
        P.tt("dve", mid[:], lo[:], hi[:], ALU.add, r=["lo", "hi"], w=["mid"])
        P.ts("dve", mid[:], mid[:], 0.5, None, ALU.mult, r=["mid"], w=["mid"])
        P.tt("dve", cmp_[:], affc[:], mid[:].unsqueeze(2).broadcast_to([128, 4, 64]), ALU.is_ge, r=["affc", "mid"], w=["cmp"])
        P.op("dve", lambda e: e.tensor_reduce(out=cnt[:], in_=cmp_[:], axis=AX.X, op=ALU.add), ["cmp"], ["cnt"])
        P.mm(B[0][:, 0:4], onesf[:], cnt[:], r=["onesf", "cnt"], w=[bk(0)])
        P.ts("dve", sel[:], B[0][:, 0:4], float(CAP), None, ALU.is_ge, r=[bk(0)], w=["sel"])
        P.tt("dve", d1[:], mid[:], lo[:], ALU.subtract, r=["mid", "lo"], w=["d1"])
        P.tt("dve", d1[:], d1[:], sel[:], ALU.mult, r=["d1", "sel"], w=["d1"])
        P.tt("dve", d2[:], hi[:], mid[:], ALU.subtract, r=["hi", "mid"], w=["d2"])
        P.tt("dve", d2[:], d2[:], sel[:], ALU.mult, r=["d2", "sel"], w=["d2"])
        P.tt("dve", lo[:], lo[:], d1[:], ALU.add, r=["lo", "d1"], w=["lo"])
        P.tt("dve", hi[:], mid[:], d2[:], ALU.add, r=["mid", "d2"], w=["hi"])
    P.tt("dve", msk[:], affc[:], lo[:].unsqueeze(2).broadcast_to([128, 4, 64]), ALU.is_ge, r=["affc", "lo"], w=["msk"])
    for g in range(4):
        P.mm(B[1][0:64, 0:1], msk[:, g, :], onesf[:, 0:1], r=["msk", "onesf"], w=[bk(1)])
        P.copy("dve", cs[:], B[1][0:64, 0:1], r=[bk(1)], w=["cs"])
        P.ts("dve", csb[:], onesf[0:64, :], cs[:], None, ALU.mult, r=["onesf", "cs"], w=["csb"])
        P.mm(B[2][:, 0:64], csb[:], ltri[0:64, 0:64], start=True, stop=False, r=["csb", "ltri"], w=[bk(2)])
        P.mm(B[2][:, 0:64], ltri[:], msk[:, g, :], start=False, stop=True, r=["ltri", "msk"], w=[bk(2)])
        P.ts("dve", tmpm[:, g, :], msk[:, g, :], -4096.0, 4096.0, ALU.mult, ALU.add, r=["msk"], w=[("tmpm", g)])
        P.tt("dve", posf[:, g, :], B[2][:, 0:64], tmpm[:, g, :], ALU.add, r=[bk(2), ("tmpm", g)], w=[("posf", g)])

    P.memset("pool", ztile[:], 0.0, w=["ztile"])
    for g in range(4):
        P.ts("dve", idxsf[:, g, :], posf[:, g, :], float(CAP), float(g * YR), ALU.min, ALU.add, r=[("posf", g)], w=[("idxsf", g)])
        P.dma("sp", ye_d[g * YR + CAP:(g + 1) * YR, :], ztile[:], r=["ztile"], w=[("ye_z", g)])
    P.copy("dve", idxsi[:], idxsf[:].rearrange("p g j -> p (g j)"), r=[("idxsf", g) for g in range(4)], w=["idxsi"])
    ncnt = {"hf": 0, "w": 0, "sg": 0, "ev": 0, "sc": 0, "wd": 0}
    sched = [("G", 0, 0), ("F", 0, 0), ("G", 0, 1), ("F", 0, 1), ("G", 1, 0), ("S", 0, 0), ("F", 1, 0), ("G", 1, 1), ("F", 1, 1), ("S", 1, 0)]
    for kind, b, el in sched:
        g = b * 2 + el
        if kind == "G":
            for j in range(64):
                i3 = ncnt["hf"] % 3
                ncnt["hf"] += 1
                P.ts("dve", selt[i3][:], iota[:], posf[:, g, j:j + 1], None, ALU.is_equal,
                     r=["iota", ("posf", g)], w=[("selt", i3)])
                for sh in range(2):
                    P.mm(B[sh][0:1, :], tokhl[:, 0, j:j + 1], selt[i3][:, sh * 512:(sh + 1) * 512], start=(j == 0), stop=False,
                         r=["tokhl", ("selt", i3)], w=[bk(sh)])
                    P.mm(B[sh][0:1, :], tokhl[:, 1, j:j + 1], selt[i3][:, sh * 512:(sh + 1) * 512], start=False, stop=(j == 63),
                         r=["tokhl", ("selt", i3)], w=[bk(sh)])
            for sh in range(2):
                P.copy("dve", idxrow[0:1, sh * 512:(sh + 1) * 512], B[sh][0:1, :], r=[bk(sh)], w=[("idxrow", sh)])
            for k in range(8):
                P.mm(B[2][:, k:k + 1], idxrow[0:1, k * 128:(k + 1) * 128], onesf[0:1, 0:1], r=[("idxrow", k // 4), "onesf"], w=[bk(2)])
            P.ts("dve", idxf[:], B[2][:, 0:8], float(b * S), None, ALU.add, r=[bk(2)], w=["idxf"])
            P.copy("dve", idxi[:], idxf[:], r=["idxf"], w=["idxi"])
            for k in range(8):
                hb_ = hft[k % 2]
                P.op("pool", lambda e, hb_=hb_, k=k: e.indirect_dma_start(
                    out=hb_[:, :], out_offset=None, in_=hf_d[:, :],
                    in_offset=bass.IndirectOffsetOnAxis(ap=idxi[:, k:k + 1], axis=0)),
                    ["idxi"], [("hft", k % 2)], dma=True)
                tb_ = 3 + (k % 2)
                ptv = B[tb_][:].bitcast(BF16).rearrange("p (a b) -> p a b", b=128)
                for dk in range(8):
                    P.tr(ptv[:, dk, :], hb_[:, dk * 128:(dk + 1) * 128], identb[:], r=[("hft", k % 2), "identb"], w=[bk(tb_)])
                P.copy("act" if k % 2 else "dve", xeT[:, :, k * 128:(k + 1) * 128], ptv, r=[bk(tb_)], w=[("xeT", k)])
        elif kind == "F":
            xk = [("xeT", k) for k in range(8)]
            for fb in range(NFC // 2):
                wi = ncnt["w"] % 2
                ncnt["w"] += 1
                for k in range(8):
                    P.dma("sp" if k % 2 else "act", wgf[:, k, :], wg_d[el, k * 128:(k + 1) * 128, fb * 256:(fb + 1) * 256], w=[("wgf", k)])
                    P.dma("act" if k % 2 else "sp", wuf[:, k, :], wu_d[el, k * 128:(k + 1) * 128, fb * 256:(fb + 1) * 256], w=[("wuf", k)])
                P.copy("act", wgs[wi][:], wgf[:], r=[("wgf", k) for k in range(8)], w=[("wgs", wi)])
                P.copy("pool", wus[wi][:], wuf[:], r=[("wuf", k) for k in range(8)], w=[("wus", wi)])
                for fcl in range(2):
                    fc = fb * 2 + fcl
                    for sh in range(2):
                        bg = (fc % 2) * 4 + sh
                        bu = (fc % 2) * 4 + 2 + sh
                        for k in range(8):
                            P.mm(B[bg][:], wgs[wi][:, k, fcl * 128:(fcl + 1) * 128], xeT[:, k, sh * 512:(sh + 1) * 512],
                                 start=(k == 0), stop=(k == 7), r=xk + [("wgs", wi)], w=[bk(bg)])
                        for k in range(8):
                            P.mm(B[bu][:], wus[wi][:, k, fcl * 128:(fcl + 1) * 128], xeT[:, k, sh * 512:(sh + 1) * 512],
                                 start=(k == 0), stop=(k == 7), r=xk + [("wus", wi)], w=[bk(bu)])
                        si = ncnt["sg"] % 2
                        ncnt["sg"] += 1
                        P.act(sg[si][:], B[bg][:], AF.Silu, r=[bk(bg)], w=[("sg", si)])
                        P.tt("dve", hidT[:, fc, sh * 512:(sh + 1) * 512], sg[si][:], B[bu][:], ALU.mult,
                             r=[("sg", si), bk(bu)], w=[("hidT", fc, sh)])
            for q4 in range(4):
                wq_ = ncnt["wd"] % 2
                ncnt["wd"] += 1
                for hh_ in range(2):
                    f0 = hh_ * (NFC // 2)
                    src = wd_d[el, f0 * 128:(f0 + NFC // 2) * 128, q4 * 256:(q4 + 1) * 256].rearrange("(f p) n -> p f n", p=128)
                    P.dma("sp" if hh_ else "act", wdf[:], src, w=["wdf"])
                    P.copy("act", wdq[wq_][:, f0:f0 + NFC // 2, :], wdf[:], r=["wdf"], w=[("wdq", wq_, hh_)])
                for st in range(8):
                    bi = ncnt["ev"] % 8
                    ncnt["ev"] += 1
                    for fc in range(NFC):
                        P.mm(B[bi][:, 0:256], hidT[:, fc, st * 128:(st + 1) * 128], wdq[wq_][:, fc, :],
                             start=(fc == 0), stop=(fc == NFC - 1), r=[("hidT", fc, st // 4), ("wdq", wq_, fc // (NFC // 2))], w=[bk(bi)])
                    P.copy("act" if bi % 2 else "dve", ye[el][:, st, q4 * 256:(q4 + 1) * 256], B[bi][:, 0:256], r=[bk(bi)], w=[("ye", st, q4)])
            for st in range(8):
                P.dma("sp" if st % 2 else "act", ye_d[g * YR + st * 128:g * YR + (st + 1) * 128, :], ye[el][:, st, :],
                      r=[("ye", st, q4) for q4 in range(4)], w=[("ye_d", g, st)])
        else:
            for j in range(64):
                ot = otile[j % 2]
                rbuf = []
                for el in range(2):
                    g = b * 2 + el
                    rb_ = rt[el][j % 2]
                    rkey = ("rt", el, j % 2)
                    P.op("pool", lambda e, rb_=rb_, g=g, j=j: e.indirect_dma_start(
                        out=rb_[:, :], out_offset=None, in_=ye_d[:, :],
                        in_offset=bass.IndirectOffsetOnAxis(ap=idxsi[:, g * 64 + j:g * 64 + j + 1], axis=0)),
                        ["idxsi"] + [("ye_d", g, st) for st in range(8)] + [("ye_z", g)], [rkey], dma=True)
                    rbuf.append((rb_, rkey))
                P.ts("dve", ot[:], rbuf[0][0][:], affc[:, b * 2, j:j + 1], None, ALU.mult, r=[rbuf[0][1], "affc"], w=[("ot", j % 2)])
                P.stt("dve", ot[:], rbuf[1][0][:], affc[:, b * 2 + 1, j:j + 1], ot[:], ALU.mult, ALU.add,
                      r=[rbuf[1][1], "affc", ("ot", j % 2)], w=[("ot", j % 2)])
                P.dma("sp", out_d[b, j * 128:(j + 1) * 128, :], ot[:], r=[("ot", j % 2)], w=[("out", b, j)], is_out=True)

    return P.build()


def run_M(hf, aff, wg, wu, wd):
    if "M" not in _cache:
        _cache["M"] = build_M()
    nc = _cache["M"]
    hf_all = np.ascontiguousarray(np.concatenate([np.asarray(h) for h in hf], 0))
    aff_all = np.concatenate([np.asarray(a) for a in aff], 0).reshape(NB, S, 16)
    ltri = np.triu(np.ones((128, 128), np.float32), 1)
    iota = np.ascontiguousarray(np.broadcast_to(np.arange(1024, dtype=np.float32)[None, :], (128, 1024)))
    iotap = (np.arange(128, dtype=np.float32)[:, None] + 128.0 * np.arange(8, dtype=np.float32)[None, :]).astype(np.float32)
    tok = (np.arange(64)[None, :] * 128 + np.arange(128)[:, None])
    tokhl = np.stack([(tok // 64) * 64, tok % 64], 1).astype(np.float32)
    maps = []
    for c in range(8):
        a = np.zeros((128, 4, 64), np.float32)
        for b in range(NB):
            for el in range(2):
                a[:, b * 2 + el, :] = aff_all[b, :, 2 * c + el].reshape(64, 128).T
        maps.append({"aff": a, "hf": hf_all, "wg": np.ascontiguousarray(wg[2 * c:2 * c + 2]), "wu": np.ascontiguousarray(wu[2 * c:2 * c + 2]),
                     "wd": np.ascontiguousarray(wd[2 * c:2 * c + 2]), "ltri": ltri, "iota": iota, "iotap": iotap, "ident": _ident_np(),
                     "tokhl": tokhl})
    res = run(nc, maps)
    return [res.results[c]["out"] for c in range(8)]


CH = 64
NCH = S // CH
NCB = 4


def build_S(nchunks=NCH):
    P = Prog()
    tm_d = P.dram("tm", [4, 64, 4, S], F32)
    fm_d = P.dram("fm", [5, 64, 4, S], F32)
    tri_d = P.dram("tri", [64, 64], F32)
    m2_d = P.dram("m2", [64, 128], F32)
    mT_d = P.dram("mT", [64, 64], F32)
    id_d = P.dram("id64", [64, 64], F32)
    y_d = P.dram("y", [64, 4, S], F32, kind="ExternalOutput")

    tri = P.sb([64, 64], F32)
    m2 = P.sb([64, 128], F32)
    mT = P.sb([64, 64], F32)
    id64 = P.sb([64, 64], F32)
    W = NCB * 64
    tmB = [[P.sb([64, 4, W], F32) for _ in range(4)] for _ in range(2)]
    fmB = [[P.sb([64, 4, W], F32) for _ in range(5)] for _ in range(2)]
    vB = [P.sb([64, 4, W], BF16) for _ in range(2)]
    yB = [P.sb([64, 4, W], F32) for _ in range(2)]
    St = P.sb([64, 4, 64], F32)
    Sb = P.sb([64, 4, 64], BF16)
    two = lambda shape, dt=F32: [P.sb(shape, dt) for _ in range(2)]
    Ep, Emm, dd, Eprev = two([64, 4, 64]), two([64, 4, 128]), two([64, 4, 64]), two([64, 4, 64])
    AR = two([64, 4, 128], BF16)
    ktf, btf, ktt, btt = two([64, 4, 64], BF16), two([64, 4, 64], BF16), two([64, 4, 64], BF16), two([64, 4, 64], BF16)
    MB, MK, Am = two([64, 4, 128], BF16), two([64, 4, 128], BF16), two([64, 4, 64], BF16)
    MA2 = [two([64, 4, 128], BF16) for _ in range(2)]
    Pm = [two([64, 4, 64], BF16) for _ in range(2)]
    Z, U, tmpS = P.sb([64, 4, 64], BF16), P.sb([64, 4, 64], BF16), P.sb([64, 4, 64], F32)
    Q = [P.ps([64, 4, 128], F32) for _ in range(8)]
    qk = lambda i: ("Q", i)

    P.dma("sp", tri[:], tri_d, w=["tri"])
    P.dma("sp", m2[:], m2_d, w=["m2"])
    P.dma("sp", mT[:], mT_d, w=["mT"])
    P.dma("sp", id64[:], id_d, w=["id64"])
    P.memset("dve", St[:], 0.0, w=["St"])
    P.memset("dve", Sb[:], 0.0, w=["Sb"])
    HS = [slice(0, 2), slice(2, 4)]
    bcm = lambda t, n, hs: t[:].unsqueeze(1).broadcast_to([64, n, t.shape[1]])
    final = {}

    def pre(c):
        blk, cl = c // NCB, c % NCB
        bp = blk % 2
        if cl == 0:
            bs = slice(blk * W, (blk + 1) * W)
            for a in range(4):
                P.dma("sp", tmB[bp][a][:], tm_d[a, :, :, bs], w=[("tmB", bp, a)])
            for a in range(5):
                P.dma("act", fmB[bp][a][:], fm_d[a, :, :, bs], w=[("fmB", bp, a)])
            P.copy("pool", vB[bp][:], tmB[bp][3][:], r=[("tmB", bp, 3)], w=[("vB", bp)])
        cs = slice(cl * 64, (cl + 1) * 64)
        lw_tm, k_tm, b_tm, v_tm = [tmB[bp][a][:, :, cs] for a in range(4)]
        lw_fm, r_fm, a_fm, k_fm, b_fm = [fmB[bp][a][:, :, cs] for a in range(5)]
        tmk = lambda a: ("tmB", bp, a)
        fmk = lambda a: ("fmB", bp, a)
        p = c % 2
        K_ = lambda name: (name, p)
        for ch in range(4):
            P.mm(Q[0][:, ch, 0:64], lw_tm[:, ch, :], tri[:], r=[tmk(0), "tri"], w=[qk(0)])
            P.mm(Q[0][:, ch, 64:128], tri[:], lw_tm[:, ch, :], r=[tmk(0), "tri"], w=[qk(0)])
        P.act(Ep[p][:], Q[0][:, :, 0:64], AF.Exp, r=[qk(0)], w=[K_("Ep"), qk(0)])
        P.act(Emm[p][:], Q[0][:], AF.Exp, r=[qk(0)], w=[K_("Emm"), qk(0)], scale=-1.0)
        P.tt("dve", dd[p][:], Q[0][:, :, 0:64], lw_fm, ALU.subtract, r=[qk(0), fmk(0)], w=[K_("dd"), qk(0)])
        P.act(Eprev[p][:], dd[p][:], AF.Exp, r=[K_("dd")], w=[K_("Eprev")])
        P.tt("dve", AR[p][:, :, 0:64], a_fm, Eprev[p][:], ALU.mult, r=[fmk(2), K_("Eprev")], w=[K_("ARa")])
        P.tt("pool", AR[p][:, :, 64:128], r_fm, Ep[p][:], ALU.mult, r=[fmk(1), K_("Ep")], w=[K_("ARr")])
        P.tt("dve", ktf[p][:], k_fm, Emm[p][:, :, 0:64], ALU.mult, r=[fmk(3), K_("Emm")], w=[K_("ktf")])
        P.tt("pool", btf[p][:], b_fm, Emm[p][:, :, 0:64], ALU.mult, r=[fmk(4), K_("Emm")], w=[K_("btf")])
        P.tt("dve", ktt[p][:], k_tm, Emm[p][:, :, 64:128], ALU.mult, r=[tmk(1), K_("Emm")], w=[K_("ktt")])
        P.tt("pool", btt[p][:], b_tm, Emm[p][:, :, 64:128], ALU.mult, r=[tmk(2), K_("Emm")], w=[K_("btt")])
        yield
        ARk = [K_("ARa"), K_("ARr")]
        for ch in range(4):
            P.mm(Q[1][:, ch, :], btf[p][:, ch, :], AR[p][:, ch, :], r=[K_("btf")] + ARk, w=[qk(1)])
            P.mm(Q[2][:, ch, :], ktf[p][:, ch, :], AR[p][:, ch, :], r=[K_("ktf")] + ARk, w=[qk(2)])
            P.mm(Q[3][:, ch, 0:64], AR[p][:, ch, 0:64], btf[p][:, ch, :], r=[K_("btf")] + ARk, w=[qk(3)])
        P.tt("dve", MB[p][:], Q[1][:], bcm(m2, 4, None), ALU.mult, r=[qk(1), "m2"], w=[K_("MB")])
        P.tt("dve", MK[p][:], Q[2][:], bcm(m2, 4, None), ALU.mult, r=[qk(2), "m2"], w=[K_("MK")])
        P.tt("dve", Am[p][:], Q[3][:, :, 0:64], bcm(mT, 4, None), ALU.mult, r=[qk(3), "mT"], w=[K_("Am")])
        P.tt("pool", Pm[p][0][:], MB[p][:, :, 0:64], bcm(id64, 4, None), ALU.add, r=[K_("MB"), "id64"], w=[K_("P0_0"), K_("P0_1")])
        yield
        curM = lambda ch: MB[p][:, ch, 0:64]
        curA = lambda ch: Am[p][:, ch, :]
        curk = lambda hf: [K_("MB"), K_("Am")]
        pi = 0
        for lvl in range(5):
            mi = lvl % 2
            for hf in range(2):
                qs_ = 4 + hf
                for ch in range(2 * hf, 2 * hf + 2):
                    P.mm(Q[qs_][:, ch, 0:64], curA(ch), curM(ch), r=curk(hf), w=[qk(qs_)])
                    P.mm(Q[qs_][:, ch, 64:128], curM(ch), curA(ch), r=curk(hf), w=[qk(qs_)])
                P.copy("act", MA2[p][mi][:, HS[hf], :], Q[qs_][:, HS[hf], :], r=[qk(qs_)], w=[K_("MA2_%d_%d" % (mi, hf))])
            curM = lambda ch, mi=mi: MA2[p][mi][:, ch, 0:64]
            curA = lambda ch, mi=mi: MA2[p][mi][:, ch, 64:128]
            curk = lambda hf, mi=mi: [K_("MA2_%d_%d" % (mi, hf))]
            yield
            for hf in range(2):
                qs_ = 4 + hf
                for ch in range(2 * hf, 2 * hf + 2):
                    cc = ch - 2 * hf + (2 - 2 * hf)
                    P.mm(Q[qs_][:, cc, 0:64], curA(ch), Pm[p][pi][:, ch, :], r=curk(hf) + [K_("P%d_%d" % (pi, hf))], w=[qk(qs_)])
                osl = slice(2 - 2 * hf, 4 - 2 * hf)
                P.tt("dve", Pm[p][1 - pi][:, HS[hf], :], Pm[p][pi][:, HS[hf], :], Q[qs_][:, osl, 0:64], ALU.add,
                     r=[K_("P%d_%d" % (pi, hf)), qk(qs_)], w=[K_("P%d_%d" % (1 - pi, hf))])
            pi = 1 - pi
            yield
        final[c] = pi

    def state(c):
        blk, cl = c // NCB, c % NCB
        bp = blk % 2
        cs = slice(cl * 64, (cl + 1) * 64)
        v_b = vB[bp][:, :, cs]
        vk = ("vB", bp)
        p = c % 2
        K_ = lambda name: (name, p)
        ARk = [K_("ARa"), K_("ARr")]
        pi = final[c]
        Pk = [K_("P%d_0" % pi), K_("P%d_1" % pi)]
        Pf = Pm[p][pi]
        for ch in range(4):
            P.mm(Q[6][:, ch, 0:64], AR[p][:, ch, 0:64], Sb[:, ch, :], start=True, stop=False, r=ARk + ["Sb"], w=[qk(6)])
            P.mm(Q[6][:, ch, 0:64], MK[p][:, ch, 0:64], v_b[:, ch, :], start=False, stop=True, r=[K_("MK"), vk], w=[qk(6)])
        P.copy("dve", Z[:], Q[6][:, :, 0:64], r=[qk(6)], w=["Z"])
        yield
        for ch in range(4):
            P.mm(Q[7][:, ch, 0:64], Pf[:, ch, :], Z[:, ch, :], r=Pk + ["Z"], w=[qk(7)])
        P.copy("dve", U[:], Q[7][:, :, 0:64], r=[qk(7)], w=["U"])
        yield
        for ch in range(4):
            P.mm(Q[6][:, ch, 64:128], btt[p][:, ch, :], U[:, ch, :], start=True, stop=False, r=[K_("btt"), "U"], w=[qk(6)])
            P.mm(Q[6][:, ch, 64:128], ktt[p][:, ch, :], v_b[:, ch, :], start=False, stop=True, r=[K_("ktt"), vk], w=[qk(6)])
        for ch in range(4):
            P.mm(Q[7][:, ch, 64:128], AR[p][:, ch, 64:128], Sb[:, ch, :], start=True, stop=False, r=ARk + ["Sb"], w=[qk(7)])
            P.mm(Q[7][:, ch, 64:128], MB[p][:, ch, 64:128], U[:, ch, :], start=False, stop=False, r=[K_("MB"), "U"], w=[qk(7)])
            P.mm(Q[7][:, ch, 64:128], MK[p][:, ch, 64:128], v_b[:, ch, :], start=False, stop=True, r=[K_("MK"), vk], w=[qk(7)])
        P.tt("dve", tmpS[:], Q[6][:, :, 64:128], St[:], ALU.add, r=[qk(6), "St"], w=["tmpS"])
        P.tt("dve", St[:], tmpS[:], Ep[p][:, :, 63:64].broadcast_to([64, 4, 64]), ALU.mult, r=["tmpS", K_("Ep")], w=["St"])
        P.copy("pool", Sb[:], St[:], r=["St"], w=["Sb"])
        P.copy("act", yB[bp][:, :, cs], Q[7][:, :, 64:128], r=[qk(7)], w=[("yB", bp, cl)])
        if cl == NCB - 1 or c == nchunks - 1:
            bs = slice(blk * W, (blk + 1) * W)
            P.dma("sp", y_d[:, :, bs], yB[bp][:], r=[("yB", bp, i) for i in range(NCB)], w=[("y", blk)], is_out=True)
        yield

    def drain(g):
        for _ in g:
            pass

    def step(g):
        try:
            next(g)
            return True
        except StopIteration:
            return False

    drain(pre(0))
    for c in range(nchunks):
        gs = state(c)
        if c + 1 < nchunks:
            gp = pre(c + 1)
            step(gp); step(gp)
            step(gs)
            step(gp)
            step(gs)
            step(gp)
            step(gs)
            drain(gp)
        else:
            drain(gs)
    return P.build()


def scan_consts():
    s = np.arange(64)[:, None]
    t = np.arange(64)[None, :]
    tri = (s <= t).astype(np.float32)
    m2 = np.concatenate([(s < t).astype(np.float32), (s <= t).astype(np.float32)], 1)
    mT = (t < s).astype(np.float32)
    return {"tri": tri, "m2": m2, "mT": mT, "id64": np.eye(64, dtype=np.float32)}


def tm_layout(x):
    return x.reshape(NCH, 64, 64).transpose(1, 0, 2).reshape(64, S)


def tm_unlayout(y):
    return y.reshape(64, NCH, 64).transpose(1, 0, 2).reshape(S, 64)


SCAN_ARRS = ["r", "v", "a", "lw0", "lw1", "kd0", "kd1", "b0", "b1", "bonus", "g"]
EXPM05 = float(np.exp(-0.5))
GELU_C = 1.5957691216057308


def gelu_tanh(P, out, x_ps, xkey, okey, t1, k1, t2, k2):
    P.copy("act", t1[:], x_ps, r=[xkey], w=[k1])
    P.tt("dve", t2[:], t1[:], t1[:], ALU.mult, r=[k1], w=[k2])
    P.ts("dve", t2[:], t2[:], 0.044715, 1.0, ALU.mult, ALU.add, r=[k2], w=[k2])
    P.tt("dve", t2[:], t2[:], t1[:], ALU.mult, r=[k2, k1], w=[k2])
    P.act(t2[:], t2[:], AF.Sigmoid, r=[k2], w=[k2], scale=GELU_C)
    P.tt("dve", out, t2[:], t1[:], ALU.mult, r=[k2, k1], w=[okey])


def build_O1():
    P = Prog()
    x2_d = P.dram("x2", [TOK, D], F32)
    pt_d = P.dram("parts", [8, TOK, D], F32)
    x2h_d = P.dram("x2h", [2, D], F32)
    pth_d = P.dram("partsh", [8, 2, D], F32)
    g_d = P.dram("g", [1, D], F32)
    w_d = P.dram("w", [D, 2944], F32)
    lng_d = P.dram("lng", [1, 512], F32)
    lnb_d = P.dram("lnb", [1, 512], F32)
    wsT_d = P.dram("wsT", [128, 4, 128], F32)
    bsT_d = P.dram("bsT", [128, 4], F32)
    mu_d = P.dram("mu", [128, 15, 2], F32)
    w2p_d = P.dram("w2p", [128, 2, 512], F32)
    a2p_d = P.dram("a2p", [128, 2, 512], F32)
    g2_d = P.dram("g2", [128, 512], F32)
    pv_d = P.dram("pvec", [128, 4, 8], F32)
    bo_d = P.dram("bones", [128, 128], F32)
    ident_d = P.dram("ident", [128, 128], F32)
    x3_o = P.dram("x3_o", [TOK, D], F32, kind="ExternalOutput")
    yc_o = P.dram("yc_o", [TOK, 512], BF16, kind="ExternalOutput")
    sc_o = P.dram("sc_o", [len(SCAN_ARRS), 4, 128, TOK], F32, kind="ExternalOutput")

    gb = P.sb([128, D], F32)
    wc = P.sb([128, 8, 2944], BF16)
    lng = P.sb([128, 512], F32)
    lnb = P.sb([128, 512], F32)
    wsTf = P.sb([128, 4, 128], F32)
    wsT = P.sb([128, 4, 128], BF16)
    bsT = P.sb([128, 4], F32)
    mu = P.sb([128, 15, 2], F32)
    c0 = P.sb([128, 15], F32)
    w2pf = P.sb([128, 2, 512], F32)
    a2pf = P.sb([128, 2, 512], F32)
    g2f = P.sb([128, 512], F32)
    w2p = P.sb([128, 2, 512], BF16)
    a2p = P.sb([128, 2, 512], BF16)
    g2 = P.sb([128, 512], BF16)
    pv = P.sb([128, 4, 8], F32)
    bones = P.sb([128, 128], F32)
    identf = P.sb([128, 128], F32)
    ident = P.sb([128, 128], BF16)
    scr = norm_scratch(P)
    acc = [P.sb([128, D], F32) for _ in range(2)]
    prt = [P.sb([128, D], F32) for _ in range(2)]
    h = P.sb([128, D], BF16)
    hT = P.sb([128, 8, TOK + 2], BF16)
    T = [P.sb([128, 512], F32) for _ in range(22)]
    tk = lambda i: ("T", i)
    ub = P.sb([128, 512], BF16)
    vnb = P.sb([128, 512], BF16)
    ycb = [P.sb([128, 512], BF16) for _ in range(2)]
    mean = P.sb([128, 1], F32)
    var = P.sb([128, 1], F32)
    fd = P.sb([128, 514], F32)
    thb = P.sb([128, 512], BF16)
    hab = P.sb([128, 512], BF16)
    sgb = P.sb([128, 512], BF16)
    Bt = P.ps([128, 8, 128], BF16)
    Bc = [P.ps([128, 512], F32) for _ in range(2)]
    Bs = P.ps([128, 4, 128], F32)
    Bm = P.ps([128, 512], F32)
    Be = P.ps([128, 512], F32)
    Bl = [P.ps([128, 512], F32) for _ in range(2)]

    P.dma("sp", gb[:], g_d.partition_broadcast(128), w=["gb"])
    P.dma("sp", lng[:], lng_d.partition_broadcast(128), w=["lng"])
    P.dma("sp", lnb[:], lnb_d.partition_broadcast(128), w=["lnb"])
    P.dma("sp", identf[:], ident_d, w=["identf"])
    P.copy("dve", ident[:], identf[:], r=["identf"], w=["ident"])
    P.dma("sp", wsTf[:], wsT_d, w=["wsTf"])
    P.copy("dve", wsT[:], wsTf[:], r=["wsTf"], w=["wsT"])
    P.dma("sp", bsT[:], bsT_d, w=["bsT"])
    P.dma("sp", mu[:], mu_d, w=["mu"])
    P.dma("sp", w2pf[:], w2p_d, w=["w2pf"])
    P.dma("sp", a2pf[:], a2p_d, w=["a2pf"])
    P.dma("sp", g2f[:], g2_d, w=["g2f"])
    P.copy("dve", w2p[:], w2pf[:], r=["w2pf"], w=["w2p"])
    P.copy("dve", a2p[:], a2pf[:], r=["a2pf"], w=["a2p"])
    P.copy("dve", g2[:], g2f[:], r=["g2f"], w=["g2"])
    P.dma("sp", pv[:], pv_d, w=["pv"])
    P.ts("dve", pv[:, :, 7], pv[:, :, 5], -1.0, 1.0, ALU.mult, ALU.add, r=["pv"], w=["pv"])
    P.dma("sp", bones[:], bo_d, w=["bones"])
    P.tt("dve", c0[:], mu[:, :, 0], mu[:, :, 1], ALU.add, r=["mu"], w=["c0"])
    P.ts("dve", c0[:], c0[:], -1.0, 1.0, ALU.mult, ALU.add, r=["c0"], w=["c0"])
    for k in range(8):
        P.dma("pool", wc[:, k, 0:1472], w_d[k * 128:(k + 1) * 128, 0:1472], w=[("wc", k)])
        P.dma("pool", wc[:, k, 1472:2944], w_d[k * 128:(k + 1) * 128, 1472:2944], w=[("wc", k)])
    wck = [("wc", k) for k in range(8)]
    P.dma("sp", acc[0][0:2, :], x2h_d, w=[("acc", 0)])
    for c in range(8):
        P.dma("sp", prt[c % 2][0:2, :], pth_d[c], w=[("prt", c % 2)])
        P.tt("dve", acc[0][0:2, :], acc[0][0:2, :], prt[c % 2][0:2, :], ALU.add, r=[("acc", 0), ("prt", c % 2)], w=[("acc", 0)])
    sq, ss, rstd = scr["sq"], scr["ss"], scr["rstd"]
    P.act(sq[0:2, :], acc[0][0:2, :], AF.Square, r=[("acc", 0)], w=["sq", "ss"], accum_out=ss[0:2, :])
    P.ts("dve", rstd[0:2, :], ss[0:2, :], 1.0 / D, 1e-6, ALU.mult, ALU.add, r=["ss"], w=["rstd"])
    P.act(rstd[0:2, :], rstd[0:2, :], AF.Sqrt, r=["rstd"], w=["rstd"])
    P.op("dve", lambda e: e.reciprocal(out=rstd[0:2, :], in_=rstd[0:2, :]), ["rstd"], ["rstd"])
    P.stt("dve", h[0:2, :], acc[0][0:2, :], rstd[0:2, :], gb[0:2, :], ALU.mult, ALU.mult, r=[("acc", 0), "rstd", "gb"], w=["h"])
    for k in range(8):
        P.tr(Bt[:, k, 0:2], h[0:2, k * 128:(k + 1) * 128], ident[0:2, 0:2], r=["h", "ident"], w=["Bt"])
    P.copy("act", hT[:, :, 0:1], Bt[:, :, 0:1], r=["Bt"], w=[("hT", "h0")])
    P.copy("act", hT[:, :, TOK + 1:TOK + 2], Bt[:, :, 1:2], r=["Bt"], w=[("hT", "h1")])
    for t in range(NT):
        a = acc[t % 2]
        ak = ("acc", t % 2)
        ts_ = slice(t * 128, (t + 1) * 128)
        P.dma("sp", a[:], x2_d[ts_, :], w=[ak])
        for c in range(8):
            pb = prt[c % 2]
            P.dma("act" if c % 2 else "sp", pb[:], pt_d[c, ts_, :], w=[("prt", c % 2)])
            P.tt("dve" if c % 2 else "pool", a[:], a[:], pb[:], ALU.add, r=[ak, ("prt", c % 2)], w=[ak])
        P.dma("sp", x3_o[ts_, :], a[:], r=[ak], w=[("x3_o", t)], is_out=True)
        rmsnorm_tile(P, a[:], ak, gb[:], h[:], "h", scr)
        for k in range(8):
            P.tr(Bt[:, k, :], h[:, k * 128:(k + 1) * 128], ident[:], r=["h", "ident"], w=["Bt"])
        P.copy("act", hT[:, :, 1 + t * 128:1 + (t + 1) * 128], Bt[:], r=["Bt"], w=[("hT", t)])
        for k in range(8):
            P.mm(Bc[0][:], hT[:, k, 1 + t * 128:1 + (t + 1) * 128], wc[:, k, 0:512], start=(k == 0), stop=(k == 7), r=[("hT", t)] + wck, w=["Bc0"])
        for k in range(8):
            P.mm(Bc[1][:], hT[:, k, 1 + t * 128:1 + (t + 1) * 128], wc[:, k, 512:1024], start=(k == 0), stop=(k == 7), r=[("hT", t)] + wck, w=["Bc1"])
        gelu_tanh(P, ub[:], Bc[0][:], "Bc0", "ub", T[0], tk(0), T[1], tk(1))
        gelu_tanh(P, T[4][:], Bc[1][:], "Bc1", tk(4), T[2], tk(2), T[3], tk(3))
        P.op("dve", lambda e: e.reduce_sum(out=mean[:], in_=T[4][:], axis=AX.X), [tk(4)], ["mean"])
        P.ts("dve", mean[:], mean[:], -1.0 / 512, None, ALU.mult, r=["mean"], w=["mean"])
        P.ts("dve", T[5][:], T[4][:], mean[:], None, ALU.add, r=[tk(4), "mean"], w=[tk(5)])
        P.act(T[2][:], T[5][:], AF.Square, r=[tk(5)], w=[tk(2), "var"], accum_out=var[:])
        P.ts("dve", var[:], var[:], 1.0 / 512, 1e-5, ALU.mult, ALU.add, r=["var"], w=["var"])
        P.act(var[:], var[:], AF.Sqrt, r=["var"], w=["var"])
        P.op("dve", lambda e: e.reciprocal(out=var[:], in_=var[:]), ["var"], ["var"])
        P.stt("dve", T[5][:], T[5][:], var[:], lng[:], ALU.mult, ALU.mult, r=[tk(5), "var", "lng"], w=[tk(5)])
        P.tt("dve", vnb[:], T[5][:], lnb[:], ALU.add, r=[tk(5), "lnb"], w=["vnb"])
        for g in range(4):
            P.mm(Bs[:, g, :], wsT[:, g, :], vnb[:, g * 128:(g + 1) * 128], r=["wsT", "vnb"], w=["Bs"])
        P.tt("dve", T[5][:].rearrange("p (g c) -> p g c", c=128), Bs[:], bsT[:].unsqueeze(2).broadcast_to([128, 4, 128]), ALU.add,
             r=["Bs", "bsT"], w=[tk(5)])
        yb = ycb[t % 2]
        P.tt("dve", yb[:], T[5][:], ub[:], ALU.mult, r=[tk(5), "ub"], w=[("ycb", t % 2)])
        P.dma("sp", yc_o[ts_, :], yb[:], r=[("ycb", t % 2)], w=[("yc_o", t)], is_out=True)
    hTk = [("hT", t) for t in range(NT)] + [("hT", "h0"), ("hT", "h1")]

    def shifted(blk, chunk, out_tile, okey):
        c0s = slice(blk * 512, blk * 512 + 514)
        col = 1024 + chunk * 128
        for k in range(8):
            P.mm(Bm[:], wc[:, k, col:col + 128], hT[:, k, 1 + blk * 512:1 + (blk + 1) * 512], start=(k == 0), stop=(k == 7), r=hTk + wck, w=["Bm"])
        for k in range(8):
            P.mm(Be[:, 0:2], wc[:, k, col:col + 128], hT[:, k, blk * 512:blk * 512 + 514:513], start=(k == 0), stop=(k == 7), r=hTk + wck, w=["Be"])
        P.copy("act", fd[:, 1:513], Bm[:], r=["Bm"], w=["fd_m"])
        P.copy("act", fd[:, 0:514:513], Be[:, 0:2], r=["Be"], w=["fd_e"])
        fk = ["fd_m", "fd_e"]
        P.ts("dve", out_tile[:], fd[:, 1:513], c0[:, chunk:chunk + 1], None, ALU.mult, r=fk + ["c0"], w=[okey])
        P.stt("dve", out_tile[:], fd[:, 0:512], mu[:, chunk, 0:1], out_tile[:], ALU.mult, ALU.add, r=fk + ["mu", okey], w=[okey])
        P.stt("dve", out_tile[:], fd[:, 2:514], mu[:, chunk, 1:2], out_tile[:], ALU.mult, ALU.add, r=fk + ["mu", okey], w=[okey])

    nb = 0
    for blk in range(TOK // 512):
        bs_ = slice(blk * 512, (blk + 1) * 512)
        shifted(blk, 12, T[0], tk(0))
        P.act(thb[:], T[0][:], AF.Tanh, r=[tk(0)], w=["thb"])
        shifted(blk, 13, T[0], tk(0))
        P.copy("act", hab[:], T[0][:], r=[tk(0)], w=["hab"])
        shifted(blk, 14, T[0], tk(0))
        P.act(sgb[:], T[0][:], AF.Sigmoid, r=[tk(0)], w=["sgb"])
        for pc in range(4):
            cs_ = slice(pc * 128, (pc + 1) * 128)
            PV = lambda i: pv[:, pc, i:i + 1]
            o = lambda name, tile, key: P.dma("sp", sc_o[SCAN_ARRS.index(name), pc, :, bs_], tile[:], r=[key], w=[("sc_o", name, pc, blk)], is_out=True)
            shifted(blk, pc, T[1], tk(1))
            shifted(blk, 4 + pc, T[2], tk(2))
            shifted(blk, 8 + pc, T[3], tk(3))
            o("r", T[1], tk(1))
            o("v", T[3], tk(3))
            for dr in range(2):
                bl = Bl[nb % 2]
                blk_ = ("Bl", nb % 2)
                nb += 1
                P.mm(bl[:], w2p[:, dr, cs_], thb[:], r=["w2p", "thb"], w=[blk_])
                P.act(T[4 + dr][:], bl[:], AF.Sigmoid, r=[blk_, "pv"], w=[tk(4 + dr)], bias=PV(dr))
                P.ts("pool", T[4 + dr][:], T[4 + dr][:], -EXPM05, None, ALU.mult, r=[tk(4 + dr)], w=[tk(4 + dr)])
                o("lw%d" % dr, T[4 + dr], tk(4 + dr))
                bl = Bl[nb % 2]
                blk_ = ("Bl", nb % 2)
                nb += 1
                P.mm(bl[:], a2p[:, dr, cs_], hab[:], r=["a2p", "hab"], w=[blk_])
                P.act(T[6 + dr][:], bl[:], AF.Sigmoid, r=[blk_, "pv"], w=[tk(6 + dr)], bias=PV(2 + dr))
            bl = Bl[nb % 2]
            blk_ = ("Bl", nb % 2)
            nb += 1
            P.mm(bl[:], g2[:, cs_], sgb[:], r=["g2", "sgb"], w=[blk_])
            P.copy("act", T[8][:], bl[:], r=[blk_], w=[tk(8)])
            o("g", T[8], tk(8))
            P.ts("dve", T[9][:], T[2][:], PV(4), None, ALU.mult, r=[tk(2), "pv"], w=[tk(9)])
            P.tt("dve", T[10][:], T[9][:], T[9][:], ALU.mult, r=[tk(9)], w=[tk(10)])
            bl = Bl[nb % 2]
            blk_ = ("Bl", nb % 2)
            nb += 1
            P.mm(bl[:], bones[:], T[10][:], r=["bones", tk(10)], w=[blk_])
            P.act(T[10][:], bl[:], AF.Sqrt, r=[blk_], w=[tk(10)])
            P.ts("dve", T[10][:], T[10][:], 1e-12, None, ALU.max, r=[tk(10)], w=[tk(10)])
            P.op("dve", lambda e: e.reciprocal(out=T[10][:], in_=T[10][:]), [tk(10)], [tk(10)])
            P.tt("dve", T[9][:], T[9][:], T[10][:], ALU.mult, r=[tk(9), tk(10)], w=[tk(9)])
            P.ts("pool", T[11][:], T[9][:], -1.0, None, ALU.mult, r=[tk(9)], w=[tk(11)])
            o("a", T[11], tk(11))
            for dr in range(2):
                P.ts("dve", T[12 + dr][:], T[6 + dr][:], PV(5), PV(7), ALU.mult, ALU.add, r=[tk(6 + dr), "pv"], w=[tk(12 + dr)])
                P.tt("dve", T[12 + dr][:], T[12 + dr][:], T[2][:], ALU.mult, r=[tk(12 + dr), tk(2)], w=[tk(12 + dr)])
                o("kd%d" % dr, T[12 + dr], tk(12 + dr))
                P.tt("pool", T[14 + dr][:], T[9][:], T[6 + dr][:], ALU.mult, r=[tk(9), tk(6 + dr)], w=[tk(14 + dr)])
                o("b%d" % dr, T[14 + dr], tk(14 + dr))
            P.tt("dve", T[16][:], T[12][:], T[13][:], ALU.add, r=[tk(12), tk(13)], w=[tk(16)])
            P.tt("dve", T[16][:], T[16][:], T[1][:], ALU.mult, r=[tk(16), tk(1)], w=[tk(16)])
            P.ts("dve", T[16][:], T[16][:], PV(6), None, ALU.mult, r=[tk(16), "pv"], w=[tk(16)])
            bl = Bl[nb % 2]
            blk_ = ("Bl", nb % 2)
            nb += 1
            P.mm(bl[:], bones[:], T[16][:], r=["bones", tk(16)], w=[blk_])
            P.tt("dve", T[17][:], bl[:], T[3][:], ALU.mult, r=[blk_, tk(3)], w=[tk(17)])
            o("bonus", T[17], tk(17))
    return P.build()


def run_O1(x2, parts, prm):
    if "O1" not in _cache:
        _cache["O1"] = build_O1()
    nc = _cache["O1"]
    wsT = np.ascontiguousarray(prm["c_w_s"].transpose(2, 0, 1))
    bsT = np.ascontiguousarray(prm["c_b_s"].T)
    shift = prm["d_shift"]
    mu = np.ascontiguousarray(shift.reshape(2, 15, 128).transpose(2, 1, 0))
    w2p = np.zeros((128, 2, 512), np.float32)
    a2p = np.zeros((128, 2, 512), np.float32)
    for dr in range(2):
        w2p[dr * 64:(dr + 1) * 64, dr, :] = prm["d_w2"][dr]
        a2p[dr * 64:(dr + 1) * 64, dr, :] = prm["d_a2"][dr]
    pvec = np.zeros((128, 4, 8), np.float32)
    col = lambda v: v.reshape(4, 128).T
    pvec[:, :, 0] = col(prm["d_w0"][0]); pvec[:, :, 1] = col(prm["d_w0"][1])
    pvec[:, :, 2] = col(prm["d_a0"][0]); pvec[:, :, 3] = col(prm["d_a0"][1])
    pvec[:, :, 4] = col(prm["d_k_k"]); pvec[:, :, 5] = col(prm["d_k_a"]); pvec[:, :, 6] = col(prm["d_r_k"].reshape(512))
    bones = np.kron(np.eye(2, dtype=np.float32), np.ones((64, 64), np.float32))
    xfull = [np.concatenate([np.asarray(x2[b * 4 + i]) for i in range(4)], 0) for b in range(NB)]
    maps = []
    for c in range(8):
        b, q = c // 4, c % 4
        lo, hi = q * TOK, (q + 1) * TOK
        x2h = np.zeros((2, D), np.float32)
        pth = np.zeros((8, 2, D), np.float32)
        if lo > 0:
            x2h[0] = xfull[b][lo - 1]
            for cc in range(8):
                pth[cc, 0] = parts[cc][b, lo - 1]
        if hi < S:
            x2h[1] = xfull[b][hi]
            for cc in range(8):
                pth[cc, 1] = parts[cc][b, hi]
        maps.append({"x2": np.ascontiguousarray(x2[c]), "parts": np.ascontiguousarray(np.stack([parts[cc][b, lo:hi] for cc in range(8)], 0)),
                     "x2h": x2h, "partsh": pth, "g": prm["o_norm"].reshape(1, D), "w": prm["o_w_in"],
                     "lng": prm["c_ln_g"].reshape(1, 512), "lnb": prm["c_ln_b"].reshape(1, 512), "wsT": wsT, "bsT": bsT, "mu": mu,
                     "w2p": w2p, "a2p": a2p, "g2": prm["d_g2"], "pvec": pvec, "bones": bones, "ident": _ident_np()})
    res = run(nc, maps)
    return ([res.results[c]["x3_o"] for c in range(8)], [res.results[c]["yc_o"] for c in range(8)],
            [res.results[c]["sc_o"] for c in range(8)])


def build_O2():
    P = Prog()
    yf_d = P.dram("yf", [TOK, 512], F32)
    yb_d = P.dram("yb", [TOK, 512], F32)
    bo_d = P.dram("bonus", [TOK, 512], F32)
    gt_d = P.dram("gt", [TOK, 512], F32)
    yc_d = P.dram("yc", [TOK, 512], BF16)
    x3_d = P.dram("x3", [TOK, D], F32)
    lng_d = P.dram("lng", [1, 512], F32)
    lnb_d = P.dram("lnb", [1, 512], F32)
    wo_d = P.dram("wo", [D, D], F32)
    ident_d = P.dram("ident", [128, 128], F32)
    x4_o = P.dram("x4_o", [TOK, D], F32, kind="ExternalOutput")

    lng = P.sb([128, 512], F32)
    lnb = P.sb([128, 512], F32)
    wo = P.sb([128, 8, D], BF16)
    identf = P.sb([128, 128], F32)
    ident = P.sb([128, 128], BF16)
    yf = [P.sb([128, 512], F32) for _ in range(2)]
    yb = [P.sb([128, 512], F32) for _ in range(2)]
    bo = [P.sb([128, 512], F32) for _ in range(2)]
    gt = [P.sb([128, 512], F32) for _ in range(2)]
    x3 = [P.sb([128, D], F32) for _ in range(2)]
    cat = [P.sb([128, D], BF16) for _ in range(2)]
    y = P.sb([128, 512], F32)
    sq = P.sb([128, 512], F32)
    st = P.sb([128, 8], F32)
    catT = P.sb([128, 8, 128], BF16)
    xo = [P.sb([128, D], F32) for _ in range(2)]
    ptr = P.ps([128, 8, 128], BF16)
    pso = [P.ps([128, 512], F32) for _ in range(2)]

    P.dma("sp", lng[:], lng_d.partition_broadcast(128), w=["lng"])
    P.dma("sp", lnb[:], lnb_d.partition_broadcast(128), w=["lnb"])
    P.dma("sp", identf[:], ident_d, w=["identf"])
    P.copy("dve", ident[:], identf[:], r=["identf"], w=["ident"])
    for k in range(8):
        P.dma("pool", wo[:, k, :], wo_d[k * 128:(k + 1) * 128, :], w=[("wo", k)])
    wok = [("wo", k) for k in range(8)]
    v3 = lambda ap: ap.rearrange("p (h c) -> p h c", c=64)
    bc = lambda ap: ap.unsqueeze(2).broadcast_to([128, 8, 64])
    for t in range(NT):
        i = t % 2
        ts_ = slice(t * 128, (t + 1) * 128)
        P.dma("sp", yf[i][:], yf_d[ts_, :], w=[("yf", i)])
        P.dma("act", yb[i][:], yb_d[ts_, :], w=[("yb", i)])
        P.dma("sp", bo[i][:], bo_d[ts_, :], w=[("bo", i)])
        P.dma("act", gt[i][:], gt_d[ts_, :], w=[("gt", i)])
        P.dma("sp", x3[i][:], x3_d[ts_, :], w=[("x3", i)])
        P.dma("act", cat[i][:, 0:512], yc_d[ts_, :], w=[("cat", i, 0)])
        P.tt("dve", y[:], yf[i][:], yb[i][:], ALU.add, r=[("yf", i), ("yb", i)], w=["y"])
        P.op("dve", lambda e: e.tensor_reduce(out=st[:], in_=v3(y[:]), axis=AX.X, op=ALU.add), ["y"], ["st"])
        P.ts("dve", st[:], st[:], -1.0 / 64, None, ALU.mult, r=["st"], w=["st"])
        P.tt("dve", v3(y[:]), v3(y[:]), bc(st[:]), ALU.add, r=["y", "st"], w=["y"])
        P.tt("pool", sq[:], y[:], y[:], ALU.mult, r=["y"], w=["sq"])
        P.op("dve", lambda e: e.tensor_reduce(out=st[:], in_=v3(sq[:]), axis=AX.X, op=ALU.add), ["sq"], ["st"])
        P.ts("dve", st[:], st[:], 1.0 / 64, 64e-5, ALU.mult, ALU.add, r=["st"], w=["st"])
        P.act(st[:], st[:], AF.Sqrt, r=["st"], w=["st"])
        P.op("dve", lambda e: e.reciprocal(out=st[:], in_=st[:]), ["st"], ["st"])
        P.tt("dve", v3(y[:]), v3(y[:]), bc(st[:]), ALU.mult, r=["y", "st"], w=["y"])
        P.tt("dve", y[:], y[:], lng[:], ALU.mult, r=["y", "lng"], w=["y"])
        P.tt("dve", y[:], y[:], lnb[:], ALU.add, r=["y", "lnb"], w=["y"])
        P.tt("dve", y[:], y[:], bo[i][:], ALU.add, r=["y", ("bo", i)], w=["y"])
        P.tt("dve", cat[i][:, 512:1024], y[:], gt[i][:], ALU.mult, r=["y", ("gt", i)], w=[("cat", i, 1)])
        ck = [("cat", i, 0), ("cat", i, 1)]
        for k in range(8):
            P.tr(ptr[:, k, :], cat[i][:, k * 128:(k + 1) * 128], ident[:], r=ck + ["ident"], w=["ptr"])
        P.copy("act", catT[:], ptr[:], r=["ptr"], w=["catT"])
        for hf_ in range(2):
            for k in range(8):
                P.mm(pso[hf_][:], catT[:, k, :], wo[:, k, hf_ * 512:(hf_ + 1) * 512], start=(k == 0), stop=(k == 7), r=["catT"] + wok, w=[("pso", hf_)])
            P.tt("dve", xo[i][:, hf_ * 512:(hf_ + 1) * 512], pso[hf_][:], x3[i][:, hf_ * 512:(hf_ + 1) * 512], ALU.add,
                 r=[("pso", hf_), ("x3", i)], w=[("xo", i, hf_)])
        P.dma("sp", x4_o[ts_, :], xo[i][:], r=[("xo", i, 0), ("xo", i, 1)], w=[("x4_o", t)], is_out=True)
    return P.build()


def build_F():
    P = Prog()
    x_d = P.dram("x", [TOK, D], F32)
    pt_d = P.dram("parts", [8, TOK, D], F32)
    g_d = P.dram("g", [1, D], F32)
    o_d = P.dram("o", [TOK, D], F32, kind="ExternalOutput")
    gb = P.sb([128, D], F32)
    scr = norm_scratch(P)
    acc = [P.sb([128, D], F32) for _ in range(2)]
    prt = [P.sb([128, D], F32) for _ in range(2)]
    ob = [P.sb([128, D], F32) for _ in range(2)]
    P.dma("sp", gb[:], g_d.partition_broadcast(128), w=["gb"])
    for t in range(NT):
        a = acc[t % 2]
        ak = ("acc", t % 2)
        ts_ = slice(t * 128, (t + 1) * 128)
        P.dma("sp", a[:], x_d[ts_, :], w=[ak])
        for c in range(8):
            pb = prt[c % 2]
            P.dma("act" if c % 2 else "sp", pb[:], pt_d[c, ts_, :], w=[("prt", c % 2)])
            P.tt("dve" if c % 2 else "pool", a[:], a[:], pb[:], ALU.add, r=[ak, ("prt", c % 2)], w=[ak])
        rmsnorm_tile(P, a[:], ak, gb[:], ob[t % 2][:], ("ob", t % 2), scr)
        P.dma("sp", o_d[ts_, :], ob[t % 2][:], r=[("ob", t % 2)], w=[("o", t)], is_out=True)
    return P.build()


def run_O2(yf, yb, bonus, gt, yc, x3, prm):
    if "O2" not in _cache:
        _cache["O2"] = build_O2()
    nc = _cache["O2"]
    maps = []
    for c in range(8):
        maps.append({"yf": yf[c], "yb": yb[c], "bonus": bonus[c], "gt": gt[c], "yc": np.ascontiguousarray(yc[c]), "x3": np.ascontiguousarray(x3[c]),
                     "lng": prm["d_ln_g"].reshape(1, 512), "lnb": prm["d_ln_b"].reshape(1, 512), "wo": prm["o_w_out"], "ident": _ident_np()})
    res = run(nc, maps)
    return [res.results[c]["x4_o"] for c in range(8)]


def run_F(x5, parts, final_norm):
    if "F" not in _cache:
        _cache["F"] = build_F()
    nc = _cache["F"]
    maps = []
    for c in range(8):
        b, q = c // 4, c % 4
        maps.append({"x": np.ascontiguousarray(x5[c]), "parts": np.ascontiguousarray(np.stack([parts[cc][b, q * TOK:(q + 1) * TOK] for cc in range(8)], 0)),
                     "g": final_norm.reshape(1, D)})
    res = run(nc, maps)
    return [res.results[c]["o"] for c in range(8)]


def run_S(sc):
    if "S" not in _cache:
        _cache["S"] = build_S()
    nc = _cache["S"]
    ai = {n: i for i, n in enumerate(SCAN_ARRS)}
    full = [np.concatenate([np.asarray(sc[b * 4 + i]).reshape(len(SCAN_ARRS), 512, TOK) for i in range(4)], axis=2) for b in range(NB)]
    consts = scan_consts()
    maps = []
    for h in range(8):
        tm = np.zeros((4, 64, 4, S), np.float32)
        fm = np.zeros((5, 64, 4, S), np.float32)
        hs = slice(h * 64, (h + 1) * 64)
        for b in range(NB):
            for dr in range(2):
                ci = b * 2 + dr
                fl = (lambda a: a) if dr == 0 else (lambda a: a[:, ::-1])
                get = lambda n: fl(full[b][ai[n], hs, :])
                lw, r, a, k, bb, v = get("lw%d" % dr), get("r"), get("a"), get("kd%d" % dr), get("b%d" % dr), get("v")
                for j, arr in enumerate([lw, r, a, k, bb]):
                    fm[j, :, ci, :] = arr
                for j, arr in enumerate([lw, k, bb, v]):
                    tm[j, :, ci, :] = tm_layout(np.ascontiguousarray(arr.T))
        m = {"tm": tm, "fm": fm}
        m.update(consts)
        maps.append(m)
    res = run(nc, maps)
    yfull = np.zeros((NB, 2, S, 512), np.float32)
    for h in range(8):
        yo = np.asarray(res.results[h]["y"])
        for b in range(NB):
            for dr in range(2):
                yy = tm_unlayout(yo[:, b * 2 + dr, :])
                yfull[b, dr, :, h * 64:(h + 1) * 64] = yy if dr == 0 else yy[::-1]
    yf = [np.ascontiguousarray(yfull[c // 4, 0, (c % 4) * TOK:(c % 4 + 1) * TOK]) for c in range(8)]
    yb = [np.ascontiguousarray(yfull[c // 4, 1, (c % 4) * TOK:(c % 4 + 1) * TOK]) for c in range(8)]
    return yf, yb


def kernel(x, mem, e_norm, e_w_in, e_sink, e_w_out, o_norm, o_w_in, c_ln_g, c_ln_b, c_w_s, c_b_s, d_shift, d_w0, d_w2, d_a0, d_a2,
           d_g2, d_k_k, d_k_a, d_r_k, d_ln_g, d_ln_b, o_w_out, x_norm, m_norm, x_wq, x_wkv, x_wo, f_norm, f_router, f_w_gate,
           f_w_up, f_w_down, final_norm):
    f = lambda a: np.asarray(a, dtype=np.float32)
    x, mem = f(x), f(mem)
    qk, v = run_A1(x, f(e_norm)[0], f(e_w_in)[0])
    x1 = run_A2(qk, v, x, f(e_sink)[0], f(e_w_out)[0])
    x2, hf, aff = run_A3(x1, mem, f(x_norm)[0], f(m_norm)[0], f(x_wq)[0], f(x_wkv)[0], f(x_wo)[0], f(f_norm)[0], f(f_router)[0])
    parts = run_M(hf, aff, f(f_w_gate)[0], f(f_w_up)[0], f(f_w_down)[0])
    prm = {"o_norm": f(o_norm)[0], "o_w_in": f(o_w_in)[0], "c_ln_g": f(c_ln_g)[0], "c_ln_b": f(c_ln_b)[0], "c_w_s": f(c_w_s)[0],
           "c_b_s": f(c_b_s)[0], "d_shift": f(d_shift)[0], "d_w0": f(d_w0)[0], "d_w2": f(d_w2)[0], "d_a0": f(d_a0)[0], "d_a2": f(d_a2)[0],
           "d_g2": f(d_g2)[0], "d_k_k": f(d_k_k)[0], "d_k_a": f(d_k_a)[0], "d_r_k": f(d_r_k)[0], "d_ln_g": f(d_ln_g)[0],
           "d_ln_b": f(d_ln_b)[0], "o_w_out": f(o_w_out)[0]}
    x3, yc, sc = run_O1(x2, parts, prm)
    yf, yb = run_S(sc)
    ai = {n: i for i, n in enumerate(SCAN_ARRS)}
    tmaj = lambda c, n: np.ascontiguousarray(np.asarray(sc[c])[ai[n]].reshape(512, TOK).T)
    bonus = [tmaj(c, "bonus") for c in range(8)]
    gt = [tmaj(c, "g") for c in range(8)]
    x4 = run_O2(yf, yb, bonus, gt, yc, x3, prm)
    x5, hf, aff = run_A3(x4, mem, f(x_norm)[1], f(m_norm)[1], f(x_wq)[1], f(x_wkv)[1], f(x_wo)[1], f(f_norm)[1], f(f_router)[1])
    parts = run_M(hf, aff, f(f_w_gate)[1], f(f_w_up)[1], f(f_w_down)[1])
    o = run_F(x5, parts, f(final_norm))
    out = np.zeros((NB, S, D), np.float32)
    for c in range(8):
        out[c // 4, (c % 4) * TOK:(c % 4 + 1) * TOK] = np.asarray(o[c])
    return out
```
